# Optimizing a Trainium2 kernel written in Bass

```python
import jax
import jax.numpy as jnp
from jax import lax
import numpy as np


D_MODEL = 1024
BATCH = 8
SEQ = 4096
DEPTH = 1

HEAD_DIM = 64
ROPE_THETA = 10000.0
RMS_EPS = 1e-6
NEG_INF = -1e30
Q_BLOCK = 64

A_HEADS = 8
IDX_HEADS = 8
IDX_DIM = 32
DSA_TOPK_MAX = 256

B_HEADS = 8
B_KV_HEADS = 2
B_GROUP = B_HEADS // B_KV_HEADS
CMP_LEN = 32
CMP_STRIDE = 16
CMP_HIDDEN = 128
SLC_LEN = 64
SLC_TOPN = 16
WINDOW = 512

N_GROUPS = 4
EXPERTS_PER_GROUP = 8
N_EXPERTS = N_GROUPS * EXPERTS_PER_GROUP
TOP_K_INNER = 2
D_EXPERT = 256
MOE_BLOCK = 256

IN_SPLITS = (A_HEADS * HEAD_DIM, HEAD_DIM, HEAD_DIM, IDX_HEADS * IDX_DIM, IDX_DIM, IDX_HEADS,
             B_HEADS * HEAD_DIM, 6 * B_KV_HEADS * HEAD_DIM, 3 * B_HEADS, D_MODEL, D_MODEL)
IN_WIDTH = sum(IN_SPLITS)
SPLIT_POINTS = tuple(int(v) for v in np.cumsum(IN_SPLITS)[:-1])

kernel_name = "hybrid_dsa_nsa_hmoe_block"


def rmsnorm(x, g):
    xf = x.astype(jnp.float32)
    y = xf * lax.rsqrt(jnp.mean(xf * xf, axis=-1, keepdims=True) + RMS_EPS)
    return (y * g.astype(jnp.float32)).astype(x.dtype)


def rope(x, pos):
    half = x.shape[-1] // 2
    inv_freq = ROPE_THETA ** (-jnp.arange(half, dtype=jnp.float32) / half)
    ang = pos.astype(jnp.float32)[:, :, None] * inv_freq
    cos = jnp.cos(ang)[:, :, None, :]
    sin = jnp.sin(ang)[:, :, None, :]
    xf = x.astype(jnp.float32)
    x1, x2 = xf[..., :half], xf[..., half:]
    return jnp.concatenate([x1 * cos - x2 * sin, x1 * sin + x2 * cos], axis=-1).astype(x.dtype)


def masked_softmax(s, mask):
    p = jax.nn.softmax(jnp.where(mask, s.astype(jnp.float32), NEG_INF), axis=-1)
    return jnp.where(mask, p, 0.0)


def _rows(a, t0):
    return lax.dynamic_slice_in_dim(a, t0, Q_BLOCK, axis=1)


def dsa_attention(q, k, v, qi, ki, wi):
    bsz, L = q.shape[0], q.shape[1]
    k_sel = min(DSA_TOPK_MAX, L // 4)
    key_pos = jnp.arange(L)
    b_ix = jnp.arange(bsz)[:, None, None]
    scale = HEAD_DIM ** -0.5

    def block(i):
        t0 = i * Q_BLOCK
        qpos = t0 + jnp.arange(Q_BLOCK)
        causal = (key_pos[None, :] <= qpos[:, None])[None]
        idx_logit = jax.nn.relu(jnp.einsum('bthd,bsd->bths', _rows(qi, t0), ki))
        score = jnp.einsum('bth,bths->bts', _rows(wi, t0), idx_logit).astype(jnp.float32)
        score = jnp.where(causal, score, -jnp.inf)
        _, sel = lax.top_k(score, k_sel)
        ks = k[b_ix, sel]
        vs = v[b_ix, sel]
        s = jnp.einsum('bthd,btkd->bthk', _rows(q, t0), ks) * scale
        mask = (sel <= qpos[None, :, None])[:, :, None, :]
        p = masked_softmax(s, mask).astype(vs.dtype)
        return jnp.einsum('bthk,btkd->bthd', p, vs)

    out = lax.map(block, jnp.arange(L // Q_BLOCK))
    return out.transpose(1, 0, 2, 3, 4).reshape(bsz, L, A_HEADS * HEAD_DIM)


def compress_tokens(kv, pe, w1, w2):
    bsz, L, hk, dh = kv.shape
    n_cmp = (L - CMP_LEN) // CMP_STRIDE + 1
    idx = np.arange(n_cmp)[:, None] * CMP_STRIDE + np.arange(CMP_LEN)[None, :]
    blocks = kv[:, idx] + pe[:, None, :]
    z = blocks.transpose(0, 1, 3, 2, 4).reshape(bsz, n_cmp, hk, CMP_LEN * dh)
    return jax.nn.silu(z @ w1) @ w2


def nsa_attention(q_nope, q_rope, k_cmp, v_cmp, k_slc, v_slc, k_win, v_win, gates,
                  pe_k, w1_k, w2_k, pe_v, w1_v, w2_v):
    bsz, L = q_nope.shape[0], q_nope.shape[1]
    scale = HEAD_DIM ** -0.5
    kc = compress_tokens(k_cmp, pe_k, w1_k, w2_k)
    vc = compress_tokens(v_cmp, pe_v, w1_v, w2_v)
    n_cmp = kc.shape[1]
    cmp_start = np.arange(n_cmp) * CMP_STRIDE
    cmp_end = jnp.asarray(cmp_start + CMP_LEN - 1)
    n_slc = L // SLC_LEN
    n_sel = min(SLC_TOPN, n_slc)
    slc_start = np.arange(n_slc) * SLC_LEN
    overlap = (cmp_start[:, None] < slc_start[None, :] + SLC_LEN) & (cmp_start[:, None] + CMP_LEN > slc_start[None, :])
    cmp_to_slc = jnp.asarray(overlap.astype(np.float32))
    k_blk = k_slc.reshape(bsz, n_slc, SLC_LEN, B_KV_HEADS, HEAD_DIM).transpose(0, 3, 1, 2, 4)
    v_blk = v_slc.reshape(bsz, n_slc, SLC_LEN, B_KV_HEADS, HEAD_DIM).transpose(0, 3, 1, 2, 4)
    k_pad = jnp.pad(k_win, ((0, 0), (WINDOW, 0), (0, 0), (0, 0)))
    v_pad = jnp.pad(v_win, ((0, 0), (WINDOW, 0), (0, 0), (0, 0)))
    b_ix = jnp.arange(bsz)[:, None, None, None]
    h_ix = jnp.arange(B_KV_HEADS)[None, None, :, None]
    j_ix = jnp.arange(n_slc)

    def block(i):
        t0 = i * Q_BLOCK
        qpos = t0 + jnp.arange(Q_BLOCK)
        qn = _rows(q_nope, t0).reshape(bsz, Q_BLOCK, B_KV_HEADS, B_GROUP, HEAD_DIM)
        qr = _rows(q_rope, t0).reshape(bsz, Q_BLOCK, B_KV_HEADS, B_GROUP, HEAD_DIM)
        g = _rows(gates, t0)
        s = jnp.einsum('btkgd,bnkd->btkgn', qn, kc) * scale
        m_cmp = (cmp_end[None, :] <= qpos[:, None])[None, :, None, None, :]
        p_cmp = masked_softmax(s, m_cmp)
        o_cmp = jnp.einsum('btkgn,bnkd->btkgd', p_cmp.astype(vc.dtype), vc)
        imp = jnp.einsum('btkgn,nj->btkj', p_cmp, cmp_to_slc)
        admiss = j_ix[None, :] * SLC_LEN <= qpos[:, None]
        cur = qpos[:, None] // SLC_LEN
        forced = admiss & ((j_ix[None, :] == 0) | (j_ix[None, :] == cur) | (j_ix[None, :] == cur - 1))
        score = jnp.where(admiss[None, :, None, :], imp, -jnp.inf)
        score = jnp.where(forced[None, :, None, :], jnp.inf, score)
        _, sel = lax.top_k(score, n_sel)
        ks = k_blk[b_ix, h_ix, sel].reshape(bsz, Q_BLOCK, B_KV_HEADS, n_sel * SLC_LEN, HEAD_DIM)
        vs = v_blk[b_ix, h_ix, sel].reshape(bsz, Q_BLOCK, B_KV_HEADS, n_sel * SLC_LEN, HEAD_DIM)
        tok = (sel[..., None] * SLC_LEN + jnp.arange(SLC_LEN)).reshape(bsz, Q_BLOCK, B_KV_HEADS, n_sel * SLC_LEN)
        m_sel = (tok <= qpos[None, :, None, None])[:, :, :, None, :]
        s = jnp.einsum('btkgd,btksd->btkgs', qr, ks) * scale
        p = masked_softmax(s, m_sel).astype(vs.dtype)
        o_slc = jnp.einsum('btkgs,btksd->btkgd', p, vs)
        kw = lax.dynamic_slice_in_dim(k_pad, t0, WINDOW + Q_BLOCK, axis=1)
        vw = lax.dynamic_slice_in_dim(v_pad, t0, WINDOW + Q_BLOCK, axis=1)
        kpos = t0 - WINDOW + jnp.arange(WINDOW + Q_BLOCK)
        rel = qpos[:, None] - kpos[None, :]
        m_win = ((rel >= 0) & (rel < WINDOW) & (kpos[None, :] >= 0))[None, :, None, None, :]
        s = jnp.einsum('btkgd,bskd->btkgs', qr, kw) * scale
        p = masked_softmax(s, m_win).astype(vw.dtype)
        o_win = jnp.einsum('btkgs,bskd->btkgd', p, vw)
        gc = g[:, :, 0].reshape(bsz, Q_BLOCK, B_KV_HEADS, B_GROUP, 1)
        gs = g[:, :, 1].reshape(bsz, Q_BLOCK, B_KV_HEADS, B_GROUP, 1)
        gw = g[:, :, 2].reshape(bsz, Q_BLOCK, B_KV_HEADS, B_GROUP, 1)
        o = gc * o_cmp + gs * o_slc + gw * o_win
        return o.reshape(bsz, Q_BLOCK, B_HEADS * HEAD_DIM)

    out = lax.map(block, jnp.arange(L // Q_BLOCK))
    return out.transpose(1, 0, 2, 3).reshape(bsz, L, B_HEADS * HEAD_DIM)


def mixer_layer(h, positions, w_in, pe_k, w1_k, w2_k, pe_v, w1_v, w2_v, w_br_a, w_br_b, w_out):
    bsz, L, _ = h.shape
    proj = jnp.einsum('bsd,dc->bsc', h, w_in)
    qa, ka, va, qi, ki, wi, qb, kvb, gb, gate_a, gate_b = jnp.split(proj, SPLIT_POINTS, axis=-1)
    qa = rope(qa.reshape(bsz, L, A_HEADS, HEAD_DIM), positions)
    ka = rope(ka[:, :, None, :], positions)[:, :, 0]
    qi = rope(qi.reshape(bsz, L, IDX_HEADS, IDX_DIM), positions)
    ki = rope(ki[:, :, None, :], positions)[:, :, 0]
    o_a = dsa_attention(qa, ka, va, qi, ki, wi)
    qb = qb.reshape(bsz, L, B_HEADS, HEAD_DIM)
    kvb = kvb.reshape(bsz, L, 6, B_KV_HEADS, HEAD_DIM)
    k_cmp, v_cmp = kvb[:, :, 0], kvb[:, :, 1]
    k_slc, v_slc = rope(kvb[:, :, 2], positions), kvb[:, :, 3]
    k_win, v_win = rope(kvb[:, :, 4], positions), kvb[:, :, 5]
    nsa_gates = jax.nn.sigmoid(gb.reshape(bsz, L, 3, B_HEADS))
    o_b = nsa_attention(qb, rope(qb, positions), k_cmp, v_cmp, k_slc, v_slc, k_win, v_win, nsa_gates,
                        pe_k, w1_k, w2_k, pe_v, w1_v, w2_v)
    merged = jax.nn.sigmoid(gate_a) * (o_a @ w_br_a) + jax.nn.sigmoid(gate_b) * (o_b @ w_br_b)
    return merged @ w_out


def hier_moe(h, w_group, b_group, w_expert, b_expert, w_gate_up, w_down):
    bsz, L, d = h.shape
    n_tok = bsz * L
    tok = h.reshape(n_tok, d)
    g_logit = jnp.einsum('nd,dg->ng', tok, w_group).astype(jnp.float32) + b_group.astype(jnp.float32)
    g_prob = jax.nn.softmax(g_logit, axis=-1)
    g_sel = jnp.argmax(g_logit, axis=-1)
    e_logit = jnp.einsum('nd,de->ne', tok, w_expert).astype(jnp.float32) + b_expert.astype(jnp.float32)
    e_in = jnp.take_along_axis(e_logit.reshape(n_tok, N_GROUPS, EXPERTS_PER_GROUP), g_sel[:, None, None], axis=1)[:, 0]
    top_val, top_idx = lax.top_k(e_in, TOP_K_INNER)
    weight = jnp.take_along_axis(g_prob, g_sel[:, None], axis=1) * jax.nn.softmax(top_val, axis=-1)
    expert = g_sel[:, None] * EXPERTS_PER_GROUP + top_idx
    n_asg = n_tok * TOP_K_INNER
    e_flat = expert.reshape(n_asg)
    tok_flat = jnp.repeat(jnp.arange(n_tok), TOP_K_INNER)
    w_flat = weight.reshape(n_asg)
    order = jnp.argsort(e_flat)
    e_s, tok_s, w_s = e_flat[order], tok_flat[order], w_flat[order]
    counts = jnp.bincount(e_flat, length=N_EXPERTS)
    starts = jnp.cumsum(counts) - counts
    padded = (counts + MOE_BLOCK - 1) // MOE_BLOCK * MOE_BLOCK
    pad_end = jnp.cumsum(padded)
    pad_start = pad_end - padded
    dest = pad_start[e_s] + jnp.arange(n_asg) - starts[e_s]
    n_blocks = -(-n_asg // MOE_BLOCK) + N_EXPERTS
    buf = jnp.zeros((n_blocks * MOE_BLOCK, d), h.dtype).at[dest].set(tok[tok_s])
    blk_expert = jnp.minimum(jnp.searchsorted(pad_end, jnp.arange(n_blocks) * MOE_BLOCK, side='right'), N_EXPERTS - 1)

    def run(args):
        xb, e = args
        gate, up = jnp.split(xb @ w_gate_up[e], 2, axis=-1)
        return (jax.nn.silu(gate) * up) @ w_down[e]

    y_buf = lax.map(run, (buf.reshape(n_blocks, MOE_BLOCK, d), blk_expert)).reshape(n_blocks * MOE_BLOCK, d)
    y = jax.ops.segment_sum(y_buf[dest] * w_s[:, None].astype(h.dtype), tok_s, num_segments=n_tok)
    return y.reshape(bsz, L, d)


def setup_inputs(seed: int = 0) -> dict:
    key = jax.random.key(seed)
    ks = jax.random.split(key, 24)

    def nrm(k, shape, scale):
        return jax.random.normal(k, shape, jnp.float32) * scale

    cmp_in = CMP_LEN * HEAD_DIM
    a_w = A_HEADS * HEAD_DIM
    b_w = B_HEADS * HEAD_DIM
    offs = jax.random.randint(ks[1], (BATCH, 1), 0, 1024, dtype=jnp.int32)
    return {
        "x": nrm(ks[0], (BATCH, SEQ, D_MODEL), 1.0),
        "positions": offs + jnp.arange(SEQ, dtype=jnp.int32)[None, :],
        "norm_mix": 1.0 + nrm(ks[2], (DEPTH, D_MODEL), 0.02),
        "w_in": nrm(ks[3], (DEPTH, D_MODEL, IN_WIDTH), D_MODEL ** -0.5),
        "pe_k": nrm(ks[4], (DEPTH, CMP_LEN, HEAD_DIM), 0.1),
        "w1_k": nrm(ks[5], (DEPTH, cmp_in, CMP_HIDDEN), cmp_in ** -0.5),
        "w2_k": nrm(ks[6], (DEPTH, CMP_HIDDEN, HEAD_DIM), CMP_HIDDEN ** -0.5),
        "pe_v": nrm(ks[7], (DEPTH, CMP_LEN, HEAD_DIM), 0.1),
        "w1_v": nrm(ks[8], (DEPTH, cmp_in, CMP_HIDDEN), cmp_in ** -0.5),
        "w2_v": nrm(ks[9], (DEPTH, CMP_HIDDEN, HEAD_DIM), CMP_HIDDEN ** -0.5),
        "w_br_a": nrm(ks[10], (DEPTH, a_w, D_MODEL), a_w ** -0.5),
        "w_br_b": nrm(ks[11], (DEPTH, b_w, D_MODEL), b_w ** -0.5),
        "w_out": nrm(ks[12], (DEPTH, D_MODEL, D_MODEL), D_MODEL ** -0.5),
        "norm_ffn": 1.0 + nrm(ks[13], (DEPTH, D_MODEL), 0.02),
        "w_group": nrm(ks[14], (DEPTH, D_MODEL, N_GROUPS), D_MODEL ** -0.5),
        "b_group": nrm(ks[15], (DEPTH, N_GROUPS), 0.01),
        "w_expert": nrm(ks[16], (DEPTH, D_MODEL, N_EXPERTS), D_MODEL ** -0.5),
        "b_expert": nrm(ks[17], (DEPTH, N_EXPERTS), 0.01),
        "w_gate_up": nrm(ks[18], (DEPTH, N_EXPERTS, D_MODEL, 2 * D_EXPERT), D_MODEL ** -0.5),
        "w_down": nrm(ks[19], (DEPTH, N_EXPERTS, D_EXPERT, D_MODEL), D_EXPERT ** -0.5),
        "norm_final": 1.0 + nrm(ks[20], (D_MODEL,), 0.02),
    }


def reference(x, positions, norm_mix, w_in, pe_k, w1_k, w2_k, pe_v, w1_v, w2_v, w_br_a, w_br_b,
              w_out, norm_ffn, w_group, b_group, w_expert, b_expert, w_gate_up, w_down, norm_final):
    for l in range(DEPTH):
        h = rmsnorm(x, norm_mix[l])
        x = x + mixer_layer(h, positions, w_in[l], pe_k[l], w1_k[l], w2_k[l], pe_v[l], w1_v[l], w2_v[l],
                            w_br_a[l], w_br_b[l], w_out[l])
        h = rmsnorm(x, norm_ffn[l])
        x = x + hier_moe(h, w_group[l], b_group[l], w_expert[l], b_expert[l], w_gate_up[l], w_down[l])
    return rmsnorm(x, norm_final)
```

```python
from contextlib import ExitStack
import numpy as np
import concourse.bass as bass
import concourse.mybir as mybir
from concourse.bass_utils import run_bass_kernel_spmd

F32 = mybir.dt.float32
BF16 = mybir.dt.bfloat16
I32 = mybir.dt.int32
ALU = mybir.AluOpType
ACT = mybir.ActivationFunctionType
AX = mybir.AxisListType

ENGS = ("tensor", "vector", "scalar", "gpsimd", "sync")

L = 4096
D = 1024
NT = 32
NCH = 8
EPS = 1e-6
PI = float(np.pi)
TWO_PI = float(2 * np.pi)

QA, KA, VA, QI, KI, WI, QB = 0, 512, 576, 640, 896, 928, 936
KC, VC, KS, VS, KW, VW, GB, GA, GBT = 1448, 1576, 1704, 1832, 1960, 2088, 2216, 2240, 3264


class Tok:
    __slots__ = ("last_w", "readers")

    def __init__(self):
        self.last_w = None
        self.readers = []


class Op:
    __slots__ = ("eng", "fn", "deps", "is_dma", "sig", "signal", "idx", "prev")

    def __init__(self, eng, fn, is_dma):
        self.eng = eng
        self.fn = fn
        self.deps = set()
        self.is_dma = is_dma
        self.sig = None
        self.signal = False
        self.prev = None


class Prog:
    N_DMA_SEMS = 8

    def __init__(self, nc, ctx):
        self.nc = nc
        self.ops = []
        self.done = 0
        self.eng_sems = {e: ctx.enter_context(nc.semaphore(f"s_{e}")) for e in ENGS}
        self.dma_sems = {e: [ctx.enter_context(nc.semaphore(f"d_{e}_{i}")) for i in range(self.N_DMA_SEMS)]
                         for e in ENGS}
        self.eng_cnt = {e: 0 for e in ENGS}
        self.dma_rr = {e: 0 for e in ENGS}
        self.dma_cnt = {e: [0] * self.N_DMA_SEMS for e in ENGS}

    def _add(self, eng, fn, reads, writes, is_dma=False):
        op = Op(eng, fn, is_dma)
        op.idx = len(self.ops)
        for t in reads:
            if t.last_w is not None:
                op.deps.add(t.last_w)
        for t in writes:
            if t.last_w is not None:
                op.deps.add(t.last_w)
            op.deps.update(t.readers)
        op.deps.discard(op.idx)
        for t in reads:
            t.readers.append(op.idx)
        for t in writes:
            t.last_w = op.idx
            t.readers = []
        self.ops.append(op)
        return op

    def op(self, eng, fn, reads=(), writes=()):
        return self._add(eng, fn, list(reads), list(writes))

    def dma(self, eng, out, in_, reads=(), writes=(), **kw):
        return self._add(eng, lambda e: e.dma_start(out=out, in_=in_, **kw), list(reads), list(writes), True)

    def _sem(self, key):
        return self.eng_sems[key[1]] if key[0] == "e" else self.dma_sems[key[1]][key[2]]

    def emit(self, final=False):
        nc = self.nc
        ops = self.ops
        new = ops[self.done:]
        pre = []
        for e in ENGS:
            if self.eng_cnt[e] > 0:
                pre.append((("e", e), self.eng_cnt[e]))
            for k in range(self.N_DMA_SEMS):
                if self.dma_cnt[e][k] > 0:
                    pre.append((("d", e, k), self.dma_cnt[e][k]))
        for op in new:
            for d in op.deps:
                if d >= self.done:
                    ops[d].signal = True
        per_eng = {e: [] for e in ENGS}
        for op in new:
            per_eng[op.eng].append(op)
        for e in ENGS:
            for op in reversed(per_eng[e]):
                if not op.is_dma:
                    op.signal = True
                    break
        for op in new:
            if op.is_dma:
                k = self.dma_rr[op.eng]
                self.dma_rr[op.eng] = (k + 1) % self.N_DMA_SEMS
                prev = self.dma_cnt[op.eng][k]
                self.dma_cnt[op.eng][k] = prev + 16
                op.sig = (("d", op.eng, k), prev + 16)
                op.prev = (("d", op.eng, k), prev)
            elif op.signal:
                self.eng_cnt[op.eng] += 1
                op.sig = (("e", op.eng), self.eng_cnt[op.eng])
        finals = []
        if final:
            for e in ENGS:
                for k in range(self.N_DMA_SEMS):
                    if self.dma_cnt[e][k] > 0:
                        finals.append((("d", e, k), self.dma_cnt[e][k]))
        done = self.done

        def run_engine(ename, eobj):
            known = {}
            for key, val in pre:
                if key == ("e", ename):
                    continue
                eobj.wait_ge(self._sem(key), val)
                known[key] = val
            for op in per_eng[ename]:
                waits = {}
                for d in op.deps:
                    if d < done:
                        continue
                    dop = ops[d]
                    key, val = dop.sig
                    if ename == "tensor" and dop.eng == "tensor" and not dop.is_dma:
                        continue
                    if known.get(key, 0) >= val:
                        continue
                    waits[key] = max(waits.get(key, 0), val)
                if op.is_dma:
                    key, val = op.prev
                    if val > 0 and known.get(key, 0) < val:
                        waits[key] = max(waits.get(key, 0), val)
                for key, val in waits.items():
                    eobj.wait_ge(self._sem(key), val)
                    known[key] = val
                ins = op.fn(eobj)
                if op.sig is not None:
                    ins.then_inc(self._sem(op.sig[0]), 16 if op.is_dma else 1)
            if ename == "sync":
                for key, val in finals:
                    if known.get(key, 0) < val:
                        eobj.wait_ge(self._sem(key), val)

        with nc.Block() as block:
            @block.tensor
            def _(e):
                run_engine("tensor", e)

            @block.vector
            def _(e):
                run_engine("vector", e)

            @block.scalar
            def _(e):
                run_engine("scalar", e)

            @block.gpsimd
            def _(e):
                run_engine("gpsimd", e)

            @block.sync
            def _(e):
                run_engine("sync", e)
        self.done = len(ops)


class Buf:
    def __init__(self, t):
        self.t = t
        self.k = Tok()

    def __getitem__(self, key):
        return self.t[key]


class View:
    def __init__(self, ap, k):
        self.ap = ap
        self.k = k

    def __getitem__(self, key):
        return self.ap[key]


class KB:
    def __init__(self, nc, dbg=None):
        self.nc = nc
        self.n = 0
        self.dbg = dbg if dbg is not None else {}
        self.dbg_out = {}

    def sb(self, ctx, shape, dt=F32):
        self.n += 1
        return Buf(ctx.enter_context(self.nc.sbuf_tensor(f"sb{self.n}", list(shape), dt)))

    def ps(self, ctx, shape, dt=F32):
        self.n += 1
        b = Buf(ctx.enter_context(self.nc.psum_tensor(f"ps{self.n}", list(shape), dt)))
        b.is_psum = True
        return b

    def dump(self, name, ap, shape, dt, reads):
        if name not in self.dbg:
            return
        d = self.nc.dram_tensor("dbg_" + name, list(shape), dt, kind="ExternalOutput").ap()
        self.dbg_out[name] = d
        self.P.dma("sync", d, ap, reads=reads)

    @staticmethod
    def _tk(lst):
        return [b.k if hasattr(b, "k") else b for b in lst]

    @staticmethod
    def _rw(r, w):
        rr_, ww_ = [], list(w)
        for b in r:
            if getattr(b, "is_psum", False):
                if b not in ww_:
                    ww_.append(b)
            else:
                rr_.append(b)
        tk = lambda lst: [b.k if hasattr(b, "k") else b for b in lst]
        return tk(rr_), tk(ww_)

    def mm(self, out, lhsT, rhs, start, stop, r, w):
        self.P.op("tensor", lambda e: e.matmul(out, lhsT=lhsT, rhs=rhs, start=start, stop=stop), *self._rw(r, w))

    def tr(self, out, in_, ident, r, w):
        self.P.op("tensor", lambda e: e.transpose(out=out, in_=in_, identity=ident), *self._rw(r, w))

    def act(self, out, in_, func, r, w, **kw):
        self.P.op("scalar", lambda e: e.activation(out=out, in_=in_, func=func, **kw), *self._rw(r, w))

    def tt(self, eng, out, in0, in1, op, r, w):
        self.P.op(eng, lambda e: e.tensor_tensor(out=out, in0=in0, in1=in1, op=op), *self._rw(r, w))

    def ts(self, eng, out, in0, s1, s2, op0, op1, r, w, accum_out=None):
        if op1 is None:
            self.P.op(eng, lambda e: e.tensor_scalar(out=out, in0=in0, scalar1=s1, scalar2=None, op0=op0), *self._rw(r, w))
        elif accum_out is None:
            self.P.op(eng, lambda e: e.tensor_scalar(out=out, in0=in0, scalar1=s1, scalar2=s2, op0=op0, op1=op1), *self._rw(r, w))
        else:
            self.P.op(eng, lambda e: e.tensor_scalar(out=out, in0=in0, scalar1=s1, scalar2=s2, op0=op0, op1=op1, accum_out=accum_out), *self._rw(r, w))

    def stt(self, eng, out, in0, scalar, in1, op0, op1, r, w):
        self.P.op(eng, lambda e: e.scalar_tensor_tensor(out=out, in0=in0, scalar=scalar, in1=in1, op0=op0, op1=op1), *self._rw(r, w))

    def cp(self, eng, out, in_, r, w):
        if eng == "scalar":
            self.P.op(eng, lambda e: e.copy(out=out, in_=in_), *self._rw(r, w))
        else:
            self.P.op(eng, lambda e: e.tensor_copy(out=out, in_=in_), *self._rw(r, w))

    def memset(self, eng, ap, val, w):
        self.P.op(eng, lambda e: e.memset(ap, val), [], self._tk(w))

    def asel(self, out, in_, pattern, cmp, fill, base, cm, r, w):
        self.P.op("gpsimd", lambda e: e.affine_select(out=out, in_=in_, pattern=pattern, compare_op=cmp, fill=fill, base=base, channel_multiplier=cm), *self._rw(r, w))

    def recip(self, out, in_, r, w):
        self.P.op("vector", lambda e: e.reciprocal(out=out, in_=in_), *self._rw(r, w))

    def dma(self, eng, out, in_, r, w, **kw):
        r_, w_ = self._rw(r, w)
        self.P.dma(eng, out, in_, r_, w_, **kw)


def bc(ap, shape):
    return ap.to_broadcast(list(shape))


def build(dbg=None, qtiles=None, stop_after=None, moe_experts=32, lvl=99, moe_from_x=False, skip_att=False):
    nc = bass.Bass("TRN2", target_bir_lowering=False)
    K = KB(nc, dbg)

    def din(name, shape, dt=F32):
        return nc.dram_tensor(name, list(shape), dt, kind="ExternalInput").ap()

    x_d = din("x", [L, D])
    pos_d = din("positions", [1, L], I32)
    norm_mix_d = din("norm_mix", [128, 8])
    w_in_d = din("w_in", [D, 4288])
    pe_k_d = din("pe_k", [32, 64]); w1_k_d = din("w1_k", [2048, 128]); w2_k_d = din("w2_k", [128, 64])
    pe_v_d = din("pe_v", [32, 64]); w1_v_d = din("w1_v", [2048, 128]); w2_v_d = din("w2_v", [128, 64])
    w_br_a_d = din("w_br_a", [512, D]); w_br_b_d = din("w_br_b", [512, D]); w_out_d = din("w_out", [D, D])
    norm_ffn_d = din("norm_ffn", [128, 8])
    w_group_d = din("w_group", [D, 4]); b_group_d = din("b_group", [1, 4])
    w_expert_d = din("w_expert", [D, 32]); b_expert_d = din("b_expert", [1, 32])
    w_gu_d = din("w_gate_up", [32, D, 512]); w_dn_d = din("w_down", [32, 256, D])
    norm_final_d = din("norm_final", [1, D])
    c_invf_d = din("c_invf", [128, 2])
    c_selb_d = din("c_selb", [64, 64])
    c_esel_d = din("c_esel", [64, 4096])
    c_eye_d = din("c_eye", [24, 24])
    out_d = nc.dram_tensor("out", [L, D], F32, kind="ExternalOutput").ap()
    x1_d = nc.dram_tensor("x1s", [L, D], F32, kind="Internal").ap()
    NG_Q = 40
    wq_d = nc.dram_tensor("wq_bf", [NG_Q, 128, 1024], BF16, kind="Internal").ap()

    w_in_v = w_in_d.rearrange("(c p) n -> p c n", p=128)
    wq_tok = Tok()
    x1_tok = Tok()

    with ExitStack() as top:
        P = Prog(nc, top)
        K.P = P
        ident = K.sb(top, [128, 128], BF16)
        diagT = K.sb(top, [128, 128], BF16)
        antiT = K.sb(top, [128, 128], BF16)
        invf = K.sb(top, [128, 2])
        gmix = K.sb(top, [128, 8])
        cpi = K.sb(top, [128, 2])
        with ExitStack() as c0:
            tmpf = K.sb(c0, [128, 128])
            K.memset("gpsimd", tmpf[:], 1.0, [tmpf])
            K.asel(tmpf[:], tmpf[:], [[-1, 128]], ALU.is_equal, 0.0, 0, 1, [tmpf], [tmpf])
            K.cp("vector", ident[:], tmpf[:], [tmpf], [ident])
            tmp2 = K.sb(c0, [128, 128])
            K.memset("gpsimd", tmp2[:], 1.0, [tmp2])
            K.asel(tmp2[:], tmp2[:], [[1, 128]], ALU.is_ge, 0.0, 0, -1, [tmp2], [tmp2])
            K.cp("vector", diagT[:], tmp2[:], [tmp2], [diagT])
            tmp3 = K.sb(c0, [128, 128])
            K.memset("gpsimd", tmp3[:], 1.0, [tmp3])
            K.asel(tmp3[:], tmp3[:], [[-1, 128]], ALU.is_gt, 0.0, 0, 1, [tmp3], [tmp3])
            K.cp("vector", antiT[:], tmp3[:], [tmp3], [antiT])
            K.dma("sync", invf[:], c_invf_d, [], [invf])
            K.dma("sync", gmix[:], norm_mix_d, [], [gmix])
            K.memset("vector", cpi[:, 0:1], PI / 2, [cpi])
            K.memset("vector", cpi[:, 1:2], EPS, [cpi])
            P.emit()

        banks = [K.ps(top, [128, 512]) for _ in range(8)]
        rr = [0]

        def bank(pool=(0, 1, 2, 3)):
            b = banks[pool[rr[0] % len(pool)]]
            rr[0] += 1
            return b

        def bfv(b):
            return b.t[:].bitcast(BF16)

        def make_hT(c, hTc, xb, sq, hn, st, g):
            for j in range(4):
                tt_ = 4 * c + j
                xt = xb[j % len(xb)]
                K.dma("sync", xt[:], x_d[tt_ * 128:(tt_ + 1) * 128, :], [], [xt])
                K.memset("vector", st[:, 0:1], 0.0, [st])
                K.act(sq[:], xt[:], ACT.Square, [xt, st], [sq, st], accum_out=st[:, 0:1])
                K.act(st[:, 1:2], st[:, 0:1], ACT.Sqrt, [st, cpi], [st], scale=1.0 / D, bias=cpi[:, 1:2])
                K.recip(st[:, 2:3], st[:, 1:2], [st], [st])
                K.ts("vector", hn[:], xt[:], st[:, 2:3], None, ALU.mult, None, [xt, st], [hn])
                pb = bank()
                for k in range(8):
                    K.tr(bfv(pb)[:, k * 128:(k + 1) * 128], hn[:, k * 128:(k + 1) * 128], ident[:], [hn, ident], [pb])
                K.tt("vector", hTc[:, :, j * 128:(j + 1) * 128], bfv(pb).rearrange("p (k t) -> p k t", k=8),
                     bc(g[:, :].unsqueeze(2), [128, 8, 128]), ALU.mult, [pb, g], [hTc])

        def make_rope(c, posi, posf, tq, tabs):
            K.dma("sync", posi[:], pos_d[:, c * 512:(c + 1) * 512].partition_broadcast(128), [], [posi])
            K.cp("vector", posf[:], posi[:], [posi], [posf])
            for col, (cn, sn) in enumerate((("cos64", "sin64"), ("cos32", "sin32"))):
                ang = tq[0]; kf = tq[1]; ki = tq[2]
                K.ts("vector", ang[:], posf[:], invf[:, col:col + 1], None, ALU.mult, None, [posf, invf], [ang])
                K.ts("vector", ki[:], ang[:], 1.0 / TWO_PI, None, ALU.mult, None, [ang], [ki])
                K.cp("vector", kf[:], ki[:], [ki], [kf])
                K.stt("vector", ang[:], kf[:], -TWO_PI, ang[:], ALU.mult, ALU.add, [kf, ang], [ang])
                K.ts("vector", kf[:], ang[:], PI, -TWO_PI, ALU.is_gt, ALU.mult, [ang], [kf])
                K.tt("vector", ang[:], ang[:], kf[:], ALU.add, [ang, kf], [ang])
                K.ts("vector", kf[:], ang[:], -PI, TWO_PI, ALU.is_lt, ALU.mult, [ang], [kf])
                K.tt("vector", ang[:], ang[:], kf[:], ALU.add, [ang, kf], [ang])
                K.act(tabs[sn][:], ang[:], ACT.Sin, [ang], [tabs[sn]])
                K.stt("vector", kf[:], ang[:], -1.0, ang[:], ALU.mult, ALU.max, [ang], [kf])
                K.act(tabs[cn][:], kf[:], ACT.Sin, [kf, cpi], [tabs[cn]], scale=-1.0, bias=cpi[:, 0:1])

        def proj_fm(hTc, wA, M, dst, dstb, wB=None, cos=None, sin=None, tmp=None, evac="scalar", func=None, N=512):
            pa = bank()
            for k in range(8):
                K.mm(pa[0:M, 0:N], wA[0][:, k, :], hTc[:, k, 0:N], k == 0, k == 7, [wA[1], hTc], [pa])
            if wB is None:
                if func is not None:
                    K.act(dst, pa[0:M, 0:N], func, [pa], [dstb])
                else:
                    K.cp(evac, dst, pa[0:M, 0:N], [pa], [dstb])
                return
            pb = bank()
            for k in range(8):
                K.mm(pb[0:M, 0:N], wB[0][:, k, :], hTc[:, k, 0:N], k == 0, k == 7, [wB[1], hTc], [pb])
            t1, t2 = tmp
            K.tt("vector", t1[0:M, 0:N], pa[0:M, 0:N], cos[0:M, 0:N], ALU.mult, [pa, cos], [t1])
            K.tt("vector", t2[0:M, 0:N], pb[0:M, 0:N], sin[0:M, 0:N], ALU.mult, [pb, sin], [t2])
            K.tt("gpsimd", dst, t1[0:M, 0:N], t2[0:M, 0:N], ALU.add, [t1, t2], [dstb])

        att = ExitStack()
        kaT2 = K.sb(att, [128, L], BF16)
        kiT3 = K.sb(att, [96, L], BF16)
        ksT = K.sb(att, [128, L], BF16)
        kwT = K.sb(att, [128, L], BF16)
        va3 = K.sb(att, [128, NT, 192], BF16)
        vs3 = K.sb(att, [128, NT, 192], BF16)
        vw3 = K.sb(att, [128, NT, 192], BF16)
        kcT2 = K.sb(att, [128, 256], BF16)
        vctm = K.sb(att, [128, 2, 128], BF16)

        with ExitStack() as pw:
            stg = [K.sb(pw, [128, 8, 512]) for _ in range(2)]
            grp = [K.sb(pw, [128, 8, 128], BF16) for _ in range(3)]
            wBfm = K.sb(pw, [128, 8, 1248], BF16)
            wBtm = K.sb(pw, [128, 8, 320], BF16)
            gi = [0]

            def load_seg(i, c0, n):
                s = stg[i % 2]
                K.dma("sync", s[:, :, 0:n], w_in_v[:, :, c0:c0 + n], [], [s])
                return s

            def rot_into(dst_ap_fn, dstb, s, off, half):
                K.ts("vector", dst_ap_fn(0, half), s[:, :, off + half:off + 2 * half], -1.0, None, ALU.mult, None, [s], [dstb])
                K.cp("gpsimd", dst_ap_fn(half, 2 * half), s[:, :, off:off + half], [s], [dstb])

            def store_grp(gidx, g):
                K.dma("sync", wq_d[gidx].rearrange("p (c m) -> p c m", c=8), g[:], [g], [wq_tok])

            def new_grp():
                g = grp[gi[0] % 3]
                gi[0] += 1
                return g

            s = load_seg(0, QA, 512)
            for g_ in range(4):
                g = new_grp()
                for u in range(2):
                    h = 4 * u + g_
                    K.cp("scalar", g[:, :, u * 64:(u + 1) * 64], s[:, :, h * 64:(h + 1) * 64], [s], [g])
                store_grp(g_, g)
                g = new_grp()
                for u in range(2):
                    h = 4 * u + g_
                    rot_into(lambda a, b, u=u, g=g: g[:, :, u * 64 + a:u * 64 + b], g, s, h * 64, 32)
                store_grp(4 + g_, g)
            s = load_seg(1, 512, 424)
            o_ka, o_va, o_qi, o_ki, o_wi = 0, 64, 128, 384, 416
            BF_KA_A, BF_KA_B, BF_KI_A, BF_KI_B, BF_KS_A, BF_KS_B, BF_KW_A, BF_KW_B, BF_KC, BF_VC = \
                0, 128, 256, 352, 448, 576, 704, 832, 960, 1088
            for u in range(2):
                K.cp("scalar", wBfm[:, :, BF_KA_A + u * 64:BF_KA_A + (u + 1) * 64], s[:, :, o_ka:o_ka + 64], [s], [wBfm])
                rot_into(lambda a, b, u=u: wBfm[:, :, BF_KA_B + u * 64 + a:BF_KA_B + u * 64 + b], wBfm, s, o_ka, 32)
            for r_ in range(3):
                K.cp("scalar", wBfm[:, :, BF_KI_A + r_ * 32:BF_KI_A + (r_ + 1) * 32], s[:, :, o_ki:o_ki + 32], [s], [wBfm])
                rot_into(lambda a, b, r_=r_: wBfm[:, :, BF_KI_B + r_ * 32 + a:BF_KI_B + r_ * 32 + b], wBfm, s, o_ki, 16)
            K.cp("scalar", wBtm[:, :, 0:64], s[:, :, o_va:o_va + 64], [s], [wBtm])
            for q_ in range(3):
                hs = [3 * q_ + i for i in range(3) if 3 * q_ + i < 8]
                g = new_grp()
                K.memset("vector", g[:], 0.0, [g])
                for i, h in enumerate(hs):
                    K.cp("scalar", g[:, :, i * 32:(i + 1) * 32], s[:, :, o_qi + h * 32:o_qi + (h + 1) * 32], [s], [g])
                store_grp(8 + q_, g)
                g = new_grp()
                K.memset("vector", g[:], 0.0, [g])
                for i, h in enumerate(hs):
                    rot_into(lambda a, b, i=i, g=g: g[:, :, i * 32 + a:i * 32 + b], g, s, o_qi + h * 32, 16)
                store_grp(11 + q_, g)
            s = load_seg(0, QB, 512)
            for g_ in range(4):
                g = new_grp() if True else None
                for u in range(2):
                    h = 4 * u + g_
                    K.cp("scalar", g[:, :, u * 64:(u + 1) * 64], s[:, :, h * 64:(h + 1) * 64], [s], [g])
                store_grp(14 + g_, g)
                g = new_grp()
                for u in range(2):
                    h = 4 * u + g_
                    rot_into(lambda a, b, u=u, g=g: g[:, :, u * 64 + a:u * 64 + b], g, s, h * 64, 32)
                store_grp(18 + g_, g)
            s = load_seg(1, KC, 512)
            K.cp("scalar", wBfm[:, :, BF_KC:BF_KC + 128], s[:, :, 0:128], [s], [wBfm])
            K.cp("scalar", wBfm[:, :, BF_VC:BF_VC + 128], s[:, :, 128:256], [s], [wBfm])
            K.cp("scalar", wBfm[:, :, BF_KS_A:BF_KS_A + 128], s[:, :, 256:384], [s], [wBfm])
            for u in range(2):
                rot_into(lambda a, b, u=u: wBfm[:, :, BF_KS_B + u * 64 + a:BF_KS_B + u * 64 + b], wBfm, s, 256 + u * 64, 32)
            K.cp("scalar", wBtm[:, :, 64:192], s[:, :, 384:512], [s], [wBtm])
            s = load_seg(0, KW, 280)
            K.cp("scalar", wBfm[:, :, BF_KW_A:BF_KW_A + 128], s[:, :, 0:128], [s], [wBfm])
            for u in range(2):
                rot_into(lambda a, b, u=u: wBfm[:, :, BF_KW_B + u * 64 + a:BF_KW_B + u * 64 + b], wBfm, s, u * 64, 32)
            K.cp("scalar", wBtm[:, :, 192:320], s[:, :, 128:256], [s], [wBtm])
            gG = K.sb(pw, [128, 8, 128], BF16)
            K.memset("vector", gG[:], 0.0, [gG])
            K.cp("scalar", gG[:, :, 0:24], s[:, :, 256:280], [s], [gG])
            wis = K.sb(pw, [128, 8, 8])
            K.dma("sync", wis[:], w_in_v[:, :, WI:WI + 8], [], [wis])
            K.cp("scalar", gG[:, :, 24:32], wis[:], [wis], [gG])
            store_grp(38, gG)
            for half in range(4):
                s = load_seg(half + 1, GA + half * 512, 512)
                for q_ in range(4):
                    g = new_grp()
                    K.cp("scalar" if q_ % 2 == 0 else "vector", g[:], s[:, :, q_ * 128:(q_ + 1) * 128], [s], [g])
                    store_grp(22 + half * 4 + q_, g)

            xb = [K.sb(pw, [128, D]) for _ in range(2)]
            sq = K.sb(pw, [128, D], BF16)
            hn = K.sb(pw, [128, D], BF16)
            st = K.sb(pw, [128, 4])
            hTc = K.sb(pw, [128, 8, 512], BF16)
            posi = K.sb(pw, [128, 512], I32)
            posf = K.sb(pw, [128, 512])
            tq = [K.sb(pw, [128, 512]), K.sb(pw, [128, 512]), K.sb(pw, [128, 512], I32)]
            tabs = {n: K.sb(pw, [128, 512], BF16) for n in ("cos64", "sin64", "cos32", "sin32")}
            rt = (K.sb(pw, [128, 512]), K.sb(pw, [128, 512]))
            kcmpT = K.sb(pw, [128, L], BF16)
            vcmpT = K.sb(pw, [128, L], BF16)
            for v3 in (va3, vs3, vw3):
                K.memset("gpsimd", v3[:, :, 64:128], 1.0, [v3])
            for c in range(NCH):
                make_hT(c, hTc, xb, sq, hn, st, gmix)
                make_rope(c, posi, posf, tq, tabs)
                cs = slice(c * 512, (c + 1) * 512)
                W = lambda off, m: (wBfm[:, :, off:off + m], wBfm)
                proj_fm(hTc, W(BF_KA_A, 128), 128, kaT2[:, cs], kaT2, W(BF_KA_B, 128), tabs["cos64"], tabs["sin64"], rt)
                proj_fm(hTc, W(BF_KI_A, 96), 96, kiT3[:, cs], kiT3, W(BF_KI_B, 96), tabs["cos32"], tabs["sin32"], rt)
                proj_fm(hTc, W(BF_KS_A, 128), 128, ksT[:, cs], ksT, W(BF_KS_B, 128), tabs["cos64"], tabs["sin64"], rt)
                proj_fm(hTc, W(BF_KW_A, 128), 128, kwT[:, cs], kwT, W(BF_KW_B, 128), tabs["cos64"], tabs["sin64"], rt)
                proj_fm(hTc, W(BF_KC, 128), 128, kcmpT[:, cs], kcmpT)
                proj_fm(hTc, W(BF_VC, 128), 128, vcmpT[:, cs], vcmpT, evac="vector")
                for j in range(4):
                    tt_ = 4 * c + j
                    pv = bank()
                    for k in range(8):
                        K.mm(pv[:, 0:320], hTc[:, k, j * 128:(j + 1) * 128], wBtm[:, k, :], k == 0, k == 7, [hTc, wBtm], [pv])
                    K.cp("scalar", va3[:, tt_, 0:64], pv[:, 0:64], [pv], [va3])
                    K.cp("vector", va3[:, tt_, 128:192], pv[:, 0:64], [pv], [va3])
                    K.cp("scalar", vs3[:, tt_, 0:64], pv[:, 64:128], [pv], [vs3])
                    K.cp("vector", vs3[:, tt_, 128:192], pv[:, 128:192], [pv], [vs3])
                    K.cp("scalar", vw3[:, tt_, 0:64], pv[:, 192:256], [pv], [vw3])
                    K.cp("vector", vw3[:, tt_, 128:192], pv[:, 256:320], [pv], [vw3])
            K.dump("kaT2", kaT2[:], [128, L], BF16, [kaT2.k])
            K.dump("kiT3", kiT3[:], [96, L], BF16, [kiT3.k])
            K.dump("ksT", ksT[:], [128, L], BF16, [ksT.k])
            K.dump("kwT", kwT[:], [128, L], BF16, [kwT.k])
            K.dump("va3", va3[:].rearrange("p a b -> p (a b)"), [128, NT * 192], BF16, [va3.k])
            K.dump("vs3", vs3[:].rearrange("p a b -> p (a b)"), [128, NT * 192], BF16, [vs3.k])

            w1 = K.sb(pw, [128, 32, 128], BF16)
            w2d = K.sb(pw, [128, 128], BF16)
            peT = K.sb(pw, [64, 32], BF16)
            hid = K.sb(pw, [128, 256], BF16)
            cb = K.sb(pw, [128, 1])
            K.memset("vector", kcT2[:], 0.0, [kcT2])
            for kind, (pe_d, w1_d, w2_d, srcT) in enumerate(((pe_k_d, w1_k_d, w2_k_d, kcmpT), (pe_v_d, w1_v_d, w2_v_d, vcmpT))):
                w1v = w1_d.rearrange("(j d) c -> d j c", d=64)
                K.dma("gpsimd", w1[0:64, :, :], w1v, [], [w1])
                K.dma("gpsimd", w1[64:128, :, :], w1v, [], [w1])
                K.dma("gpsimd", w2d[:, 0:64], w2_d, [], [w2d])
                K.dma("gpsimd", w2d[:, 64:128], w2_d, [], [w2d])
                K.dma("gpsimd", peT[:], pe_d.rearrange("j d -> d j"), [], [peT], allow_slow_non_contiguous=True)
                pbias = bank()
                for j in range(32):
                    K.mm(pbias[:, 0:1], w1[0:64, j, :], peT[:, j:j + 1], j == 0, j == 31, [w1, peT], [pbias])
                K.cp("vector", cb[:], pbias[:, 0:1], [pbias], [cb])
                for kk in range(2):
                    ph = bank()
                    lo = 64 * kk
                    for j in range(32):
                        K.mm(ph[:, 0:255], w1[lo:lo + 64, j, :], srcT[lo:lo + 64, j:j + 16 * 254 + 1:16],
                             j == 0, j == 31, [w1, srcT], [ph])
                    K.memset("vector", hid[:, 255:256], 0.0, [hid])
                    K.act(hid[:, 0:255], ph[:, 0:255], ACT.Silu, [ph, cb], [hid], bias=cb[:, 0:1])
                    if kind == 0:
                        po = bank()
                        K.mm(po[:, 0:256], w2d[:], hid[:], True, True, [w2d, hid], [po])
                        K.cp("vector", kcT2[lo:lo + 64, :], po[lo:lo + 64, 0:256], [po], [kcT2])
                    else:
                        for ch in range(2):
                            po = bank()
                            K.mm(po[:, 0:64], hid[:, ch * 128:(ch + 1) * 128], w2d[:, 0:64], True, True, [hid, w2d], [po])
                            K.cp("vector", vctm[:, ch, lo:lo + 64], po[:, 0:64], [po], [vctm])
            K.dump("kcT2", kcT2[:], [128, 256], BF16, [kcT2.k])
            K.dump("vctm", vctm[:].rearrange("p a b -> p (a b)"), [128, 256], BF16, [vctm.k])
            P.emit()
        if stop_after == "B":
            P.emit(final=True)
            att.close()
            return nc, K
        with ExitStack() as pc:
            qt_list = list(range(NT)) if qtiles is None else list(qtiles)
            ch_list = sorted(set(q // 4 for q in qt_list))
            RA = K.sb(pc, [128, L])
            idx = RA
            wout = View(RA.t[:].bitcast(BF16).rearrange("p (c f) -> p c f", c=8), RA.k)
            RBm = K.sb(pc, [128, L], BF16)
            mask = RBm
            wbra = View(RBm.t[:].rearrange("p (g f) -> p g f", g=4), RBm.k)
            RC = K.sb(pc, [128, NT, 128], BF16)
            maskT = RC
            wbrb = View(RC.t[:].rearrange("p a b -> p (a b)").rearrange("p (g f) -> p g f", g=4), RC.k)
            RD = K.sb(pc, [128, 8, 256])
            Ecmp = RD
            mergedT = View(RD.t[:].rearrange("p a b -> p (a b)").bitcast(BF16).rearrange("p (c t) -> p c t", c=8), RD.k)
            RE = K.sb(pc, [128, 2560])
            kEa, kEb, kEc = Tok(), Tok(), Tok()
            posi = View(RE.t[:, 0:512].bitcast(I32), kEa)
            posf = View(RE.t[:, 512:1024], kEa)
            ang_ = View(RE.t[:, 1024:1536], kEb)
            kf_ = View(RE.t[:, 1536:2048], kEb)
            ki_ = View(RE.t[:, 2048:2560].bitcast(I32), kEc)
            p_bf = View(RE.t[:, 0:1024].bitcast(BF16).rearrange("p (h n) -> p h n", h=8), kEa)
            pT = View(RE.t[:, 1024:2048].bitcast(BF16).rearrange("p (c h t) -> p c h t", c=2, h=8), kEb)
            R0 = View(RE.t[:, 2048:2560], kEc)
            R1 = K.sb(pc, [128, 512])
            ob = K.sb(pc, [128, 512])
            rt2 = (R1, ob)
            xb = [K.sb(pc, [128, D])]
            hn = K.sb(pc, [128, D], BF16)
            st = K.sb(pc, [128, 4])
            hTc = K.sb(pc, [128, 8, 512], BF16)
            tabs = {n: K.sb(pc, [128, 512], BF16) for n in ("cos64", "sin64", "cos32", "sin32")}
            ws = [K.sb(pc, [128, 8, 128], BF16) for _ in range(2)]
            wG = K.sb(pc, [128, 8, 128], BF16)
            qaT = K.sb(pc, [128, 4, 512], BF16)
            qiT = K.sb(pc, [96, 3, 512], BF16)
            qnT = K.sb(pc, [128, 4, 512], BF16)
            qrT = K.sb(pc, [128, 4, 512], BF16)
            gT = K.sb(pc, [32, 512], BF16)
            oaTc = K.sb(pc, [128, 4, 512], BF16)
            obTc = K.sb(pc, [128, 4, 512], BF16)
            Eb = [K.sb(pc, [128, 512], BF16) for _ in range(4)]
            Pb = [K.sb(pc, [128, 512], BF16) for _ in range(4)]
            Esel = K.sb(pc, [64, 32, 128], BF16)
            eye24 = K.sb(pc, [24, 24], BF16)
            ones24 = K.sb(pc, [24, 128], BF16)
            Dg = K.sb(pc, [24, 8, 128], BF16)
            gBs = [K.sb(pc, [128, 512], BF16) for _ in range(2)]
            rs = K.sb(pc, [128, 512])
            sm = K.sb(pc, [128, 64])
            wi_sb2 = [K.sb(pc, [128, 8]) for _ in range(2)]
            smb2 = [K.sb(pc, [128, 32]) for _ in range(2)]
            P4 = K.sb(pc, [128, 2, 256])
            imp = K.sb(pc, [128, 2, 64])
            scs = K.sb(pc, [128, 2, 64])
            sc2 = K.sb(pc, [128, 64])
            selb = K.sb(pc, [128, 64])
            bm = K.sb(pc, [128, 2, 64], BF16)
            bmT = K.sb(pc, [64, 2, 128], BF16)
            mexp = [K.sb(pc, [128, 2, 128], BF16) for _ in range(4)]
            er = [0]

            def Enext():
                er[0] += 1
                return Eb[er[0] % 4], Pb[er[0] % 4]

            def pipe(units, qk_fn, pv_fn, depth=2, hook=None):
                pend = []
                for un in units:
                    pend.append((un, qk_fn(un)))
                    if hook is not None:
                        hook()
                    if len(pend) > depth:
                        pv_fn(*pend.pop(0))
                for p_ in pend:
                    pv_fn(*p_)

            with ExitStack() as cc:
                K.dma("gpsimd", Esel[:].rearrange("p a b -> p (a b)"), c_esel_d, [], [Esel])
                K.dma("gpsimd", eye24[:], c_eye_d, [], [eye24])
                K.memset("vector", ones24[:], 1.0, [ones24])
                K.memset("vector", sm[:, 32:33], 0.5, [sm])
                P.emit()

            def load_ws(gidx, i):
                w = ws[i % 2]
                K.dma("sync", w[:], wq_d[gidx].rearrange("p (c m) -> p c m", c=8), [wq_tok], [w])
                return w

            A_banks = (banks[4], banks[5])
            B_banks = (banks[6], banks[7])
            SC = 0.125
            wsi = [0]

            for c in ch_list:
                make_hT(c, hTc, xb, hn, hn, st, gmix)
                make_rope(c, posi, posf, (ang_, kf_, ki_), tabs)
                if lvl < 0.2:
                    continue
                for g_ in range(4):
                    wA = load_ws(g_, wsi[0]); wsi[0] += 1
                    wB = load_ws(4 + g_, wsi[0]); wsi[0] += 1
                    proj_fm(hTc, (wA[:], wA), 128, qaT[:, g_, :], qaT, (wB[:], wB), tabs["cos64"], tabs["sin64"], rt2)
                for q_ in (range(3) if lvl >= 0.5 else []):
                    wA = load_ws(8 + q_, wsi[0]); wsi[0] += 1
                    wB = load_ws(11 + q_, wsi[0]); wsi[0] += 1
                    proj_fm(hTc, (wA[:, :, 0:96], wA), 96, qiT[:, q_, :], qiT, (wB[:, :, 0:96], wB), tabs["cos32"], tabs["sin32"], rt2)
                for g_ in (range(4) if lvl >= 0.75 else []):
                    wA = load_ws(14 + g_, wsi[0]); wsi[0] += 1
                    wB = load_ws(18 + g_, wsi[0]); wsi[0] += 1
                    pa = bank(); pb = bank()
                    for k in range(8):
                        K.mm(pa[:, 0:512], wA[:, k, :], hTc[:, k, :], k == 0, k == 7, [wA, hTc], [pa])
                    for k in range(8):
                        K.mm(pb[:, 0:512], wB[:, k, :], hTc[:, k, :], k == 0, k == 7, [wB, hTc], [pb])
                    K.cp("scalar", qnT[:, g_, :], pa[:, 0:512], [pa], [qnT])
                    t1, t2 = rt2
                    K.tt("vector", t1[:, :], pa[:, 0:512], tabs["cos64"][:, :], ALU.mult, [pa, tabs["cos64"]], [t1])
                    K.tt("vector", t2[:, :], pb[:, 0:512], tabs["sin64"][:, :], ALU.mult, [pb, tabs["sin64"]], [t2])
                    K.tt("gpsimd", qrT[:, g_, :], t1[:, :], t2[:, :], ALU.add, [t1, t2], [qrT])
                if lvl >= 0.9:
                    K.dma("sync", wG[:], wq_d[38].rearrange("p (c m) -> p c m", c=8), [wq_tok], [wG])
                    proj_fm(hTc, (wG[:, :, 0:32], wG), 32, gT[:, :], gT, func=ACT.Sigmoid)
                K.dump(f"qaT{c}", qaT[:].rearrange("p a b -> p (a b)"), [128, 2048], BF16, [qaT.k])
                K.dump(f"qiT{c}", qiT[:].rearrange("p a b -> p (a b)"), [96, 1536], BF16, [qiT.k])
                K.dump(f"qrT{c}", qrT[:].rearrange("p a b -> p (a b)"), [128, 2048], BF16, [qrT.k])
                K.dump(f"gT{c}", gT[:], [32, 512], BF16, [gT.k])

                NIT = 14
                tiles_c = ([q for q in qt_list if q // 4 == c] if lvl >= 2 else [])

                def pre_a(qt):
                    t0 = qt * 128
                    tl = (qt % 4) * 128
                    tsl = slice(tl, tl + 128)
                    n = t0 + 128
                    wi_ = wi_sb2[qt % 2]
                    smb = smb2[qt % 2]
                    pw_ = bank()
                    for k in range(8):
                        K.mm(pw_[:, 0:8], hTc[:, k, tsl], wG[:, k, 24:32], k == 0, k == 7, [hTc, wG], [pw_])
                    K.cp("vector", wi_[:], pw_[:, 0:8], [pw_], [wi_])
                    nsc = (n + 511) // 512
                    ri = 0
                    for sc_i in range(nsc):
                        c0 = sc_i * 512
                        ncol = min(512, n - c0)
                        for h in range(8):
                            q_, r_ = h // 3, h % 3
                            pi_ = bank()
                            K.mm(pi_[:, 0:ncol], qiT[32 * r_:32 * r_ + 32, q_, tsl], kiT3[32 * r_:32 * r_ + 32, c0:c0 + ncol],
                                 True, True, [qiT, kiT3], [pi_])
                            Rb = (R0, R1)[ri % 2]; ri += 1
                            K.act(Rb[:, 0:ncol], pi_[:, 0:ncol], ACT.Relu, [pi_], [Rb])
                            if h == 0:
                                K.ts("vector", idx[:, c0:c0 + ncol], Rb[:, 0:ncol], wi_[:, 0:1], None, ALU.mult, None, [Rb, wi_], [idx])
                            else:
                                K.stt("vector", idx[:, c0:c0 + ncol], Rb[:, 0:ncol], wi_[:, h:h + 1], idx[:, c0:c0 + ncol],
                                      ALU.mult, ALU.add, [Rb, wi_, idx], [idx])
                    P.op("vector", lambda e, n=n: e.tensor_reduce(out=smb[:, 0:1], in_=idx[:, 0:n], axis=AX.X, op=ALU.max), K._tk([idx]), K._tk([smb]))
                    P.op("vector", lambda e, n=n: e.tensor_reduce(out=smb[:, 1:2], in_=idx[:, 0:n], axis=AX.X, op=ALU.min), K._tk([idx]), K._tk([smb]))
                    K.asel(idx[:, t0:t0 + 128], idx[:, t0:t0 + 128], [[-1, 128]], ALU.is_ge, -1e30, 0, 1, [idx], [idx])
                    K.ts("vector", smb[:, 2:3], smb[:, 1:2], -1.0, None, ALU.add, None, [smb], [smb])
                    K.stt("vector", smb[:, 3:4], smb[:, 0:1], 1.0, smb[:, 2:3], ALU.add, ALU.subtract, [smb], [smb])
                    K.memset("vector", smb[:, 8:8 + NIT], 0.0, [smb])

                def bis_step(qt, it):
                    n = qt * 128 + 128
                    smb = smb2[qt % 2]
                    f = 2.0 ** -(it + 1)
                    K.stt("vector", smb[:, 4:5], smb[:, 3:4], f, smb[:, 2:3], ALU.mult, ALU.add, [smb], [smb])
                    K.ts("vector", mask[:, 0:n], idx[:, 0:n], smb[:, 4:5], 0.0, ALU.is_ge, ALU.add, [idx, smb, mask], [mask, smb],
                         accum_out=smb[:, 8 + it:9 + it])
                    K.ts("vector", smb[:, 5:6], smb[:, 8 + it:9 + it], 256.0, f, ALU.is_ge, ALU.mult, [smb], [smb])
                    K.stt("vector", smb[:, 2:3], smb[:, 3:4], smb[:, 5:6], smb[:, 2:3], ALU.mult, ALU.add, [smb], [smb])

                def pre_c(qt):
                    n = qt * 128 + 128
                    smb = smb2[qt % 2]
                    K.ts("vector", mask[:, 0:n], idx[:, 0:n], smb[:, 2:3], None, ALU.is_ge, None, [idx, smb], [mask])
                    K.dump(f"mask{qt}", mask[:, 0:n], [128, n], BF16, [mask.k])
                    if lvl < 3:
                        return
                    for b0 in range(0, qt + 1, 8):
                        nb = min(8, qt + 1 - b0)
                        pm_ = bank()
                        for i in range(nb):
                            si = b0 + i
                            K.tr(bfv(pm_)[:, i * 128:(i + 1) * 128], mask[:, si * 128:(si + 1) * 128], ident[:], [mask, ident], [pm_])
                        K.cp("scalar", maskT[:, b0:b0 + nb, :], bfv(pm_)[:, 0:nb * 128].rearrange("p (a b) -> p a b", b=128), [pm_], [maskT])

                if tiles_c:
                    pre_a(tiles_c[0])
                    for it in range(NIT):
                        bis_step(tiles_c[0], it)
                    pre_c(tiles_c[0])
                for qj, qt in enumerate(tiles_c):
                    t0 = qt * 128
                    tl = (qt % 4) * 128
                    tsl = slice(tl, tl + 128)
                    n = t0 + 128
                    nxt = tiles_c[qj + 1] if qj + 1 < len(tiles_c) else None
                    if lvl < 3:
                        if nxt is not None:
                            pre_a(nxt)
                            for it in range(NIT):
                                bis_step(nxt, it)
                            pre_c(nxt)
                        continue
                    steps_left = list(range(NIT)) if nxt is not None else []
                    if nxt is not None:
                        pre_a(nxt)

                    def bis_hook():
                        if steps_left:
                            bis_step(nxt, steps_left.pop(0))

                    def dsa_qk(un):
                        si, u = un
                        ssl = slice(si * 128, (si + 1) * 128)
                        lo = 64 * u
                        ps_ = bank()
                        K.mm(ps_[:, 0:512].rearrange("p (g t) -> p g t", g=4), kaT2[lo:lo + 64, ssl], qaT[lo:lo + 64, :, tsl],
                             True, True, [kaT2, qaT], [ps_])
                        E_, Pm_ = Enext()
                        K.act(E_[:, :], ps_[:, 0:512], ACT.Exp, [ps_], [E_], scale=SC)
                        K.tt("vector", Pm_[:, :].rearrange("p (g t) -> p g t", g=4), E_[:, :].rearrange("p (g t) -> p g t", g=4),
                             bc(maskT[:, si, :].unsqueeze(1), [128, 4, 128]), ALU.mult, [E_, maskT], [Pm_])
                        return Pm_

                    def dsa_pv(un, Pm_):
                        si, u = un
                        lo = 64 * u
                        K.mm(A_banks[u][:, 0:512], va3[:, si, lo:lo + 128], Pm_[:, :], si == 0, si == qt, [va3, Pm_], [A_banks[u]])

                    pipe([(si, u) for si in range(qt + 1) for u in range(2)], dsa_qk, dsa_pv, hook=bis_hook)
                    for u in range(2):
                        lo = 64 * u; lr = 64 * (1 - u)
                        K.recip(rs[lo:lo + 64, :], A_banks[u][lr:lr + 64, 0:512], [A_banks[u]], [rs])
                        K.tt("vector", oaTc[lo:lo + 64, :, tsl], A_banks[u][lo:lo + 64, 0:512].rearrange("p (g t) -> p g t", g=4),
                             rs[lo:lo + 64, :].rearrange("p (g t) -> p g t", g=4), ALU.mult, [A_banks[u], rs], [oaTc])
                    while steps_left:
                        bis_step(nxt, steps_left.pop(0))
                    if nxt is not None:
                        pre_c(nxt)
                    if lvl < 4:
                        continue
                    for k in range(2):
                        lo = 64 * k
                        for gp in range(2):
                            ps_ = bank()
                            for jj in range(2):
                                g_ = 2 * gp + jj
                                K.mm(ps_[:, jj * 256:(jj + 1) * 256], qnT[lo:lo + 64, g_, tsl], kcT2[lo:lo + 64, 0:256], True, True, [qnT, kcT2], [ps_])
                            h0 = 4 * k + 2 * gp
                            K.act(Ecmp[:, h0:h0 + 2, :], ps_[:, 0:512].rearrange("p (a n) -> p a n", a=2), ACT.Exp, [ps_], [Ecmp], scale=SC)
                    K.asel(Ecmp[:], Ecmp[:], [[0, 8], [-16, 256]], ALU.is_ge, 0.0, t0 - 31, 1, [Ecmp], [Ecmp])
                    P.op("vector", lambda e: e.tensor_reduce(out=sm[:, 40:48], in_=Ecmp[:], axis=AX.X, op=ALU.add), K._tk([Ecmp]), K._tk([sm]))
                    K.ts("vector", sm[:, 40:48], sm[:, 40:48], 1e-30, None, ALU.add, None, [sm], [sm])
                    K.recip(sm[:, 48:56], sm[:, 40:48], [sm], [sm])
                    K.tt("vector", Ecmp[:], Ecmp[:], bc(sm[:, 48:56].unsqueeze(2), [128, 8, 256]), ALU.mult, [Ecmp, sm], [Ecmp])
                    K.cp("gpsimd", p_bf[:], Ecmp[:], [Ecmp], [p_bf])
                    P.op("vector", lambda e: e.tensor_reduce(out=P4[:], in_=Ecmp[:].rearrange("p (k g) n -> p k n g", k=2), axis=AX.X, op=ALU.add),
                         K._tk([Ecmp]), K._tk([P4]))
                    P.op("vector", lambda e: e.tensor_reduce(out=imp[:], in_=P4[:].rearrange("p k (j i) -> p k j i", i=4), axis=AX.X, op=ALU.add),
                         K._tk([P4]), K._tk([imp]))
                    K.tt("vector", imp[:, :, 1:64], imp[:, :, 1:64], P4[:, :, 3:252:4], ALU.add, [imp, P4], [imp])
                    K.dma("sync", selb[0:64, :], c_selb_d[2 * qt:2 * qt + 1, :].partition_broadcast(64), [], [selb])
                    K.dma("sync", selb[64:128, :], c_selb_d[2 * qt + 1:2 * qt + 2, :].partition_broadcast(64), [], [selb])
                    K.tt("vector", scs[:], imp[:], bc(selb[:, :].unsqueeze(1), [128, 2, 64]), ALU.add, [imp, selb], [scs])
                    for k in range(2):
                        P.op("vector", lambda e, k=k: e.max(out=sm[:, 16:24], in_=scs[:, k, :]), K._tk([scs]), K._tk([sm]))
                        P.op("vector", lambda e, k=k: e.match_replace(out=sc2[:], in_to_replace=sm[:, 16:24], in_values=scs[:, k, :], imm_value=-1e9),
                             K._tk([scs, sm]), K._tk([sc2]))
                        P.op("vector", lambda e: e.max(out=sm[:, 24:32], in_=sc2[:]), K._tk([sc2]), K._tk([sm]))
                        K.ts("vector", bm[:, k, :], scs[:, k, :], sm[:, 31:32], None, ALU.is_ge, None, [scs, sm], [bm])
                    K.dump(f"bm{qt}", bm[:].rearrange("p a b -> p (a b)"), [128, 128], BF16, [bm.k])
                    pb_ = bank()
                    for k in range(2):
                        K.tr(bfv(pb_)[0:64, k * 128:(k + 1) * 128], bm[:, k, :], ident[:], [bm, ident], [pb_])
                    K.cp("scalar", bmT[:], bfv(pb_)[0:64, 0:256].rearrange("p (k t) -> p k t", k=2), [pb_], [bmT])
                    for ch in range(2):
                        pp_ = bank()
                        for h in range(8):
                            K.tr(bfv(pp_)[:, h * 128:(h + 1) * 128], p_bf[:, h, ch * 128:(ch + 1) * 128], ident[:], [p_bf, ident], [pp_])
                        K.cp("scalar" if ch == 0 else "vector", pT[:, ch, :, :], bfv(pp_).rearrange("p (h t) -> p h t", h=8), [pp_], [pT])
                    for k in range(2):
                        for ch in range(2):
                            K.mm(B_banks[k][:, 0:512].rearrange("p (g t) -> p g t", g=4), vctm[:, ch, :], pT[:, ch, 4 * k:4 * k + 4, :],
                                 ch == 0, ch == 1, [vctm, pT], [B_banks[k]])
                    def gate_bcast(cidx, k):
                        pg_ = bank()
                        K.mm(pg_[:, 0:512].rearrange("p (g t) -> p g t", g=4), ones24[:, :], Dg[:, 4 * k:4 * k + 4, :], True, True, [ones24, Dg], [pg_])
                        gb_ = gBs[k]
                        K.cp("scalar", gb_[64 * k:64 * k + 64, :], pg_[64 * k:64 * k + 64, 0:512], [pg_], [gb_])
                        return gb_

                    def make_Dg(cidx):
                        K.tt("vector", Dg[:], bc(gT[0:24, tsl].unsqueeze(1), [24, 8, 128]),
                             bc(eye24[:, cidx * 8:cidx * 8 + 8].unsqueeze(2), [24, 8, 128]), ALU.mult, [gT, eye24], [Dg])

                    make_Dg(0)
                    for k in range(2):
                        lo = 64 * k
                        gb_ = gate_bcast(0, k)
                        K.tt("vector", ob[lo:lo + 64, :], B_banks[k][lo:lo + 64, 0:512], gb_[lo:lo + 64, :], ALU.mult, [B_banks[k], gb_], [ob])
                    if lvl < 5:
                        continue
                    mes = {}

                    def slc_qk(un):
                        si, k = un
                        ssl = slice(si * 128, (si + 1) * 128)
                        if k == 0:
                            pm_ = bank()
                            K.mm(pm_[:, 0:256].rearrange("p (k t) -> p k t", k=2), Esel[:, si, :], bmT[:, :, :], True, True, [Esel, bmT], [pm_])
                            me = mexp[si % 4]
                            K.cp("scalar", me[:], pm_[:, 0:256].rearrange("p (k t) -> p k t", k=2), [pm_], [me])
                            if si == qt:
                                K.tt("gpsimd", me[:], me[:], bc(diagT[:, :].unsqueeze(1), [128, 2, 128]), ALU.mult, [me, diagT], [me])
                            mes[si] = me
                        me = mes[si]
                        lo = 64 * k
                        ps_ = bank()
                        K.mm(ps_[:, 0:512].rearrange("p (g t) -> p g t", g=4), ksT[lo:lo + 64, ssl], qrT[lo:lo + 64, :, tsl], True, True, [ksT, qrT], [ps_])
                        E_, Pm_ = Enext()
                        K.act(E_[:, :], ps_[:, 0:512], ACT.Exp, [ps_], [E_], scale=SC)
                        K.tt("vector", Pm_[:, :].rearrange("p (g t) -> p g t", g=4), E_[:, :].rearrange("p (g t) -> p g t", g=4),
                             bc(me[:, k, :].unsqueeze(1), [128, 4, 128]), ALU.mult, [E_, me], [Pm_])
                        return Pm_

                    def slc_pv(un, Pm_):
                        si, k = un
                        lo = 64 * k
                        K.mm(A_banks[k][:, 0:512], vs3[:, si, lo:lo + 128], Pm_[:, :], si == 0, si == qt, [vs3, Pm_], [A_banks[k]])

                    pipe([(si, k) for si in range(qt + 1) for k in range(2)], slc_qk, slc_pv)

                    def fin(acc, cidx, last):
                        make_Dg(cidx)
                        for k in range(2):
                            lo = 64 * k; lr = 64 * (1 - k)
                            gb_ = gate_bcast(cidx, k)
                            K.recip(rs[lo:lo + 64, :], acc[k][lr:lr + 64, 0:512], [acc[k]], [rs])
                            K.tt("gpsimd", rs[lo:lo + 64, :], rs[lo:lo + 64, :], gb_[lo:lo + 64, :], ALU.mult, [rs, gb_], [rs])
                            tmp = R1
                            K.tt("vector", tmp[lo:lo + 64, :], acc[k][lo:lo + 64, 0:512], rs[lo:lo + 64, :], ALU.mult, [acc[k], rs], [tmp])
                            if not last:
                                K.tt("gpsimd", ob[lo:lo + 64, :], ob[lo:lo + 64, :], tmp[lo:lo + 64, :], ALU.add, [ob, tmp], [ob])
                            else:
                                K.tt("gpsimd", obTc[lo:lo + 64, :, tsl], ob[lo:lo + 64, :].rearrange("p (g t) -> p g t", g=4),
                                     tmp[lo:lo + 64, :].rearrange("p (g t) -> p g t", g=4), ALU.add, [ob, tmp], [obTc])

                    fin(A_banks, 1, False)
                    if lvl < 6:
                        continue
                    s_lo = max(0, qt - 4)

                    def win_qk(un):
                        si, k = un
                        ssl = slice(si * 128, (si + 1) * 128)
                        lo = 64 * k
                        ps_ = bank()
                        K.mm(ps_[:, 0:512].rearrange("p (g t) -> p g t", g=4), kwT[lo:lo + 64, ssl], qrT[lo:lo + 64, :, tsl], True, True, [kwT, qrT], [ps_])
                        E_, Pm_ = Enext()
                        K.act(E_[:, :], ps_[:, 0:512], ACT.Exp, [ps_], [E_], scale=SC)
                        mk = diagT if si == qt else (antiT if si == qt - 4 else None)
                        src = E_
                        if mk is not None:
                            K.tt("vector", Pm_[:, :].rearrange("p (g t) -> p g t", g=4), E_[:, :].rearrange("p (g t) -> p g t", g=4),
                                 bc(mk[:, :].unsqueeze(1), [128, 4, 128]), ALU.mult, [E_, mk], [Pm_])
                            src = Pm_
                        return src

                    def win_pv(un, src):
                        si, k = un
                        lo = 64 * k
                        K.mm(B_banks[k][:, 0:512], vw3[:, si, lo:lo + 128], src[:, :], si == s_lo, si == qt, [vw3, src], [B_banks[k]])

                    pipe([(si, k) for si in range(s_lo, qt + 1) for k in range(2)], win_qk, win_pv)
                    fin(B_banks, 2, True)

                for qt in [q for q in qt_list if q // 4 == c]:
                    tl = (qt % 4) * 128
                    for nm_, bt_ in (("oaT", oaTc), ("obT", obTc)):
                        if f"{nm_}{qt}" in K.dbg:
                            d_ = nc.dram_tensor(f"dbg_{nm_}{qt}", [128, 4, 128], BF16, kind="ExternalOutput").ap()
                            K.dma("sync", d_, bt_[:, :, tl:tl + 128], [bt_], [])
                if lvl < 7:
                    continue
                for u in range(2):
                    K.dma("gpsimd", wbra[64 * u:64 * u + 64, :, :], w_br_a_d[256 * u:256 * (u + 1), :].rearrange("(g d) f -> d g f", d=64), [], [wbra])
                    K.dma("gpsimd", wbrb[64 * u:64 * u + 64, :, :], w_br_b_d[256 * u:256 * (u + 1), :].rearrange("(g d) f -> d g f", d=64), [], [wbrb])
                K.dma("gpsimd", wout[:], w_out_d.rearrange("(c p) f -> p c f", p=128), [], [wout])
                for fc in range(8):
                    fsl = slice(fc * 128, (fc + 1) * 128)
                    outs_ = []
                    for br, (gbase, wbr, oT) in enumerate(((22, wbra, oaTc), (30, wbrb, obTc))):
                        wg_ = load_ws(gbase + fc, wsi[0]); wsi[0] += 1
                        pg_ = bank()
                        for k in range(8):
                            K.mm(pg_[:, 0:512], wg_[:, k, :], hTc[:, k, :], k == 0, k == 7, [wg_, hTc], [pg_])
                        E_, Pm_ = Enext()
                        K.act(E_[:, :], pg_[:, 0:512], ACT.Sigmoid, [pg_], [E_])
                        pbr = bank()
                        for g_ in range(4):
                            K.mm(pbr[:, 0:512], wbr[:, g_, fsl], oT[:, g_, :], g_ == 0, g_ == 3, [wbr, oT], [pbr])
                        K.tt("vector", Pm_[:, :], pbr[:, 0:512], E_[:, :], ALU.mult, [pbr, E_], [Pm_])
                        outs_.append(Pm_)
                    K.tt("gpsimd", mergedT[:, fc, :], outs_[0][:, :], outs_[1][:, :], ALU.add, [outs_[0], outs_[1]], [mergedT])
                K.dump(f"mergedT{c}", mergedT[:].rearrange("p a b -> p (a b)"), [128, 4096], BF16, [mergedT.k])
                for j in range(4):
                    tt_ = 4 * c + j
                    xt = xb[0]
                    K.dma("sync", xt[:], x_d[tt_ * 128:(tt_ + 1) * 128, :], [], [xt])
                    for half in range(2):
                        po_ = bank()
                        for fc in range(8):
                            K.mm(po_[:, 0:512], mergedT[:, fc, j * 128:(j + 1) * 128], wout[:, fc, half * 512:(half + 1) * 512],
                                 fc == 0, fc == 7, [mergedT, wout], [po_])
                        K.tt("vector", xt[:, half * 512:(half + 1) * 512], po_[:, 0:512], xt[:, half * 512:(half + 1) * 512], ALU.add, [po_, xt], [xt])
                    K.dma("sync", x1_d[tt_ * 128:(tt_ + 1) * 128, :], xt[:], [xt], [x1_tok])
                    K.dump(f"x1_{tt_}", xt[:], [128, D], F32, [xt.k])
            P.emit()
        if stop_after == "C":
            P.emit(final=True)
            att.close()
            return nc, K
        att.close()
        if moe_from_x:
            x1_d = x_d
        with ExitStack() as pm:
            HT = 16
            identf = K.sb(pm, [128, 128])
            gffn = K.sb(pm, [128, 8])
            gfin = K.sb(pm, [128, D])
            wr = K.sb(pm, [128, 8, 36])
            rb = K.sb(pm, [128, 36])
            h2T = K.sb(pm, [128, 8, HT * 128], BF16)
            yacc = K.sb(pm, [128, HT, D])
            wgt = K.sb(pm, [128, HT, 32])
            wgu = [K.sb(pm, [128, 8, 512], BF16) for _ in range(2)]
            wdn = [K.sb(pm, [128, 2, D], BF16) for _ in range(2)]
            aT = [K.sb(pm, [128, 2, 512], BF16) for _ in range(2)]
            sg = [K.sb(pm, [128, 512], BF16) for _ in range(2)]
            xm = [K.sb(pm, [128, D]) for _ in range(2)]
            xn = K.sb(pm, [128, D])
            h2f = K.sb(pm, [128, 8, 128])
            sq2 = K.sb(pm, [128, D], BF16)
            s2 = K.sb(pm, [128, 16])
            lg = K.sb(pm, [128, 36])
            me = K.sb(pm, [128, 32])
            ex = K.sb(pm, [128, 32])
            m8 = K.sb(pm, [128, 8])
            K.memset("gpsimd", identf[:], 1.0, [identf])
            K.asel(identf[:], identf[:], [[-1, 128]], ALU.is_equal, 0.0, 0, 1, [identf], [identf])
            K.dma("sync", gffn[:], norm_ffn_d, [], [gffn])
            K.dma("sync", gfin[:], norm_final_d.partition_broadcast(128), [], [gfin])
            K.dma("sync", wr[:, :, 0:4], w_group_d.rearrange("(c p) n -> p c n", p=128), [], [wr])
            K.dma("sync", wr[:, :, 4:36], w_expert_d.rearrange("(c p) n -> p c n", p=128), [], [wr])
            K.dma("sync", rb[:, 0:4], b_group_d.partition_broadcast(128), [], [rb])
            K.dma("sync", rb[:, 4:36], b_expert_d.partition_broadcast(128), [], [rb])
            BIG = 30000.0
            xi = [0]
            wl = [0]
            for half in range(2):
                for j in range(HT):
                    tt_ = half * HT + j
                    xt = xm[xi[0] % 2]; xi[0] += 1
                    K.dma("sync", xt[:], x1_d[tt_ * 128:(tt_ + 1) * 128, :], [x1_tok], [xt])
                    K.memset("vector", s2[:, 0:1], 0.0, [s2])
                    K.act(sq2[:], xt[:], ACT.Square, [xt, s2], [sq2, s2], accum_out=s2[:, 0:1])
                    K.act(s2[:, 1:2], s2[:, 0:1], ACT.Sqrt, [s2, cpi], [s2], scale=1.0 / D, bias=cpi[:, 1:2])
                    K.recip(s2[:, 2:3], s2[:, 1:2], [s2], [s2])
                    K.ts("vector", xn[:], xt[:], s2[:, 2:3], None, ALU.mult, None, [xt, s2], [xn])
                    for hb in range(2):
                        pb = bank()
                        for k in range(4):
                            kk = hb * 4 + k
                            K.tr(pb[:, k * 128:(k + 1) * 128], xn[:, kk * 128:(kk + 1) * 128], identf[:], [xn, identf], [pb])
                        K.tt("vector", h2f[:, hb * 4:hb * 4 + 4, :], pb[:, 0:512].rearrange("p (k t) -> p k t", k=4),
                             bc(gffn[:, hb * 4:hb * 4 + 4].unsqueeze(2), [128, 4, 128]), ALU.mult, [pb, gffn], [h2f])
                    K.cp("scalar", h2T[:, :, j * 128:(j + 1) * 128], h2f[:], [h2f], [h2T])
                    pr = bank()
                    for k in range(8):
                        K.mm(pr[:, 0:36], h2f[:, k, :], wr[:, k, :], k == 0, k == 7, [h2f, wr], [pr])
                    K.tt("vector", lg[:], pr[:, 0:36], rb[:], ALU.add, [pr, rb], [lg])
                    P.op("vector", lambda e: e.tensor_reduce(out=s2[:, 4:5], in_=lg[:, 0:4], axis=AX.X, op=ALU.max), K._tk([lg]), K._tk([s2]))
                    K.ts("vector", s2[:, 5:6], s2[:, 4:5], -1.0, None, ALU.mult, None, [s2], [s2])
                    K.memset("vector", s2[:, 6:7], 0.0, [s2])
                    K.act(ex[:, 0:4], lg[:, 0:4], ACT.Exp, [lg, s2], [ex, s2], bias=s2[:, 5:6], accum_out=s2[:, 6:7])
                    K.recip(s2[:, 7:8], s2[:, 6:7], [s2], [s2])
                    K.ts("vector", ex[:, 4:8], lg[:, 0:4], s2[:, 4:5], BIG, ALU.is_ge, ALU.mult, [lg, s2], [ex])
                    K.ts("vector", ex[:, 4:8], ex[:, 4:8], -BIG, None, ALU.add, None, [ex], [ex])
                    K.tt("vector", me[:].rearrange("p (g i) -> p g i", g=4), lg[:, 4:36].rearrange("p (g i) -> p g i", g=4),
                         bc(ex[:, 4:8].unsqueeze(2), [128, 4, 8]), ALU.add, [lg, ex], [me])
                    P.op("vector", lambda e: e.max(out=m8[:], in_=me[:]), K._tk([me]), K._tk([m8]))
                    K.ts("vector", s2[:, 8:9], m8[:, 0:1], -1.0, None, ALU.mult, None, [m8], [s2])
                    K.act(ex[:], me[:], ACT.Exp, [me, s2], [ex], bias=s2[:, 8:9])
                    K.act(s2[:, 9:10], m8[:, 1:2], ACT.Exp, [m8, s2], [s2], bias=s2[:, 8:9])
                    K.ts("vector", s2[:, 9:10], s2[:, 9:10], 1.0, None, ALU.add, None, [s2], [s2])
                    K.recip(s2[:, 10:11], s2[:, 9:10], [s2], [s2])
                    K.tt("vector", s2[:, 11:12], s2[:, 10:11], s2[:, 7:8], ALU.mult, [s2], [s2])
                    K.ts("vector", me[:], me[:], m8[:, 1:2], None, ALU.is_ge, None, [me, m8], [me])
                    K.tt("vector", ex[:], ex[:], me[:], ALU.mult, [ex, me], [ex])
                    K.ts("vector", wgt[:, j, :], ex[:], s2[:, 11:12], None, ALU.mult, None, [ex, s2], [wgt])
                K.dump(f"wgt{half}", wgt[:].rearrange("p a b -> p (a b)"), [128, HT * 32], F32, [wgt.k])
                for e_ in range(moe_experts):
                    wg_ = wgu[wl[0] % 2]; wd_ = wdn[wl[0] % 2]; wl[0] += 1
                    K.dma("gpsimd", wg_[:], w_gu_d[e_].rearrange("(c p) f -> p c f", p=128), [], [wg_])
                    K.dma("gpsimd", wd_[:], w_dn_d[e_].rearrange("(c p) f -> p c f", p=128), [], [wd_])
                    for tch in range(HT // 4):
                        a_ = aT[tch % 2]
                        csl = slice(tch * 512, (tch + 1) * 512)
                        for fo in range(2):
                            pg_ = bank(); pu_ = bank()
                            for k in range(8):
                                K.mm(pg_[:, 0:512], wg_[:, k, fo * 128:(fo + 1) * 128], h2T[:, k, csl], k == 0, k == 7, [wg_, h2T], [pg_])
                            for k in range(8):
                                K.mm(pu_[:, 0:512], wg_[:, k, 256 + fo * 128:256 + (fo + 1) * 128], h2T[:, k, csl], k == 0, k == 7, [wg_, h2T], [pu_])
                            s_ = sg[fo]
                            K.act(s_[:, :], pg_[:, 0:512], ACT.Silu, [pg_], [s_])
                            K.tt("vector", a_[:, fo, :], pu_[:, 0:512], s_[:, :], ALU.mult, [pu_, s_], [a_])
                        for tj in range(4):
                            tile_ = tch * 4 + tj
                            for hf in range(2):
                                po_ = bank((4, 5, 6, 7))
                                for fo in range(2):
                                    K.mm(po_[:, 0:512], a_[:, fo, tj * 128:(tj + 1) * 128], wd_[:, fo, hf * 512:(hf + 1) * 512],
                                         fo == 0, fo == 1, [a_, wd_], [po_])
                                ysl = yacc[:, tile_, hf * 512:(hf + 1) * 512]
                                if e_ == 0:
                                    K.ts("vector", ysl, po_[:, 0:512], wgt[:, tile_, e_:e_ + 1], None, ALU.mult, None, [po_, wgt], [yacc])
                                else:
                                    K.stt("vector", ysl, po_[:, 0:512], wgt[:, tile_, e_:e_ + 1], ysl, ALU.mult, ALU.add, [po_, wgt, yacc], [yacc])
                for j in range(HT):
                    tt_ = half * HT + j
                    xt = xm[xi[0] % 2]; xi[0] += 1
                    K.dma("sync", xt[:], x1_d[tt_ * 128:(tt_ + 1) * 128, :], [x1_tok], [xt])
                    K.tt("gpsimd", xt[:], xt[:], yacc[:, j, :], ALU.add, [xt, yacc], [xt])
                    K.memset("vector", s2[:, 12:13], 0.0, [s2])
                    K.act(sq2[:], xt[:], ACT.Square, [xt, s2], [sq2, s2], accum_out=s2[:, 12:13])
                    K.act(s2[:, 13:14], s2[:, 12:13], ACT.Sqrt, [s2, cpi], [s2], scale=1.0 / D, bias=cpi[:, 1:2])
                    K.recip(s2[:, 14:15], s2[:, 13:14], [s2], [s2])
                    K.stt("vector", xn[:], xt[:], s2[:, 14:15], gfin[:], ALU.mult, ALU.mult, [xt, s2, gfin], [xn])
                    K.dma("sync", out_d[tt_ * 128:(tt_ + 1) * 128, :], xn[:], [xn], [])
            P.emit()
        P.emit(final=True)
    return nc, K


def _consts():
    p = np.arange(128)
    invf = np.stack([10000.0 ** (-(p % 32).astype(np.float32) / 32.0),
                     10000.0 ** (-(p % 16).astype(np.float32) / 16.0)], axis=1).astype(np.float32)
    selb = np.zeros((64, 64), np.float32)
    for c in range(64):
        for j in range(64):
            if j > c:
                selb[c, j] = -100.0
            elif j == 0 or j == c or j == c - 1:
                selb[c, j] = 100.0
    esel = np.zeros((64, 32, 128), np.float32)
    for i in range(32):
        for s_ in range(128):
            esel[2 * i + s_ // 64, i, s_] = 1.0
    return invf, selb, esel.reshape(64, 4096), np.eye(24, dtype=np.float32)


def make_in_map(inp, b):
    invf, selb, esel, eye = _consts()
    f = lambda a: np.ascontiguousarray(np.asarray(a))
    return {
        "x": f(inp["x"][b]), "positions": f(inp["positions"][b][None, :]),
        "norm_mix": f(np.asarray(inp["norm_mix"][0]).reshape(8, 128).T), "w_in": f(inp["w_in"][0]),
        "pe_k": f(inp["pe_k"][0]), "w1_k": f(inp["w1_k"][0]), "w2_k": f(inp["w2_k"][0]),
        "pe_v": f(inp["pe_v"][0]), "w1_v": f(inp["w1_v"][0]), "w2_v": f(inp["w2_v"][0]),
        "w_br_a": f(inp["w_br_a"][0]), "w_br_b": f(inp["w_br_b"][0]), "w_out": f(inp["w_out"][0]),
        "norm_ffn": f(np.asarray(inp["norm_ffn"][0]).reshape(8, 128).T),
        "w_group": f(inp["w_group"][0]), "b_group": f(inp["b_group"][0][None, :]),
        "w_expert": f(inp["w_expert"][0]), "b_expert": f(inp["b_expert"][0][None, :]),
        "w_gate_up": f(inp["w_gate_up"][0]), "w_down": f(inp["w_down"][0]),
        "norm_final": f(inp["norm_final"][None, :]),
        "c_invf": invf, "c_selb": selb, "c_esel": esel, "c_eye": eye,
    }


def kernel(**inputs):
    nc, K = build()
    in_maps = [make_in_map(inputs, b) for b in range(8)]
    res = run_bass_kernel_spmd(nc, in_maps, core_ids=list(range(8)))
    return np.stack([np.asarray(r["out"]) for r in res.results], axis=0).astype(np.float32)
```

```python
from contextlib import ExitStack
import numpy as np
import concourse.bass as bass
import concourse.mybir as mybir
from concourse.bass_utils import run_bass_kernel_spmd

F32 = mybir.dt.float32
BF16 = mybir.dt.bfloat16
I32 = mybir.dt.int32
ALU = mybir.AluOpType
ACT = mybir.ActivationFunctionType
AX = mybir.AxisListType

ENGS = ("tensor", "vector", "scalar", "gpsimd", "sync")

PIPE_DEPTH = 2
NEB = 4
PAIR = 1
L = 4096
D = 1024
NT = 32
NCH = 8
EPS = 1e-6
PI = float(np.pi)
TWO_PI = float(2 * np.pi)

QA, KA, VA, QI, KI, WI, QB = 0, 512, 576, 640, 896, 928, 936
KC, VC, KS, VS, KW, VW, GB, GA, GBT = 1448, 1576, 1704, 1832, 1960, 2088, 2216, 2240, 3264


class Tok:
    __slots__ = ("last_w", "readers")

    def __init__(self):
        self.last_w = None
        self.readers = []


class Op:
    __slots__ = ("eng", "fn", "deps", "is_dma", "sig", "signal", "idx", "prev")

    def __init__(self, eng, fn, is_dma):
        self.eng = eng
        self.fn = fn
        self.deps = set()
        self.is_dma = is_dma
        self.sig = None
        self.signal = False
        self.prev = None


class Prog:
    N_DMA_SEMS = 8

    def __init__(self, nc, ctx):
        self.nc = nc
        self.ops = []
        self.done = 0
        self.eng_sems = {e: ctx.enter_context(nc.semaphore(f"s_{e}")) for e in ENGS}
        self.dma_sems = {e: [ctx.enter_context(nc.semaphore(f"d_{e}_{i}")) for i in range(self.N_DMA_SEMS)]
                         for e in ENGS}
        self.eng_cnt = {e: 0 for e in ENGS}
        self.dma_rr = {e: 0 for e in ENGS}
        self.dma_cnt = {e: [0] * self.N_DMA_SEMS for e in ENGS}

    def _add(self, eng, fn, reads, writes, is_dma=False):
        op = Op(eng, fn, is_dma)
        op.idx = len(self.ops)
        for t in reads:
            if t.last_w is not None:
                op.deps.add(t.last_w)
        for t in writes:
            if t.last_w is not None:
                op.deps.add(t.last_w)
            op.deps.update(t.readers)
        op.deps.discard(op.idx)
        for t in reads:
            t.readers.append(op.idx)
        for t in writes:
            t.last_w = op.idx
            t.readers = []
        self.ops.append(op)
        return op

    def op(self, eng, fn, reads=(), writes=()):
        return self._add(eng, fn, list(reads), list(writes))

    def dma(self, eng, out, in_, reads=(), writes=(), **kw):
        return self._add(eng, lambda e: e.dma_start(out=out, in_=in_, **kw), list(reads), list(writes), True)

    def _sem(self, key):
        return self.eng_sems[key[1]] if key[0] == "e" else self.dma_sems[key[1]][key[2]]

    def emit(self, final=False):
        nc = self.nc
        ops = self.ops
        new = ops[self.done:]
        pre = []
        for e in ENGS:
            if self.eng_cnt[e] > 0:
                pre.append((("e", e), self.eng_cnt[e]))
            for k in range(self.N_DMA_SEMS):
                if self.dma_cnt[e][k] > 0:
                    pre.append((("d", e, k), self.dma_cnt[e][k]))
        for op in new:
            for d in op.deps:
                if d >= self.done:
                    ops[d].signal = True
        per_eng = {e: [] for e in ENGS}
        for op in new:
            per_eng[op.eng].append(op)
        for e in ENGS:
            for op in reversed(per_eng[e]):
                if not op.is_dma:
                    op.signal = True
                    break
        for op in new:
            if op.is_dma:
                k = self.dma_rr[op.eng]
                self.dma_rr[op.eng] = (k + 1) % self.N_DMA_SEMS
                prev = self.dma_cnt[op.eng][k]
                self.dma_cnt[op.eng][k] = prev + 16
                op.sig = (("d", op.eng, k), prev + 16)
                op.prev = (("d", op.eng, k), prev)
            elif op.signal:
                self.eng_cnt[op.eng] += 1
                op.sig = (("e", op.eng), self.eng_cnt[op.eng])
        finals = []
        if final:
            for e in ENGS:
                for k in range(self.N_DMA_SEMS):
                    if self.dma_cnt[e][k] > 0:
                        finals.append((("d", e, k), self.dma_cnt[e][k]))
        done = self.done

        def run_engine(ename, eobj):
            known = {}
            for key, val in pre:
                if key == ("e", ename):
                    continue
                eobj.wait_ge(self._sem(key), val)
                known[key] = val
            for op in per_eng[ename]:
                waits = {}
                for d in op.deps:
                    if d < done:
                        continue
                    dop = ops[d]
                    key, val = dop.sig
                    if ename == "tensor" and dop.eng == "tensor" and not dop.is_dma:
                        continue
                    if known.get(key, 0) >= val:
                        continue
                    waits[key] = max(waits.get(key, 0), val)
                if op.is_dma:
                    key, val = op.prev
                    if val > 0 and known.get(key, 0) < val:
                        waits[key] = max(waits.get(key, 0), val)
                for key, val in waits.items():
                    eobj.wait_ge(self._sem(key), val)
                    known[key] = val
                ins = op.fn(eobj)
                if op.sig is not None:
                    ins.then_inc(self._sem(op.sig[0]), 16 if op.is_dma else 1)
            if ename == "sync":
                for key, val in finals:
                    if known.get(key, 0) < val:
                        eobj.wait_ge(self._sem(key), val)

        with nc.Block() as block:
            @block.tensor
            def _(e):
                run_engine("tensor", e)

            @block.vector
            def _(e):
                run_engine("vector", e)

            @block.scalar
            def _(e):
                run_engine("scalar", e)

            @block.gpsimd
            def _(e):
                run_engine("gpsimd", e)

            @block.sync
            def _(e):
                run_engine("sync", e)
        self.done = len(ops)


class Buf:
    def __init__(self, t):
        self.t = t
        self.k = Tok()

    def __getitem__(self, key):
        return self.t[key]


class View:
    def __init__(self, ap, k):
        self.ap = ap
        self.k = k

    def __getitem__(self, key):
        return self.ap[key]


class KB:
    def __init__(self, nc, dbg=None):
        self.nc = nc
        self.n = 0
        self.dbg = dbg if dbg is not None else {}
        self.dbg_out = {}

    def sb(self, ctx, shape, dt=F32):
        self.n += 1
        return Buf(ctx.enter_context(self.nc.sbuf_tensor(f"sb{self.n}", list(shape), dt)))

    def ps(self, ctx, shape, dt=F32):
        self.n += 1
        b = Buf(ctx.enter_context(self.nc.psum_tensor(f"ps{self.n}", list(shape), dt)))
        b.is_psum = True
        return b

    def dump(self, name, ap, shape, dt, reads):
        if name not in self.dbg:
            return
        d = self.nc.dram_tensor("dbg_" + name, list(shape), dt, kind="ExternalOutput").ap()
        self.dbg_out[name] = d
        self.P.dma("sync", d, ap, reads=reads)

    @staticmethod
    def _tk(lst):
        return [b.k if hasattr(b, "k") else b for b in lst]

    @staticmethod
    def _rw(r, w):
        rr_, ww_ = [], list(w)
        for b in r:
            if getattr(b, "is_psum", False):
                if b not in ww_:
                    ww_.append(b)
            else:
                rr_.append(b)
        tk = lambda lst: [b.k if hasattr(b, "k") else b for b in lst]
        return tk(rr_), tk(ww_)

    def mm(self, out, lhsT, rhs, start, stop, r, w):
        self.P.op("tensor", lambda e: e.matmul(out, lhsT=lhsT, rhs=rhs, start=start, stop=stop), *self._rw(r, w))

    def tr(self, out, in_, ident, r, w):
        self.P.op("tensor", lambda e: e.transpose(out=out, in_=in_, identity=ident), *self._rw(r, w))

    def act(self, out, in_, func, r, w, **kw):
        self.P.op("scalar", lambda e: e.activation(out=out, in_=in_, func=func, **kw), *self._rw(r, w))

    def tt(self, eng, out, in0, in1, op, r, w):
        self.P.op(eng, lambda e: e.tensor_tensor(out=out, in0=in0, in1=in1, op=op), *self._rw(r, w))

    def ts(self, eng, out, in0, s1, s2, op0, op1, r, w, accum_out=None):
        if op1 is None:
            self.P.op(eng, lambda e: e.tensor_scalar(out=out, in0=in0, scalar1=s1, scalar2=None, op0=op0), *self._rw(r, w))
        elif accum_out is None:
            self.P.op(eng, lambda e: e.tensor_scalar(out=out, in0=in0, scalar1=s1, scalar2=s2, op0=op0, op1=op1), *self._rw(r, w))
        else:
            self.P.op(eng, lambda e: e.tensor_scalar(out=out, in0=in0, scalar1=s1, scalar2=s2, op0=op0, op1=op1, accum_out=accum_out), *self._rw(r, w))

    def stt(self, eng, out, in0, scalar, in1, op0, op1, r, w):
        self.P.op(eng, lambda e: e.scalar_tensor_tensor(out=out, in0=in0, scalar=scalar, in1=in1, op0=op0, op1=op1), *self._rw(r, w))

    def cp(self, eng, out, in_, r, w):
        if eng == "scalar":
            self.P.op(eng, lambda e: e.copy(out=out, in_=in_), *self._rw(r, w))
        else:
            self.P.op(eng, lambda e: e.tensor_copy(out=out, in_=in_), *self._rw(r, w))

    def memset(self, eng, ap, val, w):
        self.P.op(eng, lambda e: e.memset(ap, val), [], self._tk(w))

    def asel(self, out, in_, pattern, cmp, fill, base, cm, r, w):
        self.P.op("gpsimd", lambda e: e.affine_select(out=out, in_=in_, pattern=pattern, compare_op=cmp, fill=fill, base=base, channel_multiplier=cm), *self._rw(r, w))

    def recip(self, out, in_, r, w):
        self.P.op("vector", lambda e: e.reciprocal(out=out, in_=in_), *self._rw(r, w))

    def dma(self, eng, out, in_, r, w, **kw):
        r_, w_ = self._rw(r, w)
        self.P.dma(eng, out, in_, r_, w_, **kw)


def bc(ap, shape):
    return ap.to_broadcast(list(shape))


def build(dbg=None, qtiles=None, stop_after=None, moe_experts=32, lvl=99, moe_from_x=False, skip_att=False):
    nc = bass.Bass("TRN2", target_bir_lowering=False)
    K = KB(nc, dbg)

    def din(name, shape, dt=F32):
        return nc.dram_tensor(name, list(shape), dt, kind="ExternalInput").ap()

    x_d = din("x", [L, D])
    pos_d = din("positions", [1, L], I32)
    norm_mix_d = din("norm_mix", [128, 8])
    w_in_d = din("w_in", [D, 4288])
    pe_k_d = din("pe_k", [32, 64]); w1_k_d = din("w1_k", [2048, 128]); w2_k_d = din("w2_k", [128, 64])
    pe_v_d = din("pe_v", [32, 64]); w1_v_d = din("w1_v", [2048, 128]); w2_v_d = din("w2_v", [128, 64])
    w_br_a_d = din("w_br_a", [512, D]); w_br_b_d = din("w_br_b", [512, D]); w_out_d = din("w_out", [D, D])
    norm_ffn_d = din("norm_ffn", [128, 8])
    w_group_d = din("w_group", [D, 4]); b_group_d = din("b_group", [1, 4])
    w_expert_d = din("w_expert", [D, 32]); b_expert_d = din("b_expert", [1, 32])
    w_gu_d = din("w_gate_up", [32, D, 512]); w_dn_d = din("w_down", [32, 256, D])
    norm_final_d = din("norm_final", [1, D])
    c_invf_d = din("c_invf", [128, 2])
    c_selb_d = din("c_selb", [64, 64])
    c_esel_d = din("c_esel", [64, 4096])
    c_eye_d = din("c_eye", [24, 24])
    out_d = nc.dram_tensor("out", [L, D], F32, kind="ExternalOutput").ap()
    x1_d = nc.dram_tensor("x1s", [L, D], F32, kind="Internal").ap()
    NG_Q = 40
    wq_d = nc.dram_tensor("wq_bf", [NG_Q, 128, 1024], BF16, kind="Internal").ap()

    w_in_v = w_in_d.rearrange("(c p) n -> p c n", p=128)
    wq_tok = Tok()
    x1_tok = Tok()

    with ExitStack() as top:
        P = Prog(nc, top)
        K.P = P
        ident = K.sb(top, [128, 128], BF16)
        diagT = K.sb(top, [128, 128], BF16)
        antiT = K.sb(top, [128, 128], BF16)
        invf = K.sb(top, [128, 2])
        gmix = K.sb(top, [128, 8])
        cpi = K.sb(top, [128, 2])
        with ExitStack() as c0:
            tmpf = K.sb(c0, [128, 128])
            K.memset("gpsimd", tmpf[:], 1.0, [tmpf])
            K.asel(tmpf[:], tmpf[:], [[-1, 128]], ALU.is_equal, 0.0, 0, 1, [tmpf], [tmpf])
            K.cp("vector", ident[:], tmpf[:], [tmpf], [ident])
            tmp2 = K.sb(c0, [128, 128])
            K.memset("gpsimd", tmp2[:], 1.0, [tmp2])
            K.asel(tmp2[:], tmp2[:], [[1, 128]], ALU.is_ge, 0.0, 0, -1, [tmp2], [tmp2])
            K.cp("vector", diagT[:], tmp2[:], [tmp2], [diagT])
            tmp3 = K.sb(c0, [128, 128])
            K.memset("gpsimd", tmp3[:], 1.0, [tmp3])
            K.asel(tmp3[:], tmp3[:], [[-1, 128]], ALU.is_gt, 0.0, 0, 1, [tmp3], [tmp3])
            K.cp("vector", antiT[:], tmp3[:], [tmp3], [antiT])
            K.dma("sync", invf[:], c_invf_d, [], [invf])
            K.dma("sync", gmix[:], norm_mix_d, [], [gmix])
            K.memset("vector", cpi[:, 0:1], PI / 2, [cpi])
            K.memset("vector", cpi[:, 1:2], EPS, [cpi])
            P.emit()

        banks = [K.ps(top, [128, 512]) for _ in range(8)]
        rr = [0]

        def bank(pool=(0, 1, 2, 3)):
            b = banks[pool[rr[0] % len(pool)]]
            rr[0] += 1
            return b

        def bfv(b):
            return b.t[:].bitcast(BF16)

        def make_hT(c, hTc, xb, sq, hn, st, g):
            for j in range(4):
                tt_ = 4 * c + j
                xt = xb[j % len(xb)]
                K.dma("sync", xt[:], x_d[tt_ * 128:(tt_ + 1) * 128, :], [], [xt])
                K.memset("vector", st[:, 0:1], 0.0, [st])
                K.act(sq[:], xt[:], ACT.Square, [xt, st], [sq, st], accum_out=st[:, 0:1])
                K.act(st[:, 1:2], st[:, 0:1], ACT.Sqrt, [st, cpi], [st], scale=1.0 / D, bias=cpi[:, 1:2])
                K.recip(st[:, 2:3], st[:, 1:2], [st], [st])
                K.ts("vector", hn[:], xt[:], st[:, 2:3], None, ALU.mult, None, [xt, st], [hn])
                pb = bank()
                for k in range(8):
                    K.tr(bfv(pb)[:, k * 128:(k + 1) * 128], hn[:, k * 128:(k + 1) * 128], ident[:], [hn, ident], [pb])
                K.tt("vector", hTc[:, :, j * 128:(j + 1) * 128], bfv(pb).rearrange("p (k t) -> p k t", k=8),
                     bc(g[:, :].unsqueeze(2), [128, 8, 128]), ALU.mult, [pb, g], [hTc])

        def make_rope(c, posi, posf, tq, tabs):
            K.dma("sync", posi[:], pos_d[:, c * 512:(c + 1) * 512].partition_broadcast(128), [], [posi])
            K.cp("vector", posf[:], posi[:], [posi], [posf])
            for col, (cn, sn) in enumerate((("cos64", "sin64"), ("cos32", "sin32"))):
                ang = tq[0]; kf = tq[1]; ki = tq[2]
                K.ts("vector", ang[:], posf[:], invf[:, col:col + 1], None, ALU.mult, None, [posf, invf], [ang])
                K.ts("vector", ki[:], ang[:], 1.0 / TWO_PI, None, ALU.mult, None, [ang], [ki])
                K.cp("vector", kf[:], ki[:], [ki], [kf])
                K.stt("vector", ang[:], kf[:], -TWO_PI, ang[:], ALU.mult, ALU.add, [kf, ang], [ang])
                K.ts("vector", kf[:], ang[:], PI, -TWO_PI, ALU.is_gt, ALU.mult, [ang], [kf])
                K.tt("vector", ang[:], ang[:], kf[:], ALU.add, [ang, kf], [ang])
                K.ts("vector", kf[:], ang[:], -PI, TWO_PI, ALU.is_lt, ALU.mult, [ang], [kf])
                K.tt("vector", ang[:], ang[:], kf[:], ALU.add, [ang, kf], [ang])
                K.act(tabs[sn][:], ang[:], ACT.Sin, [ang], [tabs[sn]])
                K.stt("vector", kf[:], ang[:], -1.0, ang[:], ALU.mult, ALU.max, [ang], [kf])
                K.act(tabs[cn][:], kf[:], ACT.Sin, [kf, cpi], [tabs[cn]], scale=-1.0, bias=cpi[:, 0:1])

        def proj_fm(hTc, wA, M, dst, dstb, wB=None, cos=None, sin=None, tmp=None, evac="scalar", func=None, N=512):
            pa = bank()
            for k in range(8):
                K.mm(pa[0:M, 0:N], wA[0][:, k, :], hTc[:, k, 0:N], k == 0, k == 7, [wA[1], hTc], [pa])
            if wB is None:
                if func is not None:
                    K.act(dst, pa[0:M, 0:N], func, [pa], [dstb])
                else:
                    K.cp(evac, dst, pa[0:M, 0:N], [pa], [dstb])
                return
            pb = bank()
            for k in range(8):
                K.mm(pb[0:M, 0:N], wB[0][:, k, :], hTc[:, k, 0:N], k == 0, k == 7, [wB[1], hTc], [pb])
            t1, t2 = tmp
            K.tt("vector", t1[0:M, 0:N], pa[0:M, 0:N], cos[0:M, 0:N], ALU.mult, [pa, cos], [t1])
            K.tt("vector", t2[0:M, 0:N], pb[0:M, 0:N], sin[0:M, 0:N], ALU.mult, [pb, sin], [t2])
            K.tt("gpsimd", dst, t1[0:M, 0:N], t2[0:M, 0:N], ALU.add, [t1, t2], [dstb])

        att = ExitStack()
        kaT2 = K.sb(att, [128, L], BF16)
        kiT3 = K.sb(att, [96, L], BF16)
        ksT = K.sb(att, [128, L], BF16)
        kwT = K.sb(att, [128, L], BF16)
        va3 = K.sb(att, [128, NT, 192], BF16)
        vs3 = K.sb(att, [128, NT, 192], BF16)
        vw3 = K.sb(att, [128, NT, 192], BF16)
        kcT2 = K.sb(att, [128, 256], BF16)
        vctm = K.sb(att, [128, 2, 128], BF16)

        with ExitStack() as pw:
            stg = [K.sb(pw, [128, 8, 512]) for _ in range(2)]
            grp = [K.sb(pw, [128, 8, 128], BF16) for _ in range(3)]
            wBfm = K.sb(pw, [128, 8, 1248], BF16)
            wBtm = K.sb(pw, [128, 8, 320], BF16)
            gi = [0]

            def load_seg(i, c0, n):
                s = stg[i % 2]
                K.dma("sync", s[:, :, 0:n], w_in_v[:, :, c0:c0 + n], [], [s])
                return s

            def rot_into(dst_ap_fn, dstb, s, off, half):
                K.ts("vector", dst_ap_fn(0, half), s[:, :, off + half:off + 2 * half], -1.0, None, ALU.mult, None, [s], [dstb])
                K.cp("gpsimd", dst_ap_fn(half, 2 * half), s[:, :, off:off + half], [s], [dstb])

            def store_grp(gidx, g):
                K.dma("sync", wq_d[gidx].rearrange("p (c m) -> p c m", c=8), g[:], [g], [wq_tok])

            def new_grp():
                g = grp[gi[0] % 3]
                gi[0] += 1
                return g

            s = load_seg(0, QA, 512)
            for g_ in range(4):
                g = new_grp()
                for u in range(2):
                    h = 4 * u + g_
                    K.cp("scalar", g[:, :, u * 64:(u + 1) * 64], s[:, :, h * 64:(h + 1) * 64], [s], [g])
                store_grp(g_, g)
                g = new_grp()
                for u in range(2):
                    h = 4 * u + g_
                    rot_into(lambda a, b, u=u, g=g: g[:, :, u * 64 + a:u * 64 + b], g, s, h * 64, 32)
                store_grp(4 + g_, g)
            s = load_seg(1, 512, 424)
            o_ka, o_va, o_qi, o_ki, o_wi = 0, 64, 128, 384, 416
            BF_KA_A, BF_KA_B, BF_KI_A, BF_KI_B, BF_KS_A, BF_KS_B, BF_KW_A, BF_KW_B, BF_KC, BF_VC = \
                0, 128, 256, 352, 448, 576, 704, 832, 960, 1088
            for u in range(2):
                K.cp("scalar", wBfm[:, :, BF_KA_A + u * 64:BF_KA_A + (u + 1) * 64], s[:, :, o_ka:o_ka + 64], [s], [wBfm])
                rot_into(lambda a, b, u=u: wBfm[:, :, BF_KA_B + u * 64 + a:BF_KA_B + u * 64 + b], wBfm, s, o_ka, 32)
            for r_ in range(3):
                K.cp("scalar", wBfm[:, :, BF_KI_A + r_ * 32:BF_KI_A + (r_ + 1) * 32], s[:, :, o_ki:o_ki + 32], [s], [wBfm])
                rot_into(lambda a, b, r_=r_: wBfm[:, :, BF_KI_B + r_ * 32 + a:BF_KI_B + r_ * 32 + b], wBfm, s, o_ki, 16)
            K.cp("scalar", wBtm[:, :, 0:64], s[:, :, o_va:o_va + 64], [s], [wBtm])
            for q_ in range(3):
                hs = [3 * q_ + i for i in range(3) if 3 * q_ + i < 8]
                g = new_grp()
                K.memset("vector", g[:], 0.0, [g])
                for i, h in enumerate(hs):
                    K.cp("scalar", g[:, :, i * 32:(i + 1) * 32], s[:, :, o_qi + h * 32:o_qi + (h + 1) * 32], [s], [g])
                store_grp(8 + q_, g)
                g = new_grp()
                K.memset("vector", g[:], 0.0, [g])
                for i, h in enumerate(hs):
                    rot_into(lambda a, b, i=i, g=g: g[:, :, i * 32 + a:i * 32 + b], g, s, o_qi + h * 32, 16)
                store_grp(11 + q_, g)
            s = load_seg(0, QB, 512)
            for g_ in range(4):
                g = new_grp() if True else None
                for u in range(2):
                    h = 4 * u + g_
                    K.cp("scalar", g[:, :, u * 64:(u + 1) * 64], s[:, :, h * 64:(h + 1) * 64], [s], [g])
                store_grp(14 + g_, g)
                g = new_grp()
                for u in range(2):
                    h = 4 * u + g_
                    rot_into(lambda a, b, u=u, g=g: g[:, :, u * 64 + a:u * 64 + b], g, s, h * 64, 32)
                store_grp(18 + g_, g)
            s = load_seg(1, KC, 512)
            K.cp("scalar", wBfm[:, :, BF_KC:BF_KC + 128], s[:, :, 0:128], [s], [wBfm])
            K.cp("scalar", wBfm[:, :, BF_VC:BF_VC + 128], s[:, :, 128:256], [s], [wBfm])
            K.cp("scalar", wBfm[:, :, BF_KS_A:BF_KS_A + 128], s[:, :, 256:384], [s], [wBfm])
            for u in range(2):
                rot_into(lambda a, b, u=u: wBfm[:, :, BF_KS_B + u * 64 + a:BF_KS_B + u * 64 + b], wBfm, s, 256 + u * 64, 32)
            K.cp("scalar", wBtm[:, :, 64:192], s[:, :, 384:512], [s], [wBtm])
            s = load_seg(0, KW, 280)
            K.cp("scalar", wBfm[:, :, BF_KW_A:BF_KW_A + 128], s[:, :, 0:128], [s], [wBfm])
            for u in range(2):
                rot_into(lambda a, b, u=u: wBfm[:, :, BF_KW_B + u * 64 + a:BF_KW_B + u * 64 + b], wBfm, s, u * 64, 32)
            K.cp("scalar", wBtm[:, :, 192:320], s[:, :, 128:256], [s], [wBtm])
            gG = K.sb(pw, [128, 8, 128], BF16)
            K.memset("vector", gG[:], 0.0, [gG])
            K.cp("scalar", gG[:, :, 0:24], s[:, :, 256:280], [s], [gG])
            wis = K.sb(pw, [128, 8, 8])
            K.dma("sync", wis[:], w_in_v[:, :, WI:WI + 8], [], [wis])
            K.cp("scalar", gG[:, :, 24:32], wis[:], [wis], [gG])
            store_grp(38, gG)
            for half in range(4):
                s = load_seg(half + 1, GA + half * 512, 512)
                for q_ in range(4):
                    g = new_grp()
                    K.cp("scalar" if q_ % 2 == 0 else "vector", g[:], s[:, :, q_ * 128:(q_ + 1) * 128], [s], [g])
                    store_grp(22 + half * 4 + q_, g)

            xb = [K.sb(pw, [128, D]) for _ in range(2)]
            sq = K.sb(pw, [128, D], BF16)
            hn = K.sb(pw, [128, D], BF16)
            st = K.sb(pw, [128, 4])
            hTc = K.sb(pw, [128, 8, 512], BF16)
            posi = K.sb(pw, [128, 512], I32)
            posf = K.sb(pw, [128, 512])
            tq = [K.sb(pw, [128, 512]), K.sb(pw, [128, 512]), K.sb(pw, [128, 512], I32)]
            tabs = {n: K.sb(pw, [128, 512], BF16) for n in ("cos64", "sin64", "cos32", "sin32")}
            rt = (K.sb(pw, [128, 512]), K.sb(pw, [128, 512]))
            kcmpT = K.sb(pw, [128, L], BF16)
            vcmpT = K.sb(pw, [128, L], BF16)
            for v3 in (va3, vs3, vw3):
                K.memset("gpsimd", v3[:, :, 64:128], 1.0, [v3])
            for c in range(NCH):
                make_hT(c, hTc, xb, sq, hn, st, gmix)
                make_rope(c, posi, posf, tq, tabs)
                cs = slice(c * 512, (c + 1) * 512)
                W = lambda off, m: (wBfm[:, :, off:off + m], wBfm)
                proj_fm(hTc, W(BF_KA_A, 128), 128, kaT2[:, cs], kaT2, W(BF_KA_B, 128), tabs["cos64"], tabs["sin64"], rt)
                proj_fm(hTc, W(BF_KI_A, 96), 96, kiT3[:, cs], kiT3, W(BF_KI_B, 96), tabs["cos32"], tabs["sin32"], rt)
                proj_fm(hTc, W(BF_KS_A, 128), 128, ksT[:, cs], ksT, W(BF_KS_B, 128), tabs["cos64"], tabs["sin64"], rt)
                proj_fm(hTc, W(BF_KW_A, 128), 128, kwT[:, cs], kwT, W(BF_KW_B, 128), tabs["cos64"], tabs["sin64"], rt)
                proj_fm(hTc, W(BF_KC, 128), 128, kcmpT[:, cs], kcmpT)
                proj_fm(hTc, W(BF_VC, 128), 128, vcmpT[:, cs], vcmpT, evac="vector")
                for j in range(4):
                    tt_ = 4 * c + j
                    pv = bank()
                    for k in range(8):
                        K.mm(pv[:, 0:320], hTc[:, k, j * 128:(j + 1) * 128], wBtm[:, k, :], k == 0, k == 7, [hTc, wBtm], [pv])
                    K.cp("scalar", va3[:, tt_, 0:64], pv[:, 0:64], [pv], [va3])
                    K.cp("vector", va3[:, tt_, 128:192], pv[:, 0:64], [pv], [va3])
                    K.cp("scalar", vs3[:, tt_, 0:64], pv[:, 64:128], [pv], [vs3])
                    K.cp("vector", vs3[:, tt_, 128:192], pv[:, 128:192], [pv], [vs3])
                    K.cp("scalar", vw3[:, tt_, 0:64], pv[:, 192:256], [pv], [vw3])
                    K.cp("vector", vw3[:, tt_, 128:192], pv[:, 256:320], [pv], [vw3])
            K.dump("kaT2", kaT2[:], [128, L], BF16, [kaT2.k])
            K.dump("kiT3", kiT3[:], [96, L], BF16, [kiT3.k])
            K.dump("ksT", ksT[:], [128, L], BF16, [ksT.k])
            K.dump("kwT", kwT[:], [128, L], BF16, [kwT.k])
            K.dump("va3", va3[:].rearrange("p a b -> p (a b)"), [128, NT * 192], BF16, [va3.k])
            K.dump("vs3", vs3[:].rearrange("p a b -> p (a b)"), [128, NT * 192], BF16, [vs3.k])

            w1 = K.sb(pw, [128, 32, 128], BF16)
            w2d = K.sb(pw, [128, 128], BF16)
            peT = K.sb(pw, [64, 32], BF16)
            hid = K.sb(pw, [128, 256], BF16)
            cb = K.sb(pw, [128, 1])
            K.memset("vector", kcT2[:], 0.0, [kcT2])
            for kind, (pe_d, w1_d, w2_d, srcT) in enumerate(((pe_k_d, w1_k_d, w2_k_d, kcmpT), (pe_v_d, w1_v_d, w2_v_d, vcmpT))):
                w1v = w1_d.rearrange("(j d) c -> d j c", d=64)
                K.dma("gpsimd", w1[0:64, :, :], w1v, [], [w1])
                K.dma("gpsimd", w1[64:128, :, :], w1v, [], [w1])
                K.dma("gpsimd", w2d[:, 0:64], w2_d, [], [w2d])
                K.dma("gpsimd", w2d[:, 64:128], w2_d, [], [w2d])
                K.dma("gpsimd", peT[:], pe_d.rearrange("j d -> d j"), [], [peT], allow_slow_non_contiguous=True)
                pbias = bank()
                for j in range(32):
                    K.mm(pbias[:, 0:1], w1[0:64, j, :], peT[:, j:j + 1], j == 0, j == 31, [w1, peT], [pbias])
                K.cp("vector", cb[:], pbias[:, 0:1], [pbias], [cb])
                for kk in range(2):
                    ph = bank()
                    lo = 64 * kk
                    for j in range(32):
                        K.mm(ph[:, 0:255], w1[lo:lo + 64, j, :], srcT[lo:lo + 64, j:j + 16 * 254 + 1:16],
                             j == 0, j == 31, [w1, srcT], [ph])
                    K.memset("vector", hid[:, 255:256], 0.0, [hid])
                    K.act(hid[:, 0:255], ph[:, 0:255], ACT.Silu, [ph, cb], [hid], bias=cb[:, 0:1])
                    if kind == 0:
                        po = bank()
                        K.mm(po[:, 0:256], w2d[:], hid[:], True, True, [w2d, hid], [po])
                        K.cp("vector", kcT2[lo:lo + 64, :], po[lo:lo + 64, 0:256], [po], [kcT2])
                    else:
                        for ch in range(2):
                            po = bank()
                            K.mm(po[:, 0:64], hid[:, ch * 128:(ch + 1) * 128], w2d[:, 0:64], True, True, [hid, w2d], [po])
                            K.cp("vector", vctm[:, ch, lo:lo + 64], po[:, 0:64], [po], [vctm])
            K.dump("kcT2", kcT2[:], [128, 256], BF16, [kcT2.k])
            K.dump("vctm", vctm[:].rearrange("p a b -> p (a b)"), [128, 256], BF16, [vctm.k])
            P.emit()
        if stop_after == "B":
            P.emit(final=True)
            att.close()
            return nc, K
        with ExitStack() as pc:
            qt_list = list(range(NT)) if qtiles is None else list(qtiles)
            ch_list = sorted(set(q // 4 for q in qt_list))
            RA = K.sb(pc, [128, L])
            idx = RA
            wout = View(RA.t[:].bitcast(BF16).rearrange("p (c f) -> p c f", c=8), RA.k)
            RBm = K.sb(pc, [128, L], BF16)
            mask = RBm
            wbra = View(RBm.t[:].rearrange("p (g f) -> p g f", g=4), RBm.k)
            RC = K.sb(pc, [128, NT, 128], BF16)
            maskT = RC
            wbrb = View(RC.t[:].rearrange("p a b -> p (a b)").rearrange("p (g f) -> p g f", g=4), RC.k)
            RD = K.sb(pc, [128, 8, 256])
            Ecmp = RD
            mergedT = View(RD.t[:].rearrange("p a b -> p (a b)").bitcast(BF16).rearrange("p (c t) -> p c t", c=8), RD.k)
            RE = K.sb(pc, [128, 2560])
            kEa, kEb, kEc = Tok(), Tok(), Tok()
            posi = View(RE.t[:, 0:512].bitcast(I32), kEa)
            posf = View(RE.t[:, 512:1024], kEa)
            ang_ = View(RE.t[:, 1024:1536], kEb)
            kf_ = View(RE.t[:, 1536:2048], kEb)
            ki_ = View(RE.t[:, 2048:2560].bitcast(I32), kEc)
            p_bf = View(RE.t[:, 0:1024].bitcast(BF16).rearrange("p (h n) -> p h n", h=8), kEa)
            pT = View(RE.t[:, 1024:2048].bitcast(BF16).rearrange("p (c h t) -> p c h t", c=2, h=8), kEb)
            R0 = View(RE.t[:, 2048:2560], kEc)
            R1 = K.sb(pc, [128, 512])
            ob = K.sb(pc, [128, 512])
            rt2 = (R1, ob)
            xb = [K.sb(pc, [128, D])]
            hn = K.sb(pc, [128, D], BF16)
            st = K.sb(pc, [128, 4])
            hTc = K.sb(pc, [128, 8, 512], BF16)
            tabs = {n: K.sb(pc, [128, 512], BF16) for n in ("cos64", "sin64", "cos32", "sin32")}
            ws = [K.sb(pc, [128, 8, 128], BF16) for _ in range(2)]
            wG = K.sb(pc, [128, 8, 128], BF16)
            qaT = K.sb(pc, [128, 4, 512], BF16)
            qiT = K.sb(pc, [96, 3, 512], BF16)
            qnT = K.sb(pc, [128, 4, 512], BF16)
            qrT = K.sb(pc, [128, 4, 512], BF16)
            gT = K.sb(pc, [32, 512], BF16)
            oaTc = K.sb(pc, [128, 4, 512], BF16)
            obTc = K.sb(pc, [128, 4, 512], BF16)
            Eb = [K.sb(pc, [128, 512], BF16) for _ in range(NEB)]
            Pb = [K.sb(pc, [128, 512], BF16) for _ in range(NEB)]
            Esel = K.sb(pc, [64, 32, 128], BF16)
            eye24 = K.sb(pc, [24, 24], BF16)
            ones24 = K.sb(pc, [24, 128], BF16)
            Dg = K.sb(pc, [24, 8, 128], BF16)
            gBs = [K.sb(pc, [128, 512], BF16) for _ in range(2)]
            rs = K.sb(pc, [128, 512])
            sm = K.sb(pc, [128, 64])
            wi_sb2 = [K.sb(pc, [128, 8]) for _ in range(2)]
            smb2 = [K.sb(pc, [128, 32]) for _ in range(2)]
            P4 = K.sb(pc, [128, 2, 256])
            imp = K.sb(pc, [128, 2, 64])
            scs = K.sb(pc, [128, 2, 64])
            sc2 = K.sb(pc, [128, 64])
            selb = K.sb(pc, [128, 64])
            bm = K.sb(pc, [128, 2, 64], BF16)
            bmT = K.sb(pc, [64, 2, 128], BF16)
            mexp = [K.sb(pc, [128, 2, 128], BF16) for _ in range(4)]
            er = [0]

            def Enext():
                er[0] += 1
                return Eb[er[0] % NEB], Pb[er[0] % NEB]

            def pipe(units, qk_fn, pv_fn, depth=PIPE_DEPTH, hook=None):
                if PAIR and len(units) >= 2 and hasattr(qk_fn, "mm"):
                    sis = []
                    for un in units:
                        if un[0] not in sis:
                            sis.append(un[0])
                    pend = []
                    for si in sis:
                        sts = [qk_fn.mm((si, u)) for u in range(2)]
                        outs = [qk_fn.post((si, u), sts[u]) for u in range(2)]
                        pend.append((si, outs))
                        if hook is not None:
                            hook(); hook()
                        if len(pend) > 1:
                            si0, o0 = pend.pop(0)
                            for u in range(2):
                                pv_fn((si0, u), o0[u])
                    for si0, o0 in pend:
                        for u in range(2):
                            pv_fn((si0, u), o0[u])
                    return
                pend = []
                for un in units:
                    pend.append((un, qk_fn(un)))
                    if hook is not None:
                        hook()
                    if len(pend) > depth:
                        pv_fn(*pend.pop(0))
                for p_ in pend:
                    pv_fn(*p_)

            with ExitStack() as cc:
                K.dma("gpsimd", Esel[:].rearrange("p a b -> p (a b)"), c_esel_d, [], [Esel])
                K.dma("gpsimd", eye24[:], c_eye_d, [], [eye24])
                K.memset("vector", ones24[:], 1.0, [ones24])
                K.memset("vector", sm[:, 32:33], 0.5, [sm])
                P.emit()

            def load_ws(gidx, i):
                w = ws[i % 2]
                K.dma("sync", w[:], wq_d[gidx].rearrange("p (c m) -> p c m", c=8), [wq_tok], [w])
                return w

            A_banks = (banks[4], banks[5])
            B_banks = (banks[6], banks[7])
            SC = 0.125
            wsi = [0]

            for c in ch_list:
                make_hT(c, hTc, xb, hn, hn, st, gmix)
                make_rope(c, posi, posf, (ang_, kf_, ki_), tabs)
                if lvl < 0.2:
                    continue
                for g_ in range(4):
                    wA = load_ws(g_, wsi[0]); wsi[0] += 1
                    wB = load_ws(4 + g_, wsi[0]); wsi[0] += 1
                    proj_fm(hTc, (wA[:], wA), 128, qaT[:, g_, :], qaT, (wB[:], wB), tabs["cos64"], tabs["sin64"], rt2)
                for q_ in (range(3) if lvl >= 0.5 else []):
                    wA = load_ws(8 + q_, wsi[0]); wsi[0] += 1
                    wB = load_ws(11 + q_, wsi[0]); wsi[0] += 1
                    proj_fm(hTc, (wA[:, :, 0:96], wA), 96, qiT[:, q_, :], qiT, (wB[:, :, 0:96], wB), tabs["cos32"], tabs["sin32"], rt2)
                for g_ in (range(4) if lvl >= 0.75 else []):
                    wA = load_ws(14 + g_, wsi[0]); wsi[0] += 1
                    wB = load_ws(18 + g_, wsi[0]); wsi[0] += 1
                    pa = bank(); pb = bank()
                    for k in range(8):
                        K.mm(pa[:, 0:512], wA[:, k, :], hTc[:, k, :], k == 0, k == 7, [wA, hTc], [pa])
                    for k in range(8):
                        K.mm(pb[:, 0:512], wB[:, k, :], hTc[:, k, :], k == 0, k == 7, [wB, hTc], [pb])
                    K.cp("scalar", qnT[:, g_, :], pa[:, 0:512], [pa], [qnT])
                    t1, t2 = rt2
                    K.tt("vector", t1[:, :], pa[:, 0:512], tabs["cos64"][:, :], ALU.mult, [pa, tabs["cos64"]], [t1])
                    K.tt("vector", t2[:, :], pb[:, 0:512], tabs["sin64"][:, :], ALU.mult, [pb, tabs["sin64"]], [t2])
                    K.tt("gpsimd", qrT[:, g_, :], t1[:, :], t2[:, :], ALU.add, [t1, t2], [qrT])
                if lvl >= 0.9:
                    K.dma("sync", wG[:], wq_d[38].rearrange("p (c m) -> p c m", c=8), [wq_tok], [wG])
                    proj_fm(hTc, (wG[:, :, 0:32], wG), 32, gT[:, :], gT, func=ACT.Sigmoid)
                K.dump(f"qaT{c}", qaT[:].rearrange("p a b -> p (a b)"), [128, 2048], BF16, [qaT.k])
                K.dump(f"qiT{c}", qiT[:].rearrange("p a b -> p (a b)"), [96, 1536], BF16, [qiT.k])
                K.dump(f"qrT{c}", qrT[:].rearrange("p a b -> p (a b)"), [128, 2048], BF16, [qrT.k])
                K.dump(f"gT{c}", gT[:], [32, 512], BF16, [gT.k])

                NIT = 14
                tiles_c = ([q for q in qt_list if q // 4 == c] if lvl >= 2 else [])

                def pre_a(qt):
                    t0 = qt * 128
                    tl = (qt % 4) * 128
                    tsl = slice(tl, tl + 128)
                    n = t0 + 128
                    wi_ = wi_sb2[qt % 2]
                    smb = smb2[qt % 2]
                    pw_ = bank()
                    for k in range(8):
                        K.mm(pw_[:, 0:8], hTc[:, k, tsl], wG[:, k, 24:32], k == 0, k == 7, [hTc, wG], [pw_])
                    K.cp("vector", wi_[:], pw_[:, 0:8], [pw_], [wi_])
                    nsc = (n + 511) // 512
                    ri = 0
                    for sc_i in range(nsc):
                        c0 = sc_i * 512
                        ncol = min(512, n - c0)
                        for h in range(8):
                            q_, r_ = h // 3, h % 3
                            pi_ = bank()
                            K.mm(pi_[:, 0:ncol], qiT[32 * r_:32 * r_ + 32, q_, tsl], kiT3[32 * r_:32 * r_ + 32, c0:c0 + ncol],
                                 True, True, [qiT, kiT3], [pi_])
                            Rb = (R0, R1)[ri % 2]; ri += 1
                            K.act(Rb[:, 0:ncol], pi_[:, 0:ncol], ACT.Relu, [pi_], [Rb])
                            if h == 0:
                                K.ts("vector", idx[:, c0:c0 + ncol], Rb[:, 0:ncol], wi_[:, 0:1], None, ALU.mult, None, [Rb, wi_], [idx])
                            else:
                                K.stt("vector", idx[:, c0:c0 + ncol], Rb[:, 0:ncol], wi_[:, h:h + 1], idx[:, c0:c0 + ncol],
                                      ALU.mult, ALU.add, [Rb, wi_, idx], [idx])
                    P.op("vector", lambda e, n=n: e.tensor_reduce(out=smb[:, 0:1], in_=idx[:, 0:n], axis=AX.X, op=ALU.max), K._tk([idx]), K._tk([smb]))
                    P.op("vector", lambda e, n=n: e.tensor_reduce(out=smb[:, 1:2], in_=idx[:, 0:n], axis=AX.X, op=ALU.min), K._tk([idx]), K._tk([smb]))
                    K.asel(idx[:, t0:t0 + 128], idx[:, t0:t0 + 128], [[-1, 128]], ALU.is_ge, -1e30, 0, 1, [idx], [idx])
                    K.ts("vector", smb[:, 2:3], smb[:, 1:2], -1.0, None, ALU.add, None, [smb], [smb])
                    K.stt("vector", smb[:, 3:4], smb[:, 0:1], 1.0, smb[:, 2:3], ALU.add, ALU.subtract, [smb], [smb])
                    K.memset("vector", smb[:, 8:8 + NIT], 0.0, [smb])

                def bis_step(qt, it):
                    n = qt * 128 + 128
                    smb = smb2[qt % 2]
                    f = 2.0 ** -(it + 1)
                    K.stt("vector", smb[:, 4:5], smb[:, 3:4], f, smb[:, 2:3], ALU.mult, ALU.add, [smb], [smb])
                    K.ts("vector", mask[:, 0:n], idx[:, 0:n], smb[:, 4:5], 0.0, ALU.is_ge, ALU.add, [idx, smb, mask], [mask, smb],
                         accum_out=smb[:, 8 + it:9 + it])
                    K.ts("vector", smb[:, 5:6], smb[:, 8 + it:9 + it], 256.0, f, ALU.is_ge, ALU.mult, [smb], [smb])
                    K.stt("vector", smb[:, 2:3], smb[:, 3:4], smb[:, 5:6], smb[:, 2:3], ALU.mult, ALU.add, [smb], [smb])

                def pre_c(qt):
                    n = qt * 128 + 128
                    smb = smb2[qt % 2]
                    K.ts("vector", mask[:, 0:n], idx[:, 0:n], smb[:, 2:3], None, ALU.is_ge, None, [idx, smb], [mask])
                    K.dump(f"mask{qt}", mask[:, 0:n], [128, n], BF16, [mask.k])
                    if lvl < 3:
                        return
                    for b0 in range(0, qt + 1, 8):
                        nb = min(8, qt + 1 - b0)
                        pm_ = bank()
                        for i in range(nb):
                            si = b0 + i
                            K.tr(bfv(pm_)[:, i * 128:(i + 1) * 128], mask[:, si * 128:(si + 1) * 128], ident[:], [mask, ident], [pm_])
                        K.cp("scalar", maskT[:, b0:b0 + nb, :], bfv(pm_)[:, 0:nb * 128].rearrange("p (a b) -> p a b", b=128), [pm_], [maskT])

                if tiles_c:
                    pre_a(tiles_c[0])
                    for it in range(NIT):
                        bis_step(tiles_c[0], it)
                    pre_c(tiles_c[0])
                for qj, qt in enumerate(tiles_c):
                    t0 = qt * 128
                    tl = (qt % 4) * 128
                    tsl = slice(tl, tl + 128)
                    n = t0 + 128
                    nxt = tiles_c[qj + 1] if qj + 1 < len(tiles_c) else None
                    if lvl < 3:
                        if nxt is not None:
                            pre_a(nxt)
                            for it in range(NIT):
                                bis_step(nxt, it)
                            pre_c(nxt)
                        continue
                    steps_left = list(range(NIT)) if nxt is not None else []
                    if nxt is not None:
                        pre_a(nxt)

                    def bis_hook():
                        if steps_left:
                            bis_step(nxt, steps_left.pop(0))

                    def dsa_mm(un):
                        si, u = un
                        ssl = slice(si * 128, (si + 1) * 128)
                        lo = 64 * u
                        ps_ = bank()
                        K.mm(ps_[:, 0:512].rearrange("p (g t) -> p g t", g=4), kaT2[lo:lo + 64, ssl], qaT[lo:lo + 64, :, tsl],
                             True, True, [kaT2, qaT], [ps_])
                        return ps_

                    def dsa_post(un, ps_):
                        si, u = un
                        E_, Pm_ = Enext()
                        K.act(E_[:, :], ps_[:, 0:512], ACT.Exp, [ps_], [E_], scale=SC)
                        K.tt("vector", Pm_[:, :].rearrange("p (g t) -> p g t", g=4), E_[:, :].rearrange("p (g t) -> p g t", g=4),
                             bc(maskT[:, si, :].unsqueeze(1), [128, 4, 128]), ALU.mult, [E_, maskT], [Pm_])
                        return Pm_

                    def dsa_qk(un):
                        return dsa_post(un, dsa_mm(un))
                    dsa_qk.mm = dsa_mm
                    dsa_qk.post = dsa_post

                    def dsa_pv(un, Pm_):
                        si, u = un
                        lo = 64 * u
                        K.mm(A_banks[u][:, 0:512], va3[:, si, lo:lo + 128], Pm_[:, :], si == 0, si == qt, [va3, Pm_], [A_banks[u]])

                    pipe([(si, u) for si in range(qt + 1) for u in range(2)], dsa_qk, dsa_pv, hook=bis_hook)
                    for u in range(2):
                        lo = 64 * u; lr = 64 * (1 - u)
                        K.recip(rs[lo:lo + 64, :], A_banks[u][lr:lr + 64, 0:512], [A_banks[u]], [rs])
                        K.tt("vector", oaTc[lo:lo + 64, :, tsl], A_banks[u][lo:lo + 64, 0:512].rearrange("p (g t) -> p g t", g=4),
                             rs[lo:lo + 64, :].rearrange("p (g t) -> p g t", g=4), ALU.mult, [A_banks[u], rs], [oaTc])
                    while steps_left:
                        bis_step(nxt, steps_left.pop(0))
                    if nxt is not None:
                        pre_c(nxt)
                    if lvl < 4:
                        continue
                    for k in range(2):
                        lo = 64 * k
                        for gp in range(2):
                            ps_ = bank()
                            for jj in range(2):
                                g_ = 2 * gp + jj
                                K.mm(ps_[:, jj * 256:(jj + 1) * 256], qnT[lo:lo + 64, g_, tsl], kcT2[lo:lo + 64, 0:256], True, True, [qnT, kcT2], [ps_])
                            h0 = 4 * k + 2 * gp
                            K.act(Ecmp[:, h0:h0 + 2, :], ps_[:, 0:512].rearrange("p (a n) -> p a n", a=2), ACT.Exp, [ps_], [Ecmp], scale=SC)
                    K.asel(Ecmp[:], Ecmp[:], [[0, 8], [-16, 256]], ALU.is_ge, 0.0, t0 - 31, 1, [Ecmp], [Ecmp])
                    P.op("vector", lambda e: e.tensor_reduce(out=sm[:, 40:48], in_=Ecmp[:], axis=AX.X, op=ALU.add), K._tk([Ecmp]), K._tk([sm]))
                    K.ts("vector", sm[:, 40:48], sm[:, 40:48], 1e-30, None, ALU.add, None, [sm], [sm])
                    K.recip(sm[:, 48:56], sm[:, 40:48], [sm], [sm])
                    K.tt("vector", Ecmp[:], Ecmp[:], bc(sm[:, 48:56].unsqueeze(2), [128, 8, 256]), ALU.mult, [Ecmp, sm], [Ecmp])
                    K.cp("gpsimd", p_bf[:], Ecmp[:], [Ecmp], [p_bf])
                    P.op("vector", lambda e: e.tensor_reduce(out=P4[:], in_=Ecmp[:].rearrange("p (k g) n -> p k n g", k=2), axis=AX.X, op=ALU.add),
                         K._tk([Ecmp]), K._tk([P4]))
                    P.op("vector", lambda e: e.tensor_reduce(out=imp[:], in_=P4[:].rearrange("p k (j i) -> p k j i", i=4), axis=AX.X, op=ALU.add),
                         K._tk([P4]), K._tk([imp]))
                    K.tt("vector", imp[:, :, 1:64], imp[:, :, 1:64], P4[:, :, 3:252:4], ALU.add, [imp, P4], [imp])
                    K.dma("sync", selb[0:64, :], c_selb_d[2 * qt:2 * qt + 1, :].partition_broadcast(64), [], [selb])
                    K.dma("sync", selb[64:128, :], c_selb_d[2 * qt + 1:2 * qt + 2, :].partition_broadcast(64), [], [selb])
                    K.tt("vector", scs[:], imp[:], bc(selb[:, :].unsqueeze(1), [128, 2, 64]), ALU.add, [imp, selb], [scs])
                    for k in range(2):
                        P.op("vector", lambda e, k=k: e.max(out=sm[:, 16:24], in_=scs[:, k, :]), K._tk([scs]), K._tk([sm]))
                        P.op("vector", lambda e, k=k: e.match_replace(out=sc2[:], in_to_replace=sm[:, 16:24], in_values=scs[:, k, :], imm_value=-1e9),
                             K._tk([scs, sm]), K._tk([sc2]))
                        P.op("vector", lambda e: e.max(out=sm[:, 24:32], in_=sc2[:]), K._tk([sc2]), K._tk([sm]))
                        K.ts("vector", bm[:, k, :], scs[:, k, :], sm[:, 31:32], None, ALU.is_ge, None, [scs, sm], [bm])
                    K.dump(f"bm{qt}", bm[:].rearrange("p a b -> p (a b)"), [128, 128], BF16, [bm.k])
                    pb_ = bank()
                    for k in range(2):
                        K.tr(bfv(pb_)[0:64, k * 128:(k + 1) * 128], bm[:, k, :], ident[:], [bm, ident], [pb_])
                    K.cp("scalar", bmT[:], bfv(pb_)[0:64, 0:256].rearrange("p (k t) -> p k t", k=2), [pb_], [bmT])
                    for ch in range(2):
                        pp_ = bank()
                        for h in range(8):
                            K.tr(bfv(pp_)[:, h * 128:(h + 1) * 128], p_bf[:, h, ch * 128:(ch + 1) * 128], ident[:], [p_bf, ident], [pp_])
                        K.cp("scalar" if ch == 0 else "vector", pT[:, ch, :, :], bfv(pp_).rearrange("p (h t) -> p h t", h=8), [pp_], [pT])
                    for k in range(2):
                        for ch in range(2):
                            K.mm(B_banks[k][:, 0:512].rearrange("p (g t) -> p g t", g=4), vctm[:, ch, :], pT[:, ch, 4 * k:4 * k + 4, :],
                                 ch == 0, ch == 1, [vctm, pT], [B_banks[k]])
                    def gate_bcast(cidx, k):
                        pg_ = bank()
                        K.mm(pg_[:, 0:512].rearrange("p (g t) -> p g t", g=4), ones24[:, :], Dg[:, 4 * k:4 * k + 4, :], True, True, [ones24, Dg], [pg_])
                        gb_ = gBs[k]
                        K.cp("scalar", gb_[64 * k:64 * k + 64, :], pg_[64 * k:64 * k + 64, 0:512], [pg_], [gb_])
                        return gb_

                    def make_Dg(cidx):
                        K.tt("vector", Dg[:], bc(gT[0:24, tsl].unsqueeze(1), [24, 8, 128]),
                             bc(eye24[:, cidx * 8:cidx * 8 + 8].unsqueeze(2), [24, 8, 128]), ALU.mult, [gT, eye24], [Dg])

                    make_Dg(0)
                    for k in range(2):
                        lo = 64 * k
                        gb_ = gate_bcast(0, k)
                        K.tt("vector", ob[lo:lo + 64, :], B_banks[k][lo:lo + 64, 0:512], gb_[lo:lo + 64, :], ALU.mult, [B_banks[k], gb_], [ob])
                    if lvl < 5:
                        continue
                    mes = {}

                    def slc_mm(un):
                        si, k = un
                        ssl = slice(si * 128, (si + 1) * 128)
                        if k == 0:
                            pm_ = bank()
                            K.mm(pm_[:, 0:256].rearrange("p (k t) -> p k t", k=2), Esel[:, si, :], bmT[:, :, :], True, True, [Esel, bmT], [pm_])
                            me = mexp[si % 4]
                            K.cp("scalar", me[:], pm_[:, 0:256].rearrange("p (k t) -> p k t", k=2), [pm_], [me])
                            if si == qt:
                                K.tt("gpsimd", me[:], me[:], bc(diagT[:, :].unsqueeze(1), [128, 2, 128]), ALU.mult, [me, diagT], [me])
                            mes[si] = me
                        lo = 64 * k
                        ps_ = bank()
                        K.mm(ps_[:, 0:512].rearrange("p (g t) -> p g t", g=4), ksT[lo:lo + 64, ssl], qrT[lo:lo + 64, :, tsl], True, True, [ksT, qrT], [ps_])
                        return ps_

                    def slc_post(un, ps_):
                        si, k = un
                        me = mes[si]
                        E_, Pm_ = Enext()
                        K.act(E_[:, :], ps_[:, 0:512], ACT.Exp, [ps_], [E_], scale=SC)
                        K.tt("vector", Pm_[:, :].rearrange("p (g t) -> p g t", g=4), E_[:, :].rearrange("p (g t) -> p g t", g=4),
                             bc(me[:, k, :].unsqueeze(1), [128, 4, 128]), ALU.mult, [E_, me], [Pm_])
                        return Pm_

                    def slc_qk(un):
                        return slc_post(un, slc_mm(un))
                    slc_qk.mm = slc_mm
                    slc_qk.post = slc_post

                    def slc_pv(un, Pm_):
                        si, k = un
                        lo = 64 * k
                        K.mm(A_banks[k][:, 0:512], vs3[:, si, lo:lo + 128], Pm_[:, :], si == 0, si == qt, [vs3, Pm_], [A_banks[k]])

                    pipe([(si, k) for si in range(qt + 1) for k in range(2)], slc_qk, slc_pv)

                    def fin(acc, cidx, last):
                        make_Dg(cidx)
                        for k in range(2):
                            lo = 64 * k; lr = 64 * (1 - k)
                            gb_ = gate_bcast(cidx, k)
                            K.recip(rs[lo:lo + 64, :], acc[k][lr:lr + 64, 0:512], [acc[k]], [rs])
                            K.tt("gpsimd", rs[lo:lo + 64, :], rs[lo:lo + 64, :], gb_[lo:lo + 64, :], ALU.mult, [rs, gb_], [rs])
                            tmp = R1
                            K.tt("vector", tmp[lo:lo + 64, :], acc[k][lo:lo + 64, 0:512], rs[lo:lo + 64, :], ALU.mult, [acc[k], rs], [tmp])
                            if not last:
                                K.tt("gpsimd", ob[lo:lo + 64, :], ob[lo:lo + 64, :], tmp[lo:lo + 64, :], ALU.add, [ob, tmp], [ob])
                            else:
                                K.tt("gpsimd", obTc[lo:lo + 64, :, tsl], ob[lo:lo + 64, :].rearrange("p (g t) -> p g t", g=4),
                                     tmp[lo:lo + 64, :].rearrange("p (g t) -> p g t", g=4), ALU.add, [ob, tmp], [obTc])

                    fin(A_banks, 1, False)
                    if lvl < 6:
                        continue
                    s_lo = max(0, qt - 4)

                    def win_mm(un):
                        si, k = un
                        ssl = slice(si * 128, (si + 1) * 128)
                        lo = 64 * k
                        ps_ = bank()
                        K.mm(ps_[:, 0:512].rearrange("p (g t) -> p g t", g=4), kwT[lo:lo + 64, ssl], qrT[lo:lo + 64, :, tsl], True, True, [kwT, qrT], [ps_])
                        return ps_

                    def win_post(un, ps_):
                        si, k = un
                        E_, Pm_ = Enext()
                        K.act(E_[:, :], ps_[:, 0:512], ACT.Exp, [ps_], [E_], scale=SC)
                        mk = diagT if si == qt else (antiT if si == qt - 4 else None)
                        src = E_
                        if mk is not None:
                            K.tt("vector", Pm_[:, :].rearrange("p (g t) -> p g t", g=4), E_[:, :].rearrange("p (g t) -> p g t", g=4),
                                 bc(mk[:, :].unsqueeze(1), [128, 4, 128]), ALU.mult, [E_, mk], [Pm_])
                            src = Pm_
                        return src

                    def win_qk(un):
                        return win_post(un, win_mm(un))
                    win_qk.mm = win_mm
                    win_qk.post = win_post

                    def win_pv(un, src):
                        si, k = un
                        lo = 64 * k
                        K.mm(B_banks[k][:, 0:512], vw3[:, si, lo:lo + 128], src[:, :], si == s_lo, si == qt, [vw3, src], [B_banks[k]])

                    pipe([(si, k) for si in range(s_lo, qt + 1) for k in range(2)], win_qk, win_pv)
                    fin(B_banks, 2, True)

                for qt in [q for q in qt_list if q // 4 == c]:
                    tl = (qt % 4) * 128
                    for nm_, bt_ in (("oaT", oaTc), ("obT", obTc)):
                        if f"{nm_}{qt}" in K.dbg:
                            d_ = nc.dram_tensor(f"dbg_{nm_}{qt}", [128, 4, 128], BF16, kind="ExternalOutput").ap()
                            K.dma("sync", d_, bt_[:, :, tl:tl + 128], [bt_], [])
                if lvl < 7:
                    continue
                for u in range(2):
                    K.dma("gpsimd", wbra[64 * u:64 * u + 64, :, :], w_br_a_d[256 * u:256 * (u + 1), :].rearrange("(g d) f -> d g f", d=64), [], [wbra])
                    K.dma("gpsimd", wbrb[64 * u:64 * u + 64, :, :], w_br_b_d[256 * u:256 * (u + 1), :].rearrange("(g d) f -> d g f", d=64), [], [wbrb])
                K.dma("gpsimd", wout[:], w_out_d.rearrange("(c p) f -> p c f", p=128), [], [wout])
                for fc in range(8):
                    fsl = slice(fc * 128, (fc + 1) * 128)
                    outs_ = []
                    for br, (gbase, wbr, oT) in enumerate(((22, wbra, oaTc), (30, wbrb, obTc))):
                        wg_ = load_ws(gbase + fc, wsi[0]); wsi[0] += 1
                        pg_ = bank()
                        for k in range(8):
                            K.mm(pg_[:, 0:512], wg_[:, k, :], hTc[:, k, :], k == 0, k == 7, [wg_, hTc], [pg_])
                        E_, Pm_ = Enext()
                        K.act(E_[:, :], pg_[:, 0:512], ACT.Sigmoid, [pg_], [E_])
                        pbr = bank()
                        for g_ in range(4):
                            K.mm(pbr[:, 0:512], wbr[:, g_, fsl], oT[:, g_, :], g_ == 0, g_ == 3, [wbr, oT], [pbr])
                        K.tt("vector", Pm_[:, :], pbr[:, 0:512], E_[:, :], ALU.mult, [pbr, E_], [Pm_])
                        outs_.append(Pm_)
                    K.tt("gpsimd", mergedT[:, fc, :], outs_[0][:, :], outs_[1][:, :], ALU.add, [outs_[0], outs_[1]], [mergedT])
                K.dump(f"mergedT{c}", mergedT[:].rearrange("p a b -> p (a b)"), [128, 4096], BF16, [mergedT.k])
                for j in range(4):
                    tt_ = 4 * c + j
                    xt = xb[0]
                    K.dma("sync", xt[:], x_d[tt_ * 128:(tt_ + 1) * 128, :], [], [xt])
                    for half in range(2):
                        po_ = bank()
                        for fc in range(8):
                            K.mm(po_[:, 0:512], mergedT[:, fc, j * 128:(j + 1) * 128], wout[:, fc, half * 512:(half + 1) * 512],
                                 fc == 0, fc == 7, [mergedT, wout], [po_])
                        K.tt("vector", xt[:, half * 512:(half + 1) * 512], po_[:, 0:512], xt[:, half * 512:(half + 1) * 512], ALU.add, [po_, xt], [xt])
                    K.dma("sync", x1_d[tt_ * 128:(tt_ + 1) * 128, :], xt[:], [xt], [x1_tok])
                    K.dump(f"x1_{tt_}", xt[:], [128, D], F32, [xt.k])
            P.emit()
        if stop_after == "C":
            P.emit(final=True)
            att.close()
            return nc, K
        att.close()
        if moe_from_x:
            x1_d = x_d
        with ExitStack() as pm:
            HT = 16
            identf = K.sb(pm, [128, 128])
            gffn = K.sb(pm, [128, 8])
            gfin = K.sb(pm, [128, D])
            wr = K.sb(pm, [128, 8, 36])
            rb = K.sb(pm, [128, 36])
            h2T = K.sb(pm, [128, 8, HT * 128], BF16)
            yacc = K.sb(pm, [128, HT, D])
            wgt = K.sb(pm, [128, HT, 32])
            wgu = [K.sb(pm, [128, 8, 512], BF16) for _ in range(2)]
            wdn = [K.sb(pm, [128, 2, D], BF16) for _ in range(2)]
            aT = [K.sb(pm, [128, 2, 512], BF16) for _ in range(2)]
            sg = [K.sb(pm, [128, 512], BF16) for _ in range(2)]
            xm = [K.sb(pm, [128, D]) for _ in range(2)]
            xn = K.sb(pm, [128, D])
            h2f = K.sb(pm, [128, 8, 128])
            sq2 = K.sb(pm, [128, D], BF16)
            s2 = K.sb(pm, [128, 16])
            lg = K.sb(pm, [128, 36])
            me = K.sb(pm, [128, 32])
            ex = K.sb(pm, [128, 32])
            m8 = K.sb(pm, [128, 8])
            K.memset("gpsimd", identf[:], 1.0, [identf])
            K.asel(identf[:], identf[:], [[-1, 128]], ALU.is_equal, 0.0, 0, 1, [identf], [identf])
            K.dma("sync", gffn[:], norm_ffn_d, [], [gffn])
            K.dma("sync", gfin[:], norm_final_d.partition_broadcast(128), [], [gfin])
            K.dma("sync", wr[:, :, 0:4], w_group_d.rearrange("(c p) n -> p c n", p=128), [], [wr])
            K.dma("sync", wr[:, :, 4:36], w_expert_d.rearrange("(c p) n -> p c n", p=128), [], [wr])
            K.dma("sync", rb[:, 0:4], b_group_d.partition_broadcast(128), [], [rb])
            K.dma("sync", rb[:, 4:36], b_expert_d.partition_broadcast(128), [], [rb])
            BIG = 30000.0
            xi = [0]
            wl = [0]
            for half in range(2):
                for j in range(HT):
                    tt_ = half * HT + j
                    xt = xm[xi[0] % 2]; xi[0] += 1
                    K.dma("sync", xt[:], x1_d[tt_ * 128:(tt_ + 1) * 128, :], [x1_tok], [xt])
                    K.memset("vector", s2[:, 0:1], 0.0, [s2])
                    K.act(sq2[:], xt[:], ACT.Square, [xt, s2], [sq2, s2], accum_out=s2[:, 0:1])
                    K.act(s2[:, 1:2], s2[:, 0:1], ACT.Sqrt, [s2, cpi], [s2], scale=1.0 / D, bias=cpi[:, 1:2])
                    K.recip(s2[:, 2:3], s2[:, 1:2], [s2], [s2])
                    K.ts("vector", xn[:], xt[:], s2[:, 2:3], None, ALU.mult, None, [xt, s2], [xn])
                    for hb in range(2):
                        pb = bank()
                        for k in range(4):
                            kk = hb * 4 + k
                            K.tr(pb[:, k * 128:(k + 1) * 128], xn[:, kk * 128:(kk + 1) * 128], identf[:], [xn, identf], [pb])
                        K.tt("vector", h2f[:, hb * 4:hb * 4 + 4, :], pb[:, 0:512].rearrange("p (k t) -> p k t", k=4),
                             bc(gffn[:, hb * 4:hb * 4 + 4].unsqueeze(2), [128, 4, 128]), ALU.mult, [pb, gffn], [h2f])
                    K.cp("scalar", h2T[:, :, j * 128:(j + 1) * 128], h2f[:], [h2f], [h2T])
                    pr = bank()
                    for k in range(8):
                        K.mm(pr[:, 0:36], h2f[:, k, :], wr[:, k, :], k == 0, k == 7, [h2f, wr], [pr])
                    K.tt("vector", lg[:], pr[:, 0:36], rb[:], ALU.add, [pr, rb], [lg])
                    P.op("vector", lambda e: e.tensor_reduce(out=s2[:, 4:5], in_=lg[:, 0:4], axis=AX.X, op=ALU.max), K._tk([lg]), K._tk([s2]))
                    K.ts("vector", s2[:, 5:6], s2[:, 4:5], -1.0, None, ALU.mult, None, [s2], [s2])
                    K.memset("vector", s2[:, 6:7], 0.0, [s2])
                    K.act(ex[:, 0:4], lg[:, 0:4], ACT.Exp, [lg, s2], [ex, s2], bias=s2[:, 5:6], accum_out=s2[:, 6:7])
                    K.recip(s2[:, 7:8], s2[:, 6:7], [s2], [s2])
                    K.ts("vector", ex[:, 4:8], lg[:, 0:4], s2[:, 4:5], BIG, ALU.is_ge, ALU.mult, [lg, s2], [ex])
                    K.ts("vector", ex[:, 4:8], ex[:, 4:8], -BIG, None, ALU.add, None, [ex], [ex])
                    K.tt("vector", me[:].rearrange("p (g i) -> p g i", g=4), lg[:, 4:36].rearrange("p (g i) -> p g i", g=4),
                         bc(ex[:, 4:8].unsqueeze(2), [128, 4, 8]), ALU.add, [lg, ex], [me])
                    P.op("vector", lambda e: e.max(out=m8[:], in_=me[:]), K._tk([me]), K._tk([m8]))
                    K.ts("vector", s2[:, 8:9], m8[:, 0:1], -1.0, None, ALU.mult, None, [m8], [s2])
                    K.act(ex[:], me[:], ACT.Exp, [me, s2], [ex], bias=s2[:, 8:9])
                    K.act(s2[:, 9:10], m8[:, 1:2], ACT.Exp, [m8, s2], [s2], bias=s2[:, 8:9])
                    K.ts("vector", s2[:, 9:10], s2[:, 9:10], 1.0, None, ALU.add, None, [s2], [s2])
                    K.recip(s2[:, 10:11], s2[:, 9:10], [s2], [s2])
                    K.tt("vector", s2[:, 11:12], s2[:, 10:11], s2[:, 7:8], ALU.mult, [s2], [s2])
                    K.ts("vector", me[:], me[:], m8[:, 1:2], None, ALU.is_ge, None, [me, m8], [me])
                    K.tt("vector", ex[:], ex[:], me[:], ALU.mult, [ex, me], [ex])
                    K.ts("vector", wgt[:, j, :], ex[:], s2[:, 11:12], None, ALU.mult, None, [ex, s2], [wgt])
                K.dump(f"wgt{half}", wgt[:].rearrange("p a b -> p (a b)"), [128, HT * 32], F32, [wgt.k])
                for e_ in range(moe_experts):
                    wg_ = wgu[wl[0] % 2]; wd_ = wdn[wl[0] % 2]; wl[0] += 1
                    K.dma("gpsimd", wg_[:], w_gu_d[e_].rearrange("(c p) f -> p c f", p=128), [], [wg_])
                    K.dma("gpsimd", wd_[:], w_dn_d[e_].rearrange("(c p) f -> p c f", p=128), [], [wd_])
                    for tch in range(HT // 4):
                        a_ = aT[tch % 2]
                        csl = slice(tch * 512, (tch + 1) * 512)
                        for fo in range(2):
                            pg_ = bank(); pu_ = bank()
                            for k in range(8):
                                K.mm(pg_[:, 0:512], wg_[:, k, fo * 128:(fo + 1) * 128], h2T[:, k, csl], k == 0, k == 7, [wg_, h2T], [pg_])
                            for k in range(8):
                                K.mm(pu_[:, 0:512], wg_[:, k, 256 + fo * 128:256 + (fo + 1) * 128], h2T[:, k, csl], k == 0, k == 7, [wg_, h2T], [pu_])
                            s_ = sg[fo]
                            K.act(s_[:, :], pg_[:, 0:512], ACT.Silu, [pg_], [s_])
                            K.tt("vector", a_[:, fo, :], pu_[:, 0:512], s_[:, :], ALU.mult, [pu_, s_], [a_])
                        for tj in range(4):
                            tile_ = tch * 4 + tj
                            for hf in range(2):
                                po_ = bank((4, 5, 6, 7))
                                for fo in range(2):
                                    K.mm(po_[:, 0:512], a_[:, fo, tj * 128:(tj + 1) * 128], wd_[:, fo, hf * 512:(hf + 1) * 512],
                                         fo == 0, fo == 1, [a_, wd_], [po_])
                                ysl = yacc[:, tile_, hf * 512:(hf + 1) * 512]
                                if e_ == 0:
                                    K.ts("vector", ysl, po_[:, 0:512], wgt[:, tile_, e_:e_ + 1], None, ALU.mult, None, [po_, wgt], [yacc])
                                else:
                                    K.stt("vector", ysl, po_[:, 0:512], wgt[:, tile_, e_:e_ + 1], ysl, ALU.mult, ALU.add, [po_, wgt, yacc], [yacc])
                for j in range(HT):
                    tt_ = half * HT + j
                    xt = xm[xi[0] % 2]; xi[0] += 1
                    K.dma("sync", xt[:], x1_d[tt_ * 128:(tt_ + 1) * 128, :], [x1_tok], [xt])
                    K.tt("gpsimd", xt[:], xt[:], yacc[:, j, :], ALU.add, [xt, yacc], [xt])
                    K.memset("vector", s2[:, 12:13], 0.0, [s2])
                    K.act(sq2[:], xt[:], ACT.Square, [xt, s2], [sq2, s2], accum_out=s2[:, 12:13])
                    K.act(s2[:, 13:14], s2[:, 12:13], ACT.Sqrt, [s2, cpi], [s2], scale=1.0 / D, bias=cpi[:, 1:2])
                    K.recip(s2[:, 14:15], s2[:, 13:14], [s2], [s2])
                    K.stt("vector", xn[:], xt[:], s2[:, 14:15], gfin[:], ALU.mult, ALU.mult, [xt, s2, gfin], [xn])
                    K.dma("sync", out_d[tt_ * 128:(tt_ + 1) * 128, :], xn[:], [xn], [])
            P.emit()
        P.emit(final=True)
    return nc, K


def _consts():
    p = np.arange(128)
    invf = np.stack([10000.0 ** (-(p % 32).astype(np.float32) / 32.0),
                     10000.0 ** (-(p % 16).astype(np.float32) / 16.0)], axis=1).astype(np.float32)
    selb = np.zeros((64, 64), np.float32)
    for c in range(64):
        for j in range(64):
            if j > c:
                selb[c, j] = -100.0
            elif j == 0 or j == c or j == c - 1:
                selb[c, j] = 100.0
    esel = np.zeros((64, 32, 128), np.float32)
    for i in range(32):
        for s_ in range(128):
            esel[2 * i + s_ // 64, i, s_] = 1.0
    return invf, selb, esel.reshape(64, 4096), np.eye(24, dtype=np.float32)


def make_in_map(inp, b):
    invf, selb, esel, eye = _consts()
    f = lambda a: np.ascontiguousarray(np.asarray(a))
    return {
        "x": f(inp["x"][b]), "positions": f(inp["positions"][b][None, :]),
        "norm_mix": f(np.asarray(inp["norm_mix"][0]).reshape(8, 128).T), "w_in": f(inp["w_in"][0]),
        "pe_k": f(inp["pe_k"][0]), "w1_k": f(inp["w1_k"][0]), "w2_k": f(inp["w2_k"][0]),
        "pe_v": f(inp["pe_v"][0]), "w1_v": f(inp["w1_v"][0]), "w2_v": f(inp["w2_v"][0]),
        "w_br_a": f(inp["w_br_a"][0]), "w_br_b": f(inp["w_br_b"][0]), "w_out": f(inp["w_out"][0]),
        "norm_ffn": f(np.asarray(inp["norm_ffn"][0]).reshape(8, 128).T),
        "w_group": f(inp["w_group"][0]), "b_group": f(inp["b_group"][0][None, :]),
        "w_expert": f(inp["w_expert"][0]), "b_expert": f(inp["b_expert"][0][None, :]),
        "w_gate_up": f(inp["w_gate_up"][0]), "w_down": f(inp["w_down"][0]),
        "norm_final": f(inp["norm_final"][None, :]),
        "c_invf": invf, "c_selb": selb, "c_esel": esel, "c_eye": eye,
    }


def kernel(**inputs):
    nc, K = build()
    in_maps = [make_in_map(inputs, b) for b in range(8)]
    res = run_bass_kernel_spmd(nc, in_maps, core_ids=list(range(8)))
    return np.stack([np.asarray(r["out"]) for r in res.results], axis=0).astype(np.float32)
```

```python
from contextlib import ExitStack
import numpy as np
import concourse.bass as bass
import concourse.mybir as mybir
from concourse.bass_utils import run_bass_kernel_spmd

F32 = mybir.dt.float32
BF16 = mybir.dt.bfloat16
I32 = mybir.dt.int32
ALU = mybir.AluOpType
ACT = mybir.ActivationFunctionType
AX = mybir.AxisListType

ENGS = ("tensor", "vector", "scalar", "gpsimd", "sync")

PIPE_DEPTH = 2
NEB = 4
PAIR = 1
L = 4096
D = 1024
NT = 32
NCH = 8
EPS = 1e-6
PI = float(np.pi)
TWO_PI = float(2 * np.pi)

QA, KA, VA, QI, KI, WI, QB = 0, 512, 576, 640, 896, 928, 936
KC, VC, KS, VS, KW, VW, GB, GA, GBT = 1448, 1576, 1704, 1832, 1960, 2088, 2216, 2240, 3264


class Tok:
    __slots__ = ("last_w", "readers")

    def __init__(self):
        self.last_w = None
        self.readers = []


class Op:
    __slots__ = ("eng", "fn", "deps", "is_dma", "sig", "signal", "idx", "prev")

    def __init__(self, eng, fn, is_dma):
        self.eng = eng
        self.fn = fn
        self.deps = set()
        self.is_dma = is_dma
        self.sig = None
        self.signal = False
        self.prev = None


class Prog:
    N_DMA_SEMS = 8

    def __init__(self, nc, ctx):
        self.nc = nc
        self.ops = []
        self.done = 0
        self.eng_sems = {e: ctx.enter_context(nc.semaphore(f"s_{e}")) for e in ENGS}
        self.dma_sems = {e: [ctx.enter_context(nc.semaphore(f"d_{e}_{i}")) for i in range(self.N_DMA_SEMS)]
                         for e in ENGS}
        self.eng_cnt = {e: 0 for e in ENGS}
        self.dma_rr = {e: 0 for e in ENGS}
        self.dma_cnt = {e: [0] * self.N_DMA_SEMS for e in ENGS}

    def _add(self, eng, fn, reads, writes, is_dma=False):
        op = Op(eng, fn, is_dma)
        op.idx = len(self.ops)
        for t in reads:
            if t.last_w is not None:
                op.deps.add(t.last_w)
        for t in writes:
            if t.last_w is not None:
                op.deps.add(t.last_w)
            op.deps.update(t.readers)
        op.deps.discard(op.idx)
        for t in reads:
            t.readers.append(op.idx)
        for t in writes:
            t.last_w = op.idx
            t.readers = []
        self.ops.append(op)
        return op

    def op(self, eng, fn, reads=(), writes=()):
        return self._add(eng, fn, list(reads), list(writes))

    def dma(self, eng, out, in_, reads=(), writes=(), **kw):
        return self._add(eng, lambda e: e.dma_start(out=out, in_=in_, **kw), list(reads), list(writes), True)

    def _sem(self, key):
        return self.eng_sems[key[1]] if key[0] == "e" else self.dma_sems[key[1]][key[2]]

    def emit(self, final=False):
        nc = self.nc
        ops = self.ops
        new = ops[self.done:]
        pre = []
        for e in ENGS:
            if self.eng_cnt[e] > 0:
                pre.append((("e", e), self.eng_cnt[e]))
            for k in range(self.N_DMA_SEMS):
                if self.dma_cnt[e][k] > 0:
                    pre.append((("d", e, k), self.dma_cnt[e][k]))
        for op in new:
            for d in op.deps:
                if d >= self.done:
                    ops[d].signal = True
        per_eng = {e: [] for e in ENGS}
        for op in new:
            per_eng[op.eng].append(op)
        for e in ENGS:
            for op in reversed(per_eng[e]):
                if not op.is_dma:
                    op.signal = True
                    break
        for op in new:
            if op.is_dma:
                k = self.dma_rr[op.eng]
                self.dma_rr[op.eng] = (k + 1) % self.N_DMA_SEMS
                prev = self.dma_cnt[op.eng][k]
                self.dma_cnt[op.eng][k] = prev + 16
                op.sig = (("d", op.eng, k), prev + 16)
                op.prev = (("d", op.eng, k), prev)
            elif op.signal:
                self.eng_cnt[op.eng] += 1
                op.sig = (("e", op.eng), self.eng_cnt[op.eng])
        finals = []
        if final:
            for e in ENGS:
                for k in range(self.N_DMA_SEMS):
                    if self.dma_cnt[e][k] > 0:
                        finals.append((("d", e, k), self.dma_cnt[e][k]))
        done = self.done

        def run_engine(ename, eobj):
            known = {}
            for key, val in pre:
                if key == ("e", ename):
                    continue
                eobj.wait_ge(self._sem(key), val)
                known[key] = val
            for op in per_eng[ename]:
                waits = {}
                for d in op.deps:
                    if d < done:
                        continue
                    dop = ops[d]
                    key, val = dop.sig
                    if ename == "tensor" and dop.eng == "tensor" and not dop.is_dma:
                        continue
                    if known.get(key, 0) >= val:
                        continue
                    waits[key] = max(waits.get(key, 0), val)
                if op.is_dma:
                    key, val = op.prev
                    if val > 0 and known.get(key, 0) < val:
                        waits[key] = max(waits.get(key, 0), val)
                for key, val in waits.items():
                    eobj.wait_ge(self._sem(key), val)
                    known[key] = val
                ins = op.fn(eobj)
                if op.sig is not None:
                    ins.then_inc(self._sem(op.sig[0]), 16 if op.is_dma else 1)
            if ename == "sync":
                for key, val in finals:
                    if known.get(key, 0) < val:
                        eobj.wait_ge(self._sem(key), val)

        with nc.Block() as block:
            @block.tensor
            def _(e):
                run_engine("tensor", e)

            @block.vector
            def _(e):
                run_engine("vector", e)

            @block.scalar
            def _(e):
                run_engine("scalar", e)

            @block.gpsimd
            def _(e):
                run_engine("gpsimd", e)

            @block.sync
            def _(e):
                run_engine("sync", e)
        self.done = len(ops)


class Buf:
    def __init__(self, t):
        self.t = t
        self.k = Tok()

    def __getitem__(self, key):
        return self.t[key]


class View:
    def __init__(self, ap, k):
        self.ap = ap
        self.k = k

    def __getitem__(self, key):
        return self.ap[key]


class KB:
    def __init__(self, nc, dbg=None):
        self.nc = nc
        self.n = 0
        self.dbg = dbg if dbg is not None else {}
        self.dbg_out = {}

    def sb(self, ctx, shape, dt=F32):
        self.n += 1
        return Buf(ctx.enter_context(self.nc.sbuf_tensor(f"sb{self.n}", list(shape), dt)))

    def ps(self, ctx, shape, dt=F32):
        self.n += 1
        b = Buf(ctx.enter_context(self.nc.psum_tensor(f"ps{self.n}", list(shape), dt)))
        b.is_psum = True
        return b

    def dump(self, name, ap, shape, dt, reads):
        if name not in self.dbg:
            return
        d = self.nc.dram_tensor("dbg_" + name, list(shape), dt, kind="ExternalOutput").ap()
        self.dbg_out[name] = d
        self.P.dma("sync", d, ap, reads=reads)

    @staticmethod
    def _tk(lst):
        return [b.k if hasattr(b, "k") else b for b in lst]

    @staticmethod
    def _rw(r, w):
        rr_, ww_ = [], list(w)
        for b in r:
            if getattr(b, "is_psum", False):
                if b not in ww_:
                    ww_.append(b)
            else:
                rr_.append(b)
        tk = lambda lst: [b.k if hasattr(b, "k") else b for b in lst]
        return tk(rr_), tk(ww_)

    def mm(self, out, lhsT, rhs, start, stop, r, w):
        self.P.op("tensor", lambda e: e.matmul(out, lhsT=lhsT, rhs=rhs, start=start, stop=stop), *self._rw(r, w))

    def tr(self, out, in_, ident, r, w):
        self.P.op("tensor", lambda e: e.transpose(out=out, in_=in_, identity=ident), *self._rw(r, w))

    def act(self, out, in_, func, r, w, **kw):
        self.P.op("scalar", lambda e: e.activation(out=out, in_=in_, func=func, **kw), *self._rw(r, w))

    def tt(self, eng, out, in0, in1, op, r, w):
        self.P.op(eng, lambda e: e.tensor_tensor(out=out, in0=in0, in1=in1, op=op), *self._rw(r, w))

    def ts(self, eng, out, in0, s1, s2, op0, op1, r, w, accum_out=None):
        if op1 is None:
            self.P.op(eng, lambda e: e.tensor_scalar(out=out, in0=in0, scalar1=s1, scalar2=None, op0=op0), *self._rw(r, w))
        elif accum_out is None:
            self.P.op(eng, lambda e: e.tensor_scalar(out=out, in0=in0, scalar1=s1, scalar2=s2, op0=op0, op1=op1), *self._rw(r, w))
        else:
            self.P.op(eng, lambda e: e.tensor_scalar(out=out, in0=in0, scalar1=s1, scalar2=s2, op0=op0, op1=op1, accum_out=accum_out), *self._rw(r, w))

    def stt(self, eng, out, in0, scalar, in1, op0, op1, r, w):
        self.P.op(eng, lambda e: e.scalar_tensor_tensor(out=out, in0=in0, scalar=scalar, in1=in1, op0=op0, op1=op1), *self._rw(r, w))

    def cp(self, eng, out, in_, r, w):
        if eng == "scalar":
            self.P.op(eng, lambda e: e.copy(out=out, in_=in_), *self._rw(r, w))
        else:
            self.P.op(eng, lambda e: e.tensor_copy(out=out, in_=in_), *self._rw(r, w))

    def memset(self, eng, ap, val, w):
        self.P.op(eng, lambda e: e.memset(ap, val), [], self._tk(w))

    def asel(self, out, in_, pattern, cmp, fill, base, cm, r, w):
        self.P.op("gpsimd", lambda e: e.affine_select(out=out, in_=in_, pattern=pattern, compare_op=cmp, fill=fill, base=base, channel_multiplier=cm), *self._rw(r, w))

    def recip(self, out, in_, r, w):
        self.P.op("vector", lambda e: e.reciprocal(out=out, in_=in_), *self._rw(r, w))

    def dma(self, eng, out, in_, r, w, **kw):
        r_, w_ = self._rw(r, w)
        self.P.dma(eng, out, in_, r_, w_, **kw)


def bc(ap, shape):
    return ap.to_broadcast(list(shape))


def build(dbg=None, qtiles=None, stop_after=None, moe_experts=32, lvl=99, moe_from_x=False, skip_att=False):
    nc = bass.Bass("TRN2", target_bir_lowering=False)
    K = KB(nc, dbg)

    def din(name, shape, dt=F32):
        return nc.dram_tensor(name, list(shape), dt, kind="ExternalInput").ap()

    x_d = din("x", [L, D])
    pos_d = din("positions", [1, L], I32)
    norm_mix_d = din("norm_mix", [128, 8])
    w_in_d = din("w_in", [D, 4288])
    pe_k_d = din("pe_k", [32, 64]); w1_k_d = din("w1_k", [2048, 128]); w2_k_d = din("w2_k", [128, 64])
    pe_v_d = din("pe_v", [32, 64]); w1_v_d = din("w1_v", [2048, 128]); w2_v_d = din("w2_v", [128, 64])
    w_br_a_d = din("w_br_a", [512, D]); w_br_b_d = din("w_br_b", [512, D]); w_out_d = din("w_out", [D, D])
    norm_ffn_d = din("norm_ffn", [128, 8])
    w_group_d = din("w_group", [D, 4]); b_group_d = din("b_group", [1, 4])
    w_expert_d = din("w_expert", [D, 32]); b_expert_d = din("b_expert", [1, 32])
    w_gu_d = din("w_gate_up", [32, D, 512]); w_dn_d = din("w_down", [32, 256, D])
    norm_final_d = din("norm_final", [1, D])
    c_invf_d = din("c_invf", [128, 2])
    c_selb_d = din("c_selb", [64, 64])
    c_esel_d = din("c_esel", [64, 4096])
    c_eye_d = din("c_eye", [24, 24])
    out_d = nc.dram_tensor("out", [L, D], F32, kind="ExternalOutput").ap()
    x1_d = nc.dram_tensor("x1s", [L, D], F32, kind="Internal").ap()
    NG_Q = 40
    wq_d = nc.dram_tensor("wq_bf", [NG_Q, 128, 1024], BF16, kind="Internal").ap()

    w_in_v = w_in_d.rearrange("(c p) n -> p c n", p=128)
    wq_tok = Tok()
    x1_tok = Tok()

    with ExitStack() as top:
        P = Prog(nc, top)
        K.P = P
        ident = K.sb(top, [128, 128], BF16)
        diagT = K.sb(top, [128, 128], BF16)
        antiT = K.sb(top, [128, 128], BF16)
        invf = K.sb(top, [128, 2])
        gmix = K.sb(top, [128, 8])
        cpi = K.sb(top, [128, 2])
        with ExitStack() as c0:
            tmpf = K.sb(c0, [128, 128])
            K.memset("gpsimd", tmpf[:], 1.0, [tmpf])
            K.asel(tmpf[:], tmpf[:], [[-1, 128]], ALU.is_equal, 0.0, 0, 1, [tmpf], [tmpf])
            K.cp("vector", ident[:], tmpf[:], [tmpf], [ident])
            tmp2 = K.sb(c0, [128, 128])
            K.memset("gpsimd", tmp2[:], 1.0, [tmp2])
            K.asel(tmp2[:], tmp2[:], [[1, 128]], ALU.is_ge, 0.0, 0, -1, [tmp2], [tmp2])
            K.cp("vector", diagT[:], tmp2[:], [tmp2], [diagT])
            tmp3 = K.sb(c0, [128, 128])
            K.memset("gpsimd", tmp3[:], 1.0, [tmp3])
            K.asel(tmp3[:], tmp3[:], [[-1, 128]], ALU.is_gt, 0.0, 0, 1, [tmp3], [tmp3])
            K.cp("vector", antiT[:], tmp3[:], [tmp3], [antiT])
            K.dma("sync", invf[:], c_invf_d, [], [invf])
            K.dma("sync", gmix[:], norm_mix_d, [], [gmix])
            K.memset("vector", cpi[:, 0:1], PI / 2, [cpi])
            K.memset("vector", cpi[:, 1:2], EPS, [cpi])
            P.emit()

        banks = [K.ps(top, [128, 512]) for _ in range(8)]
        rr = [0]

        def bank(pool=(0, 1, 2, 3)):
            b = banks[pool[rr[0] % len(pool)]]
            rr[0] += 1
            return b

        def bfv(b):
            return b.t[:].bitcast(BF16)

        def make_hT(c, hTc, xb, sq, hn, st, g):
            for j in range(4):
                tt_ = 4 * c + j
                xt = xb[j % len(xb)]
                K.dma("sync", xt[:], x_d[tt_ * 128:(tt_ + 1) * 128, :], [], [xt])
                K.memset("vector", st[:, 0:1], 0.0, [st])
                K.act(sq[:], xt[:], ACT.Square, [xt, st], [sq, st], accum_out=st[:, 0:1])
                K.act(st[:, 1:2], st[:, 0:1], ACT.Sqrt, [st, cpi], [st], scale=1.0 / D, bias=cpi[:, 1:2])
                K.recip(st[:, 2:3], st[:, 1:2], [st], [st])
                K.ts("vector", hn[:], xt[:], st[:, 2:3], None, ALU.mult, None, [xt, st], [hn])
                pb = bank()
                for k in range(8):
                    K.tr(bfv(pb)[:, k * 128:(k + 1) * 128], hn[:, k * 128:(k + 1) * 128], ident[:], [hn, ident], [pb])
                K.tt("vector", hTc[:, :, j * 128:(j + 1) * 128], bfv(pb).rearrange("p (k t) -> p k t", k=8),
                     bc(g[:, :].unsqueeze(2), [128, 8, 128]), ALU.mult, [pb, g], [hTc])

        def make_rope(c, posi, posf, tq, tabs):
            K.dma("sync", posi[:], pos_d[:, c * 512:(c + 1) * 512].partition_broadcast(128), [], [posi])
            K.cp("vector", posf[:], posi[:], [posi], [posf])
            for col, (cn, sn) in enumerate((("cos64", "sin64"), ("cos32", "sin32"))):
                ang = tq[0]; kf = tq[1]; ki = tq[2]
                K.ts("vector", ang[:], posf[:], invf[:, col:col + 1], None, ALU.mult, None, [posf, invf], [ang])
                K.ts("vector", ki[:], ang[:], 1.0 / TWO_PI, None, ALU.mult, None, [ang], [ki])
                K.cp("vector", kf[:], ki[:], [ki], [kf])
                K.stt("vector", ang[:], kf[:], -TWO_PI, ang[:], ALU.mult, ALU.add, [kf, ang], [ang])
                K.ts("vector", kf[:], ang[:], PI, -TWO_PI, ALU.is_gt, ALU.mult, [ang], [kf])
                K.tt("vector", ang[:], ang[:], kf[:], ALU.add, [ang, kf], [ang])
                K.ts("vector", kf[:], ang[:], -PI, TWO_PI, ALU.is_lt, ALU.mult, [ang], [kf])
                K.tt("vector", ang[:], ang[:], kf[:], ALU.add, [ang, kf], [ang])
                K.act(tabs[sn][:], ang[:], ACT.Sin, [ang], [tabs[sn]])
                K.stt("vector", kf[:], ang[:], -1.0, ang[:], ALU.mult, ALU.max, [ang], [kf])
                K.act(tabs[cn][:], kf[:], ACT.Sin, [kf, cpi], [tabs[cn]], scale=-1.0, bias=cpi[:, 0:1])

        def proj_fm(hTc, wA, M, dst, dstb, wB=None, cos=None, sin=None, tmp=None, evac="scalar", func=None, N=512):
            pa = bank()
            for k in range(8):
                K.mm(pa[0:M, 0:N], wA[0][:, k, :], hTc[:, k, 0:N], k == 0, k == 7, [wA[1], hTc], [pa])
            if wB is None:
                if func is not None:
                    K.act(dst, pa[0:M, 0:N], func, [pa], [dstb])
                else:
                    K.cp(evac, dst, pa[0:M, 0:N], [pa], [dstb])
                return
            pb = bank()
            for k in range(8):
                K.mm(pb[0:M, 0:N], wB[0][:, k, :], hTc[:, k, 0:N], k == 0, k == 7, [wB[1], hTc], [pb])
            t1, t2 = tmp
            K.tt("vector", t1[0:M, 0:N], pa[0:M, 0:N], cos[0:M, 0:N], ALU.mult, [pa, cos], [t1])
            K.tt("vector", t2[0:M, 0:N], pb[0:M, 0:N], sin[0:M, 0:N], ALU.mult, [pb, sin], [t2])
            K.tt("gpsimd", dst, t1[0:M, 0:N], t2[0:M, 0:N], ALU.add, [t1, t2], [dstb])

        att = ExitStack()
        kaT2 = K.sb(att, [128, L], BF16)
        kiT3 = K.sb(att, [96, L], BF16)
        ksT = K.sb(att, [128, L], BF16)
        kwT = K.sb(att, [128, L], BF16)
        va3 = K.sb(att, [128, NT, 192], BF16)
        vs3 = K.sb(att, [128, NT, 192], BF16)
        vw3 = K.sb(att, [128, NT, 192], BF16)
        kcT2 = K.sb(att, [128, 256], BF16)
        vctm = K.sb(att, [128, 2, 128], BF16)

        with ExitStack() as pw:
            stg = [K.sb(pw, [128, 8, 512]) for _ in range(2)]
            grp = [K.sb(pw, [128, 8, 128], BF16) for _ in range(3)]
            wBfm = K.sb(pw, [128, 8, 1248], BF16)
            wBtm = K.sb(pw, [128, 8, 320], BF16)
            gi = [0]

            def load_seg(i, c0, n):
                s = stg[i % 2]
                K.dma("sync", s[:, :, 0:n], w_in_v[:, :, c0:c0 + n], [], [s])
                return s

            def rot_into(dst_ap_fn, dstb, s, off, half):
                K.ts("vector", dst_ap_fn(0, half), s[:, :, off + half:off + 2 * half], -1.0, None, ALU.mult, None, [s], [dstb])
                K.cp("gpsimd", dst_ap_fn(half, 2 * half), s[:, :, off:off + half], [s], [dstb])

            def store_grp(gidx, g):
                K.dma("sync", wq_d[gidx].rearrange("p (c m) -> p c m", c=8), g[:], [g], [wq_tok])

            def new_grp():
                g = grp[gi[0] % 3]
                gi[0] += 1
                return g

            s = load_seg(0, QA, 512)
            for g_ in range(4):
                g = new_grp()
                for u in range(2):
                    h = 4 * u + g_
                    K.cp("scalar", g[:, :, u * 64:(u + 1) * 64], s[:, :, h * 64:(h + 1) * 64], [s], [g])
                store_grp(g_, g)
                g = new_grp()
                for u in range(2):
                    h = 4 * u + g_
                    rot_into(lambda a, b, u=u, g=g: g[:, :, u * 64 + a:u * 64 + b], g, s, h * 64, 32)
                store_grp(4 + g_, g)
            s = load_seg(1, 512, 424)
            o_ka, o_va, o_qi, o_ki, o_wi = 0, 64, 128, 384, 416
            BF_KA_A, BF_KA_B, BF_KI_A, BF_KI_B, BF_KS_A, BF_KS_B, BF_KW_A, BF_KW_B, BF_KC, BF_VC = \
                0, 128, 256, 352, 448, 576, 704, 832, 960, 1088
            for u in range(2):
                K.cp("scalar", wBfm[:, :, BF_KA_A + u * 64:BF_KA_A + (u + 1) * 64], s[:, :, o_ka:o_ka + 64], [s], [wBfm])
                rot_into(lambda a, b, u=u: wBfm[:, :, BF_KA_B + u * 64 + a:BF_KA_B + u * 64 + b], wBfm, s, o_ka, 32)
            for r_ in range(3):
                K.cp("scalar", wBfm[:, :, BF_KI_A + r_ * 32:BF_KI_A + (r_ + 1) * 32], s[:, :, o_ki:o_ki + 32], [s], [wBfm])
                rot_into(lambda a, b, r_=r_: wBfm[:, :, BF_KI_B + r_ * 32 + a:BF_KI_B + r_ * 32 + b], wBfm, s, o_ki, 16)
            K.cp("scalar", wBtm[:, :, 0:64], s[:, :, o_va:o_va + 64], [s], [wBtm])
            for q_ in range(3):
                hs = [3 * q_ + i for i in range(3) if 3 * q_ + i < 8]
                g = new_grp()
                K.memset("vector", g[:], 0.0, [g])
                for i, h in enumerate(hs):
                    K.cp("scalar", g[:, :, i * 32:(i + 1) * 32], s[:, :, o_qi + h * 32:o_qi + (h + 1) * 32], [s], [g])
                store_grp(8 + q_, g)
                g = new_grp()
                K.memset("vector", g[:], 0.0, [g])
                for i, h in enumerate(hs):
                    rot_into(lambda a, b, i=i, g=g: g[:, :, i * 32 + a:i * 32 + b], g, s, o_qi + h * 32, 16)
                store_grp(11 + q_, g)
            s = load_seg(0, QB, 512)
            for g_ in range(4):
                g = new_grp() if True else None
                for u in range(2):
                    h = 4 * u + g_
                    K.cp("scalar", g[:, :, u * 64:(u + 1) * 64], s[:, :, h * 64:(h + 1) * 64], [s], [g])
                store_grp(14 + g_, g)
                g = new_grp()
                for u in range(2):
                    h = 4 * u + g_
                    rot_into(lambda a, b, u=u, g=g: g[:, :, u * 64 + a:u * 64 + b], g, s, h * 64, 32)
                store_grp(18 + g_, g)
            s = load_seg(1, KC, 512)
            K.cp("scalar", wBfm[:, :, BF_KC:BF_KC + 128], s[:, :, 0:128], [s], [wBfm])
            K.cp("scalar", wBfm[:, :, BF_VC:BF_VC + 128], s[:, :, 128:256], [s], [wBfm])
            K.cp("scalar", wBfm[:, :, BF_KS_A:BF_KS_A + 128], s[:, :, 256:384], [s], [wBfm])
            for u in range(2):
                rot_into(lambda a, b, u=u: wBfm[:, :, BF_KS_B + u * 64 + a:BF_KS_B + u * 64 + b], wBfm, s, 256 + u * 64, 32)
            K.cp("scalar", wBtm[:, :, 64:192], s[:, :, 384:512], [s], [wBtm])
            s = load_seg(0, KW, 280)
            K.cp("scalar", wBfm[:, :, BF_KW_A:BF_KW_A + 128], s[:, :, 0:128], [s], [wBfm])
            for u in range(2):
                rot_into(lambda a, b, u=u: wBfm[:, :, BF_KW_B + u * 64 + a:BF_KW_B + u * 64 + b], wBfm, s, u * 64, 32)
            K.cp("scalar", wBtm[:, :, 192:320], s[:, :, 128:256], [s], [wBtm])
            gG = K.sb(pw, [128, 8, 128], BF16)
            K.memset("vector", gG[:], 0.0, [gG])
            K.cp("scalar", gG[:, :, 0:24], s[:, :, 256:280], [s], [gG])
            wis = K.sb(pw, [128, 8, 8])
            K.dma("sync", wis[:], w_in_v[:, :, WI:WI + 8], [], [wis])
            K.cp("scalar", gG[:, :, 24:32], wis[:], [wis], [gG])
            store_grp(38, gG)
            for half in range(4):
                s = load_seg(half + 1, GA + half * 512, 512)
                for q_ in range(4):
                    g = new_grp()
                    K.cp("scalar" if q_ % 2 == 0 else "vector", g[:], s[:, :, q_ * 128:(q_ + 1) * 128], [s], [g])
                    store_grp(22 + half * 4 + q_, g)

            xb = [K.sb(pw, [128, D]) for _ in range(2)]
            sq = K.sb(pw, [128, D], BF16)
            hn = K.sb(pw, [128, D], BF16)
            st = K.sb(pw, [128, 4])
            hTc = K.sb(pw, [128, 8, 512], BF16)
            posi = K.sb(pw, [128, 512], I32)
            posf = K.sb(pw, [128, 512])
            tq = [K.sb(pw, [128, 512]), K.sb(pw, [128, 512]), K.sb(pw, [128, 512], I32)]
            tabs = {n: K.sb(pw, [128, 512], BF16) for n in ("cos64", "sin64", "cos32", "sin32")}
            rt = (K.sb(pw, [128, 512]), K.sb(pw, [128, 512]))
            kcmpT = K.sb(pw, [128, L], BF16)
            vcmpT = K.sb(pw, [128, L], BF16)
            for v3 in (va3, vs3, vw3):
                K.memset("gpsimd", v3[:, :, 64:128], 1.0, [v3])
            for c in range(NCH):
                make_hT(c, hTc, xb, sq, hn, st, gmix)
                make_rope(c, posi, posf, tq, tabs)
                cs = slice(c * 512, (c + 1) * 512)
                W = lambda off, m: (wBfm[:, :, off:off + m], wBfm)
                proj_fm(hTc, W(BF_KA_A, 128), 128, kaT2[:, cs], kaT2, W(BF_KA_B, 128), tabs["cos64"], tabs["sin64"], rt)
                proj_fm(hTc, W(BF_KI_A, 96), 96, kiT3[:, cs], kiT3, W(BF_KI_B, 96), tabs["cos32"], tabs["sin32"], rt)
                proj_fm(hTc, W(BF_KS_A, 128), 128, ksT[:, cs], ksT, W(BF_KS_B, 128), tabs["cos64"], tabs["sin64"], rt)
                proj_fm(hTc, W(BF_KW_A, 128), 128, kwT[:, cs], kwT, W(BF_KW_B, 128), tabs["cos64"], tabs["sin64"], rt)
                proj_fm(hTc, W(BF_KC, 128), 128, kcmpT[:, cs], kcmpT)
                proj_fm(hTc, W(BF_VC, 128), 128, vcmpT[:, cs], vcmpT, evac="vector")
                for j in range(4):
                    tt_ = 4 * c + j
                    pv = bank()
                    for k in range(8):
                        K.mm(pv[:, 0:320], hTc[:, k, j * 128:(j + 1) * 128], wBtm[:, k, :], k == 0, k == 7, [hTc, wBtm], [pv])
                    K.cp("scalar", va3[:, tt_, 0:64], pv[:, 0:64], [pv], [va3])
                    K.cp("vector", va3[:, tt_, 128:192], pv[:, 0:64], [pv], [va3])
                    K.cp("scalar", vs3[:, tt_, 0:64], pv[:, 64:128], [pv], [vs3])
                    K.cp("vector", vs3[:, tt_, 128:192], pv[:, 128:192], [pv], [vs3])
                    K.cp("scalar", vw3[:, tt_, 0:64], pv[:, 192:256], [pv], [vw3])
                    K.cp("vector", vw3[:, tt_, 128:192], pv[:, 256:320], [pv], [vw3])
            K.dump("kaT2", kaT2[:], [128, L], BF16, [kaT2.k])
            K.dump("kiT3", kiT3[:], [96, L], BF16, [kiT3.k])
            K.dump("ksT", ksT[:], [128, L], BF16, [ksT.k])
            K.dump("kwT", kwT[:], [128, L], BF16, [kwT.k])
            K.dump("va3", va3[:].rearrange("p a b -> p (a b)"), [128, NT * 192], BF16, [va3.k])
            K.dump("vs3", vs3[:].rearrange("p a b -> p (a b)"), [128, NT * 192], BF16, [vs3.k])

            w1 = K.sb(pw, [128, 32, 128], BF16)
            w2d = K.sb(pw, [128, 128], BF16)
            peT = K.sb(pw, [64, 32], BF16)
            hid = K.sb(pw, [128, 256], BF16)
            cb = K.sb(pw, [128, 1])
            K.memset("vector", kcT2[:], 0.0, [kcT2])
            for kind, (pe_d, w1_d, w2_d, srcT) in enumerate(((pe_k_d, w1_k_d, w2_k_d, kcmpT), (pe_v_d, w1_v_d, w2_v_d, vcmpT))):
                w1v = w1_d.rearrange("(j d) c -> d j c", d=64)
                K.dma("gpsimd", w1[0:64, :, :], w1v, [], [w1])
                K.dma("gpsimd", w1[64:128, :, :], w1v, [], [w1])
                K.dma("gpsimd", w2d[:, 0:64], w2_d, [], [w2d])
                K.dma("gpsimd", w2d[:, 64:128], w2_d, [], [w2d])
                K.dma("gpsimd", peT[:], pe_d.rearrange("j d -> d j"), [], [peT], allow_slow_non_contiguous=True)
                pbias = bank()
                for j in range(32):
                    K.mm(pbias[:, 0:1], w1[0:64, j, :], peT[:, j:j + 1], j == 0, j == 31, [w1, peT], [pbias])
                K.cp("vector", cb[:], pbias[:, 0:1], [pbias], [cb])
                for kk in range(2):
                    ph = bank()
                    lo = 64 * kk
                    for j in range(32):
                        K.mm(ph[:, 0:255], w1[lo:lo + 64, j, :], srcT[lo:lo + 64, j:j + 16 * 254 + 1:16],
                             j == 0, j == 31, [w1, srcT], [ph])
                    K.memset("vector", hid[:, 255:256], 0.0, [hid])
                    K.act(hid[:, 0:255], ph[:, 0:255], ACT.Silu, [ph, cb], [hid], bias=cb[:, 0:1])
                    if kind == 0:
                        po = bank()
                        K.mm(po[:, 0:256], w2d[:], hid[:], True, True, [w2d, hid], [po])
                        K.cp("vector", kcT2[lo:lo + 64, :], po[lo:lo + 64, 0:256], [po], [kcT2])
                    else:
                        for ch in range(2):
                            po = bank()
                            K.mm(po[:, 0:64], hid[:, ch * 128:(ch + 1) * 128], w2d[:, 0:64], True, True, [hid, w2d], [po])
                            K.cp("vector", vctm[:, ch, lo:lo + 64], po[:, 0:64], [po], [vctm])
            K.dump("kcT2", kcT2[:], [128, 256], BF16, [kcT2.k])
            K.dump("vctm", vctm[:].rearrange("p a b -> p (a b)"), [128, 256], BF16, [vctm.k])
            P.emit()
        if stop_after == "B":
            P.emit(final=True)
            att.close()
            return nc, K
        with ExitStack() as pc:
            qt_list = list(range(NT)) if qtiles is None else list(qtiles)
            ch_list = sorted(set(q // 4 for q in qt_list))
            RA = K.sb(pc, [128, L])
            idx = RA
            wout = View(RA.t[:].bitcast(BF16).rearrange("p (c f) -> p c f", c=8), RA.k)
            RBm = K.sb(pc, [128, L], BF16)
            mask = RBm
            wbra = View(RBm.t[:].rearrange("p (g f) -> p g f", g=4), RBm.k)
            RC = K.sb(pc, [128, NT, 128], BF16)
            maskT = RC
            wbrb = View(RC.t[:].rearrange("p a b -> p (a b)").rearrange("p (g f) -> p g f", g=4), RC.k)
            RD = K.sb(pc, [128, 8, 256])
            Ecmp = RD
            mergedT = View(RD.t[:].rearrange("p a b -> p (a b)").bitcast(BF16).rearrange("p (c t) -> p c t", c=8), RD.k)
            RE = K.sb(pc, [128, 2560])
            kEa, kEb, kEc = Tok(), Tok(), Tok()
            posi = View(RE.t[:, 0:512].bitcast(I32), kEa)
            posf = View(RE.t[:, 512:1024], kEa)
            ang_ = View(RE.t[:, 1024:1536], kEb)
            kf_ = View(RE.t[:, 1536:2048], kEb)
            ki_ = View(RE.t[:, 2048:2560].bitcast(I32), kEc)
            p_bf = View(RE.t[:, 0:1024].bitcast(BF16).rearrange("p (h n) -> p h n", h=8), kEa)
            pT = View(RE.t[:, 1024:2048].bitcast(BF16).rearrange("p (c h t) -> p c h t", c=2, h=8), kEb)
            R0 = View(RE.t[:, 2048:2560], kEc)
            R1 = K.sb(pc, [128, 512])
            ob = K.sb(pc, [128, 512])
            rt2 = (R1, ob)
            xb = [K.sb(pc, [128, D])]
            hn = K.sb(pc, [128, D], BF16)
            st = K.sb(pc, [128, 4])
            hTc = K.sb(pc, [128, 8, 512], BF16)
            tabs = {n: K.sb(pc, [128, 512], BF16) for n in ("cos64", "sin64", "cos32", "sin32")}
            ws = [K.sb(pc, [128, 8, 128], BF16) for _ in range(2)]
            wG = K.sb(pc, [128, 8, 128], BF16)
            qaT = K.sb(pc, [128, 4, 512], BF16)
            qiT = K.sb(pc, [96, 3, 512], BF16)
            qnT = K.sb(pc, [128, 4, 512], BF16)
            qrT = K.sb(pc, [128, 4, 512], BF16)
            gT = K.sb(pc, [32, 512], BF16)
            oaTc = K.sb(pc, [128, 4, 512], BF16)
            obTc = K.sb(pc, [128, 4, 512], BF16)
            Eb = [K.sb(pc, [128, 512], BF16) for _ in range(NEB)]
            Pb = [K.sb(pc, [128, 512], BF16) for _ in range(NEB)]
            Esel = K.sb(pc, [64, 32, 128], BF16)
            eye24 = K.sb(pc, [24, 24], BF16)
            ones24 = K.sb(pc, [24, 128], BF16)
            Dg = K.sb(pc, [24, 8, 128], BF16)
            gBs = [K.sb(pc, [128, 512], BF16) for _ in range(2)]
            rs = K.sb(pc, [128, 512])
            sm = K.sb(pc, [128, 64])
            wi_sb2 = [K.sb(pc, [128, 8]) for _ in range(2)]
            smb2 = [K.sb(pc, [128, 32]) for _ in range(2)]
            P4 = K.sb(pc, [128, 2, 256])
            imp = K.sb(pc, [128, 2, 64])
            scs = K.sb(pc, [128, 2, 64])
            sc2 = K.sb(pc, [128, 64])
            selb = K.sb(pc, [128, 64])
            bm = K.sb(pc, [128, 2, 64], BF16)
            bmT = K.sb(pc, [64, 2, 128], BF16)
            mexp = [K.sb(pc, [128, 2, 128], BF16) for _ in range(4)]
            er = [0]

            def Enext():
                er[0] += 1
                return Eb[er[0] % NEB], Pb[er[0] % NEB]

            def pipe(units, qk_fn, pv_fn, depth=PIPE_DEPTH, hook=None):
                if PAIR and len(units) >= 2 and hasattr(qk_fn, "mm"):
                    sis = []
                    for un in units:
                        if un[0] not in sis:
                            sis.append(un[0])
                    pend = []
                    for si in sis:
                        sts = [qk_fn.mm((si, u)) for u in range(2)]
                        outs = [qk_fn.post((si, u), sts[u]) for u in range(2)]
                        pend.append((si, outs))
                        if hook is not None:
                            hook(); hook()
                        if len(pend) > 1:
                            si0, o0 = pend.pop(0)
                            for u in range(2):
                                pv_fn((si0, u), o0[u])
                    for si0, o0 in pend:
                        for u in range(2):
                            pv_fn((si0, u), o0[u])
                    return
                pend = []
                for un in units:
                    pend.append((un, qk_fn(un)))
                    if hook is not None:
                        hook()
                    if len(pend) > depth:
                        pv_fn(*pend.pop(0))
                for p_ in pend:
                    pv_fn(*p_)

            with ExitStack() as cc:
                K.dma("gpsimd", Esel[:].rearrange("p a b -> p (a b)"), c_esel_d, [], [Esel])
                K.dma("gpsimd", eye24[:], c_eye_d, [], [eye24])
                K.memset("vector", ones24[:], 1.0, [ones24])
                K.memset("vector", sm[:, 32:33], 0.5, [sm])
                P.emit()

            def load_ws(gidx, i):
                w = ws[i % 2]
                K.dma("sync", w[:], wq_d[gidx].rearrange("p (c m) -> p c m", c=8), [wq_tok], [w])
                return w

            A_banks = (banks[4], banks[5])
            B_banks = (banks[6], banks[7])
            SC = 0.125
            wsi = [0]

            for c in ch_list:
                make_hT(c, hTc, xb, hn, hn, st, gmix)
                make_rope(c, posi, posf, (ang_, kf_, ki_), tabs)
                if lvl < 0.2:
                    continue
                for g_ in range(4):
                    wA = load_ws(g_, wsi[0]); wsi[0] += 1
                    wB = load_ws(4 + g_, wsi[0]); wsi[0] += 1
                    proj_fm(hTc, (wA[:], wA), 128, qaT[:, g_, :], qaT, (wB[:], wB), tabs["cos64"], tabs["sin64"], rt2)
                for q_ in (range(3) if lvl >= 0.5 else []):
                    wA = load_ws(8 + q_, wsi[0]); wsi[0] += 1
                    wB = load_ws(11 + q_, wsi[0]); wsi[0] += 1
                    proj_fm(hTc, (wA[:, :, 0:96], wA), 96, qiT[:, q_, :], qiT, (wB[:, :, 0:96], wB), tabs["cos32"], tabs["sin32"], rt2)
                for g_ in (range(4) if lvl >= 0.75 else []):
                    wA = load_ws(14 + g_, wsi[0]); wsi[0] += 1
                    wB = load_ws(18 + g_, wsi[0]); wsi[0] += 1
                    pa = bank(); pb = bank()
                    for k in range(8):
                        K.mm(pa[:, 0:512], wA[:, k, :], hTc[:, k, :], k == 0, k == 7, [wA, hTc], [pa])
                    for k in range(8):
                        K.mm(pb[:, 0:512], wB[:, k, :], hTc[:, k, :], k == 0, k == 7, [wB, hTc], [pb])
                    K.cp("scalar", qnT[:, g_, :], pa[:, 0:512], [pa], [qnT])
                    t1, t2 = rt2
                    K.tt("vector", t1[:, :], pa[:, 0:512], tabs["cos64"][:, :], ALU.mult, [pa, tabs["cos64"]], [t1])
                    K.tt("vector", t2[:, :], pb[:, 0:512], tabs["sin64"][:, :], ALU.mult, [pb, tabs["sin64"]], [t2])
                    K.tt("gpsimd", qrT[:, g_, :], t1[:, :], t2[:, :], ALU.add, [t1, t2], [qrT])
                if lvl >= 0.9:
                    K.dma("sync", wG[:], wq_d[38].rearrange("p (c m) -> p c m", c=8), [wq_tok], [wG])
                    proj_fm(hTc, (wG[:, :, 0:32], wG), 32, gT[:, :], gT, func=ACT.Sigmoid)
                K.dump(f"qaT{c}", qaT[:].rearrange("p a b -> p (a b)"), [128, 2048], BF16, [qaT.k])
                K.dump(f"qiT{c}", qiT[:].rearrange("p a b -> p (a b)"), [96, 1536], BF16, [qiT.k])
                K.dump(f"qrT{c}", qrT[:].rearrange("p a b -> p (a b)"), [128, 2048], BF16, [qrT.k])
                K.dump(f"gT{c}", gT[:], [32, 512], BF16, [gT.k])

                NIT = 14
                tiles_c = ([q for q in qt_list if q // 4 == c] if lvl >= 2 else [])

                def pre_a(qt):
                    t0 = qt * 128
                    tl = (qt % 4) * 128
                    tsl = slice(tl, tl + 128)
                    n = t0 + 128
                    wi_ = wi_sb2[qt % 2]
                    smb = smb2[qt % 2]
                    pw_ = bank()
                    for k in range(8):
                        K.mm(pw_[:, 0:8], hTc[:, k, tsl], wG[:, k, 24:32], k == 0, k == 7, [hTc, wG], [pw_])
                    K.cp("vector", wi_[:], pw_[:, 0:8], [pw_], [wi_])
                    nsc = (n + 511) // 512
                    ri = 0
                    for sc_i in range(nsc):
                        c0 = sc_i * 512
                        ncol = min(512, n - c0)
                        for h in range(8):
                            q_, r_ = h // 3, h % 3
                            pi_ = bank()
                            K.mm(pi_[:, 0:ncol], qiT[32 * r_:32 * r_ + 32, q_, tsl], kiT3[32 * r_:32 * r_ + 32, c0:c0 + ncol],
                                 True, True, [qiT, kiT3], [pi_])
                            Rb = (R0, R1)[ri % 2]; ri += 1
                            K.act(Rb[:, 0:ncol], pi_[:, 0:ncol], ACT.Relu, [pi_], [Rb])
                            if h == 0:
                                K.ts("vector", idx[:, c0:c0 + ncol], Rb[:, 0:ncol], wi_[:, 0:1], None, ALU.mult, None, [Rb, wi_], [idx])
                            else:
                                K.stt("vector", idx[:, c0:c0 + ncol], Rb[:, 0:ncol], wi_[:, h:h + 1], idx[:, c0:c0 + ncol],
                                      ALU.mult, ALU.add, [Rb, wi_, idx], [idx])
                    P.op("vector", lambda e, n=n: e.tensor_reduce(out=smb[:, 0:1], in_=idx[:, 0:n], axis=AX.X, op=ALU.max), K._tk([idx]), K._tk([smb]))
                    P.op("vector", lambda e, n=n: e.tensor_reduce(out=smb[:, 1:2], in_=idx[:, 0:n], axis=AX.X, op=ALU.min), K._tk([idx]), K._tk([smb]))
                    K.asel(idx[:, t0:t0 + 128], idx[:, t0:t0 + 128], [[-1, 128]], ALU.is_ge, -1e30, 0, 1, [idx], [idx])
                    K.ts("vector", smb[:, 2:3], smb[:, 1:2], -1.0, None, ALU.add, None, [smb], [smb])
                    K.stt("vector", smb[:, 3:4], smb[:, 0:1], 1.0, smb[:, 2:3], ALU.add, ALU.subtract, [smb], [smb])
                    K.memset("vector", smb[:, 8:8 + NIT], 0.0, [smb])

                def bis_step(qt, it):
                    n = qt * 128 + 128
                    smb = smb2[qt % 2]
                    f = 2.0 ** -(it + 1)
                    K.stt("vector", smb[:, 4:5], smb[:, 3:4], f, smb[:, 2:3], ALU.mult, ALU.add, [smb], [smb])
                    K.ts("vector", mask[:, 0:n], idx[:, 0:n], smb[:, 4:5], 0.0, ALU.is_ge, ALU.add, [idx, smb, mask], [mask, smb],
                         accum_out=smb[:, 8 + it:9 + it])
                    K.ts("vector", smb[:, 5:6], smb[:, 8 + it:9 + it], 256.0, f, ALU.is_ge, ALU.mult, [smb], [smb])
                    K.stt("vector", smb[:, 2:3], smb[:, 3:4], smb[:, 5:6], smb[:, 2:3], ALU.mult, ALU.add, [smb], [smb])

                def pre_c(qt):
                    n = qt * 128 + 128
                    smb = smb2[qt % 2]
                    K.ts("vector", mask[:, 0:n], idx[:, 0:n], smb[:, 2:3], None, ALU.is_ge, None, [idx, smb], [mask])
                    K.dump(f"mask{qt}", mask[:, 0:n], [128, n], BF16, [mask.k])
                    if lvl < 3:
                        return
                    for b0 in range(0, qt + 1, 8):
                        nb = min(8, qt + 1 - b0)
                        pm_ = bank()
                        for i in range(nb):
                            si = b0 + i
                            K.tr(bfv(pm_)[:, i * 128:(i + 1) * 128], mask[:, si * 128:(si + 1) * 128], ident[:], [mask, ident], [pm_])
                        K.cp("scalar", maskT[:, b0:b0 + nb, :], bfv(pm_)[:, 0:nb * 128].rearrange("p (a b) -> p a b", b=128), [pm_], [maskT])

                if tiles_c:
                    pre_a(tiles_c[0])
                    for it in range(NIT):
                        bis_step(tiles_c[0], it)
                    pre_c(tiles_c[0])
                for qj, qt in enumerate(tiles_c):
                    t0 = qt * 128
                    tl = (qt % 4) * 128
                    tsl = slice(tl, tl + 128)
                    n = t0 + 128
                    nxt = tiles_c[qj + 1] if qj + 1 < len(tiles_c) else None
                    if lvl < 3:
                        if nxt is not None:
                            pre_a(nxt)
                            for it in range(NIT):
                                bis_step(nxt, it)
                            pre_c(nxt)
                        continue
                    steps_left = list(range(NIT)) if nxt is not None else []
                    if nxt is not None:
                        pre_a(nxt)

                    def bis_hook():
                        if steps_left:
                            bis_step(nxt, steps_left.pop(0))

                    def dsa_mm(un):
                        si, u = un
                        ssl = slice(si * 128, (si + 1) * 128)
                        lo = 64 * u
                        ps_ = bank()
                        K.mm(ps_[:, 0:512].rearrange("p (g t) -> p g t", g=4), kaT2[lo:lo + 64, ssl], qaT[lo:lo + 64, :, tsl],
                             True, True, [kaT2, qaT], [ps_])
                        return ps_

                    def dsa_post(un, ps_):
                        si, u = un
                        E_, Pm_ = Enext()
                        K.act(E_[:, :], ps_[:, 0:512], ACT.Exp, [ps_], [E_], scale=SC)
                        K.tt("vector", Pm_[:, :].rearrange("p (g t) -> p g t", g=4), E_[:, :].rearrange("p (g t) -> p g t", g=4),
                             bc(maskT[:, si, :].unsqueeze(1), [128, 4, 128]), ALU.mult, [E_, maskT], [Pm_])
                        return Pm_

                    def dsa_qk(un):
                        return dsa_post(un, dsa_mm(un))
                    dsa_qk.mm = dsa_mm
                    dsa_qk.post = dsa_post

                    def dsa_pv(un, Pm_):
                        si, u = un
                        lo = 64 * u
                        K.mm(A_banks[u][:, 0:512], va3[:, si, lo:lo + 128], Pm_[:, :], si == 0, si == qt, [va3, Pm_], [A_banks[u]])

                    pipe([(si, u) for si in range(qt + 1) for u in range(2)], dsa_qk, dsa_pv, hook=bis_hook)
                    for u in range(2):
                        lo = 64 * u; lr = 64 * (1 - u)
                        K.recip(rs[lo:lo + 64, :], A_banks[u][lr:lr + 64, 0:512], [A_banks[u]], [rs])
                        K.tt("vector", oaTc[lo:lo + 64, :, tsl], A_banks[u][lo:lo + 64, 0:512].rearrange("p (g t) -> p g t", g=4),
                             rs[lo:lo + 64, :].rearrange("p (g t) -> p g t", g=4), ALU.mult, [A_banks[u], rs], [oaTc])
                    while steps_left:
                        bis_step(nxt, steps_left.pop(0))
                    if nxt is not None:
                        pre_c(nxt)
                    if lvl < 4:
                        continue
                    for k in range(2):
                        lo = 64 * k
                        for gp in range(2):
                            ps_ = bank()
                            for jj in range(2):
                                g_ = 2 * gp + jj
                                K.mm(ps_[:, jj * 256:(jj + 1) * 256], qnT[lo:lo + 64, g_, tsl], kcT2[lo:lo + 64, 0:256], True, True, [qnT, kcT2], [ps_])
                            h0 = 4 * k + 2 * gp
                            K.act(Ecmp[:, h0:h0 + 2, :], ps_[:, 0:512].rearrange("p (a n) -> p a n", a=2), ACT.Exp, [ps_], [Ecmp], scale=SC)
                    K.asel(Ecmp[:], Ecmp[:], [[0, 8], [-16, 256]], ALU.is_ge, 0.0, t0 - 31, 1, [Ecmp], [Ecmp])
                    P.op("vector", lambda e: e.tensor_reduce(out=sm[:, 40:48], in_=Ecmp[:], axis=AX.X, op=ALU.add), K._tk([Ecmp]), K._tk([sm]))
                    K.ts("vector", sm[:, 40:48], sm[:, 40:48], 1e-30, None, ALU.add, None, [sm], [sm])
                    K.recip(sm[:, 48:56], sm[:, 40:48], [sm], [sm])
                    K.tt("vector", Ecmp[:], Ecmp[:], bc(sm[:, 48:56].unsqueeze(2), [128, 8, 256]), ALU.mult, [Ecmp, sm], [Ecmp])
                    K.cp("gpsimd", p_bf[:], Ecmp[:], [Ecmp], [p_bf])
                    P.op("vector", lambda e: e.tensor_reduce(out=P4[:], in_=Ecmp[:].rearrange("p (k g) n -> p k n g", k=2), axis=AX.X, op=ALU.add),
                         K._tk([Ecmp]), K._tk([P4]))
                    P.op("vector", lambda e: e.tensor_reduce(out=imp[:], in_=P4[:].rearrange("p k (j i) -> p k j i", i=4), axis=AX.X, op=ALU.add),
                         K._tk([P4]), K._tk([imp]))
                    K.tt("vector", imp[:, :, 1:64], imp[:, :, 1:64], P4[:, :, 3:252:4], ALU.add, [imp, P4], [imp])
                    K.dma("sync", selb[0:64, :], c_selb_d[2 * qt:2 * qt + 1, :].partition_broadcast(64), [], [selb])
                    K.dma("sync", selb[64:128, :], c_selb_d[2 * qt + 1:2 * qt + 2, :].partition_broadcast(64), [], [selb])
                    K.tt("vector", scs[:], imp[:], bc(selb[:, :].unsqueeze(1), [128, 2, 64]), ALU.add, [imp, selb], [scs])
                    for k in range(2):
                        P.op("vector", lambda e, k=k: e.max(out=sm[:, 16:24], in_=scs[:, k, :]), K._tk([scs]), K._tk([sm]))
                        P.op("vector", lambda e, k=k: e.match_replace(out=sc2[:], in_to_replace=sm[:, 16:24], in_values=scs[:, k, :], imm_value=-1e9),
                             K._tk([scs, sm]), K._tk([sc2]))
                        P.op("vector", lambda e: e.max(out=sm[:, 24:32], in_=sc2[:]), K._tk([sc2]), K._tk([sm]))
                        K.ts("vector", bm[:, k, :], scs[:, k, :], sm[:, 31:32], None, ALU.is_ge, None, [scs, sm], [bm])
                    K.dump(f"bm{qt}", bm[:].rearrange("p a b -> p (a b)"), [128, 128], BF16, [bm.k])
                    pb_ = bank()
                    for k in range(2):
                        K.tr(bfv(pb_)[0:64, k * 128:(k + 1) * 128], bm[:, k, :], ident[:], [bm, ident], [pb_])
                    K.cp("scalar", bmT[:], bfv(pb_)[0:64, 0:256].rearrange("p (k t) -> p k t", k=2), [pb_], [bmT])
                    for ch in range(2):
                        pp_ = bank()
                        for h in range(8):
                            K.tr(bfv(pp_)[:, h * 128:(h + 1) * 128], p_bf[:, h, ch * 128:(ch + 1) * 128], ident[:], [p_bf, ident], [pp_])
                        K.cp("scalar" if ch == 0 else "vector", pT[:, ch, :, :], bfv(pp_).rearrange("p (h t) -> p h t", h=8), [pp_], [pT])
                    for k in range(2):
                        for ch in range(2):
                            K.mm(B_banks[k][:, 0:512].rearrange("p (g t) -> p g t", g=4), vctm[:, ch, :], pT[:, ch, 4 * k:4 * k + 4, :],
                                 ch == 0, ch == 1, [vctm, pT], [B_banks[k]])
                    def gate_bcast(cidx, k):
                        pg_ = bank()
                        K.mm(pg_[:, 0:512].rearrange("p (g t) -> p g t", g=4), ones24[:, :], Dg[:, 4 * k:4 * k + 4, :], True, True, [ones24, Dg], [pg_])
                        gb_ = gBs[k]
                        K.cp("scalar", gb_[64 * k:64 * k + 64, :], pg_[64 * k:64 * k + 64, 0:512], [pg_], [gb_])
                        return gb_

                    def make_Dg(cidx):
                        K.tt("vector", Dg[:], bc(gT[0:24, tsl].unsqueeze(1), [24, 8, 128]),
                             bc(eye24[:, cidx * 8:cidx * 8 + 8].unsqueeze(2), [24, 8, 128]), ALU.mult, [gT, eye24], [Dg])

                    make_Dg(0)
                    for k in range(2):
                        lo = 64 * k
                        gb_ = gate_bcast(0, k)
                        K.tt("vector", ob[lo:lo + 64, :], B_banks[k][lo:lo + 64, 0:512], gb_[lo:lo + 64, :], ALU.mult, [B_banks[k], gb_], [ob])
                    if lvl < 5:
                        continue
                    mes = {}

                    def slc_mm(un):
                        si, k = un
                        ssl = slice(si * 128, (si + 1) * 128)
                        if k == 0:
                            pm_ = bank()
                            K.mm(pm_[:, 0:256].rearrange("p (k t) -> p k t", k=2), Esel[:, si, :], bmT[:, :, :], True, True, [Esel, bmT], [pm_])
                            me = mexp[si % 4]
                            K.cp("scalar", me[:], pm_[:, 0:256].rearrange("p (k t) -> p k t", k=2), [pm_], [me])
                            if si == qt:
                                K.tt("gpsimd", me[:], me[:], bc(diagT[:, :].unsqueeze(1), [128, 2, 128]), ALU.mult, [me, diagT], [me])
                            mes[si] = me
                        lo = 64 * k
                        ps_ = bank()
                        K.mm(ps_[:, 0:512].rearrange("p (g t) -> p g t", g=4), ksT[lo:lo + 64, ssl], qrT[lo:lo + 64, :, tsl], True, True, [ksT, qrT], [ps_])
                        return ps_

                    def slc_post(un, ps_):
                        si, k = un
                        me = mes[si]
                        E_, Pm_ = Enext()
                        K.act(E_[:, :], ps_[:, 0:512], ACT.Exp, [ps_], [E_], scale=SC)
                        K.tt("vector", Pm_[:, :].rearrange("p (g t) -> p g t", g=4), E_[:, :].rearrange("p (g t) -> p g t", g=4),
                             bc(me[:, k, :].unsqueeze(1), [128, 4, 128]), ALU.mult, [E_, me], [Pm_])
                        return Pm_

                    def slc_qk(un):
                        return slc_post(un, slc_mm(un))
                    slc_qk.mm = slc_mm
                    slc_qk.post = slc_post

                    def slc_pv(un, Pm_):
                        si, k = un
                        lo = 64 * k
                        K.mm(A_banks[k][:, 0:512], vs3[:, si, lo:lo + 128], Pm_[:, :], si == 0, si == qt, [vs3, Pm_], [A_banks[k]])

                    pipe([(si, k) for si in range(qt + 1) for k in range(2)], slc_qk, slc_pv)

                    def fin(acc, cidx, last):
                        make_Dg(cidx)
                        for k in range(2):
                            lo = 64 * k; lr = 64 * (1 - k)
                            gb_ = gate_bcast(cidx, k)
                            K.recip(rs[lo:lo + 64, :], acc[k][lr:lr + 64, 0:512], [acc[k]], [rs])
                            K.tt("gpsimd", rs[lo:lo + 64, :], rs[lo:lo + 64, :], gb_[lo:lo + 64, :], ALU.mult, [rs, gb_], [rs])
                            tmp = R1
                            K.tt("vector", tmp[lo:lo + 64, :], acc[k][lo:lo + 64, 0:512], rs[lo:lo + 64, :], ALU.mult, [acc[k], rs], [tmp])
                            if not last:
                                K.tt("gpsimd", ob[lo:lo + 64, :], ob[lo:lo + 64, :], tmp[lo:lo + 64, :], ALU.add, [ob, tmp], [ob])
                            else:
                                K.tt("gpsimd", obTc[lo:lo + 64, :, tsl], ob[lo:lo + 64, :].rearrange("p (g t) -> p g t", g=4),
                                     tmp[lo:lo + 64, :].rearrange("p (g t) -> p g t", g=4), ALU.add, [ob, tmp], [obTc])

                    fin(A_banks, 1, False)
                    if lvl < 6:
                        continue
                    s_lo = max(0, qt - 4)

                    def win_mm(un):
                        si, k = un
                        ssl = slice(si * 128, (si + 1) * 128)
                        lo = 64 * k
                        ps_ = bank()
                        K.mm(ps_[:, 0:512].rearrange("p (g t) -> p g t", g=4), kwT[lo:lo + 64, ssl], qrT[lo:lo + 64, :, tsl], True, True, [kwT, qrT], [ps_])
                        return ps_

                    def win_post(un, ps_):
                        si, k = un
                        E_, Pm_ = Enext()
                        K.act(E_[:, :], ps_[:, 0:512], ACT.Exp, [ps_], [E_], scale=SC)
                        mk = diagT if si == qt else (antiT if si == qt - 4 else None)
                        src = E_
                        if mk is not None:
                            K.tt("vector", Pm_[:, :].rearrange("p (g t) -> p g t", g=4), E_[:, :].rearrange("p (g t) -> p g t", g=4),
                                 bc(mk[:, :].unsqueeze(1), [128, 4, 128]), ALU.mult, [E_, mk], [Pm_])
                            src = Pm_
                        return src

                    def win_qk(un):
                        return win_post(un, win_mm(un))
                    win_qk.mm = win_mm
                    win_qk.post = win_post

                    def win_pv(un, src):
                        si, k = un
                        lo = 64 * k
                        K.mm(B_banks[k][:, 0:512], vw3[:, si, lo:lo + 128], src[:, :], si == s_lo, si == qt, [vw3, src], [B_banks[k]])

                    pipe([(si, k) for si in range(s_lo, qt + 1) for k in range(2)], win_qk, win_pv)
                    fin(B_banks, 2, True)

                for qt in [q for q in qt_list if q // 4 == c]:
                    tl = (qt % 4) * 128
                    for nm_, bt_ in (("oaT", oaTc), ("obT", obTc)):
                        if f"{nm_}{qt}" in K.dbg:
                            d_ = nc.dram_tensor(f"dbg_{nm_}{qt}", [128, 4, 128], BF16, kind="ExternalOutput").ap()
                            K.dma("sync", d_, bt_[:, :, tl:tl + 128], [bt_], [])
                if lvl < 7:
                    continue
                for u in range(2):
                    K.dma("gpsimd", wbra[64 * u:64 * u + 64, :, :], w_br_a_d[256 * u:256 * (u + 1), :].rearrange("(g d) f -> d g f", d=64), [], [wbra])
                    K.dma("gpsimd", wbrb[64 * u:64 * u + 64, :, :], w_br_b_d[256 * u:256 * (u + 1), :].rearrange("(g d) f -> d g f", d=64), [], [wbrb])
                K.dma("gpsimd", wout[:], w_out_d.rearrange("(c p) f -> p c f", p=128), [], [wout])
                for fc in range(8):
                    fsl = slice(fc * 128, (fc + 1) * 128)
                    outs_ = []
                    for br, (gbase, wbr, oT) in enumerate(((22, wbra, oaTc), (30, wbrb, obTc))):
                        wg_ = load_ws(gbase + fc, wsi[0]); wsi[0] += 1
                        pg_ = bank()
                        for k in range(8):
                            K.mm(pg_[:, 0:512], wg_[:, k, :], hTc[:, k, :], k == 0, k == 7, [wg_, hTc], [pg_])
                        E_, Pm_ = Enext()
                        K.act(E_[:, :], pg_[:, 0:512], ACT.Sigmoid, [pg_], [E_])
                        pbr = bank()
                        for g_ in range(4):
                            K.mm(pbr[:, 0:512], wbr[:, g_, fsl], oT[:, g_, :], g_ == 0, g_ == 3, [wbr, oT], [pbr])
                        K.tt("vector", Pm_[:, :], pbr[:, 0:512], E_[:, :], ALU.mult, [pbr, E_], [Pm_])
                        outs_.append(Pm_)
                    K.tt("gpsimd", mergedT[:, fc, :], outs_[0][:, :], outs_[1][:, :], ALU.add, [outs_[0], outs_[1]], [mergedT])
                K.dump(f"mergedT{c}", mergedT[:].rearrange("p a b -> p (a b)"), [128, 4096], BF16, [mergedT.k])
                for j in range(4):
                    tt_ = 4 * c + j
                    xt = xb[0]
                    K.dma("sync", xt[:], x_d[tt_ * 128:(tt_ + 1) * 128, :], [], [xt])
                    for half in range(2):
                        po_ = bank()
                        for fc in range(8):
                            K.mm(po_[:, 0:512], mergedT[:, fc, j * 128:(j + 1) * 128], wout[:, fc, half * 512:(half + 1) * 512],
                                 fc == 0, fc == 7, [mergedT, wout], [po_])
                        K.tt("vector", xt[:, half * 512:(half + 1) * 512], po_[:, 0:512], xt[:, half * 512:(half + 1) * 512], ALU.add, [po_, xt], [xt])
                    K.dma("sync", x1_d[tt_ * 128:(tt_ + 1) * 128, :], xt[:], [xt], [x1_tok])
                    K.dump(f"x1_{tt_}", xt[:], [128, D], F32, [xt.k])
            P.emit()
        if stop_after == "C":
            P.emit(final=True)
            att.close()
            return nc, K
        att.close()
        if moe_from_x:
            x1_d = x_d
        with ExitStack() as pm:
            HT = 16
            identf = K.sb(pm, [128, 128])
            gffn = K.sb(pm, [128, 8])
            gfin = K.sb(pm, [128, D])
            wr = K.sb(pm, [128, 8, 36])
            rb = K.sb(pm, [128, 36])
            h2T = K.sb(pm, [128, 8, HT * 128], BF16)
            yacc = K.sb(pm, [128, HT, D])
            wgt = K.sb(pm, [128, HT, 32])
            wgu = [K.sb(pm, [128, 8, 512], BF16) for _ in range(2)]
            wdn = [K.sb(pm, [128, 2, D], BF16) for _ in range(2)]
            aT = [K.sb(pm, [128, 2, 512], BF16) for _ in range(2)]
            sg = [K.sb(pm, [128, 512], BF16) for _ in range(2)]
            xm = [K.sb(pm, [128, D]) for _ in range(2)]
            xn = K.sb(pm, [128, D])
            h2f = K.sb(pm, [128, 8, 128])
            sq2 = K.sb(pm, [128, D], BF16)
            s2 = K.sb(pm, [128, 16])
            lg = K.sb(pm, [128, 36])
            me = K.sb(pm, [128, 32])
            ex = K.sb(pm, [128, 32])
            m8 = K.sb(pm, [128, 8])
            K.memset("gpsimd", identf[:], 1.0, [identf])
            K.asel(identf[:], identf[:], [[-1, 128]], ALU.is_equal, 0.0, 0, 1, [identf], [identf])
            K.dma("sync", gffn[:], norm_ffn_d, [], [gffn])
            K.dma("sync", gfin[:], norm_final_d.partition_broadcast(128), [], [gfin])
            K.dma("sync", wr[:, :, 0:4], w_group_d.rearrange("(c p) n -> p c n", p=128), [], [wr])
            K.dma("sync", wr[:, :, 4:36], w_expert_d.rearrange("(c p) n -> p c n", p=128), [], [wr])
            K.dma("sync", rb[:, 0:4], b_group_d.partition_broadcast(128), [], [rb])
            K.dma("sync", rb[:, 4:36], b_expert_d.partition_broadcast(128), [], [rb])
            BIG = 30000.0
            xi = [0]
            wl = [0]
            for half in range(2):
                for j in range(HT):
                    tt_ = half * HT + j
                    xt = xm[xi[0] % 2]; xi[0] += 1
                    K.dma("sync", xt[:], x1_d[tt_ * 128:(tt_ + 1) * 128, :], [x1_tok], [xt])
                    K.memset("vector", s2[:, 0:1], 0.0, [s2])
                    K.act(sq2[:], xt[:], ACT.Square, [xt, s2], [sq2, s2], accum_out=s2[:, 0:1])
                    K.act(s2[:, 1:2], s2[:, 0:1], ACT.Sqrt, [s2, cpi], [s2], scale=1.0 / D, bias=cpi[:, 1:2])
                    K.recip(s2[:, 2:3], s2[:, 1:2], [s2], [s2])
                    K.ts("vector", xn[:], xt[:], s2[:, 2:3], None, ALU.mult, None, [xt, s2], [xn])
                    for hb in range(2):
                        pb = bank()
                        for k in range(4):
                            kk = hb * 4 + k
                            K.tr(pb[:, k * 128:(k + 1) * 128], xn[:, kk * 128:(kk + 1) * 128], identf[:], [xn, identf], [pb])
                        K.tt("vector", h2f[:, hb * 4:hb * 4 + 4, :], pb[:, 0:512].rearrange("p (k t) -> p k t", k=4),
                             bc(gffn[:, hb * 4:hb * 4 + 4].unsqueeze(2), [128, 4, 128]), ALU.mult, [pb, gffn], [h2f])
                    K.cp("scalar", h2T[:, :, j * 128:(j + 1) * 128], h2f[:], [h2f], [h2T])
                    pr = bank()
                    for k in range(8):
                        K.mm(pr[:, 0:36], h2f[:, k, :], wr[:, k, :], k == 0, k == 7, [h2f, wr], [pr])
                    K.tt("vector", lg[:], pr[:, 0:36], rb[:], ALU.add, [pr, rb], [lg])
                    P.op("vector", lambda e: e.tensor_reduce(out=s2[:, 4:5], in_=lg[:, 0:4], axis=AX.X, op=ALU.max), K._tk([lg]), K._tk([s2]))
                    K.ts("vector", s2[:, 5:6], s2[:, 4:5], -1.0, None, ALU.mult, None, [s2], [s2])
                    K.memset("vector", s2[:, 6:7], 0.0, [s2])
                    K.act(ex[:, 0:4], lg[:, 0:4], ACT.Exp, [lg, s2], [ex, s2], bias=s2[:, 5:6], accum_out=s2[:, 6:7])
                    K.recip(s2[:, 7:8], s2[:, 6:7], [s2], [s2])
                    K.ts("vector", ex[:, 4:8], lg[:, 0:4], s2[:, 4:5], BIG, ALU.is_ge, ALU.mult, [lg, s2], [ex])
                    K.ts("vector", ex[:, 4:8], ex[:, 4:8], -BIG, None, ALU.add, None, [ex], [ex])
                    K.tt("vector", me[:].rearrange("p (g i) -> p g i", g=4), lg[:, 4:36].rearrange("p (g i) -> p g i", g=4),
                         bc(ex[:, 4:8].unsqueeze(2), [128, 4, 8]), ALU.add, [lg, ex], [me])
                    P.op("vector", lambda e: e.max(out=m8[:], in_=me[:]), K._tk([me]), K._tk([m8]))
                    K.ts("vector", s2[:, 8:9], m8[:, 0:1], -1.0, None, ALU.mult, None, [m8], [s2])
                    K.act(ex[:], me[:], ACT.Exp, [me, s2], [ex], bias=s2[:, 8:9])
                    K.act(s2[:, 9:10], m8[:, 1:2], ACT.Exp, [m8, s2], [s2], bias=s2[:, 8:9])
                    K.ts("vector", s2[:, 9:10], s2[:, 9:10], 1.0, None, ALU.add, None, [s2], [s2])
                    K.recip(s2[:, 10:11], s2[:, 9:10], [s2], [s2])
                    K.tt("vector", s2[:, 11:12], s2[:, 10:11], s2[:, 7:8], ALU.mult, [s2], [s2])
                    K.ts("vector", me[:], me[:], m8[:, 1:2], None, ALU.is_ge, None, [me, m8], [me])
                    K.tt("vector", ex[:], ex[:], me[:], ALU.mult, [ex, me], [ex])
                    K.ts("vector", wgt[:, j, :], ex[:], s2[:, 11:12], None, ALU.mult, None, [ex, s2], [wgt])
                K.dump(f"wgt{half}", wgt[:].rearrange("p a b -> p (a b)"), [128, HT * 32], F32, [wgt.k])
                wts = {}

                def moe_up(item):
                    e_, tch = item
                    if tch == 0:
                        wg_ = wgu[wl[0] % 2]; wd_ = wdn[wl[0] % 2]; wl[0] += 1
                        K.dma("gpsimd", wg_[:], w_gu_d[e_].rearrange("(c p) f -> p c f", p=128), [], [wg_])
                        K.dma("gpsimd", wd_[:], w_dn_d[e_].rearrange("(c p) f -> p c f", p=128), [], [wd_])
                        wts[e_] = (wg_, wd_)
                    wg_, wd_ = wts[e_]
                    a_ = aT[tch % 2]
                    csl = slice(tch * 512, (tch + 1) * 512)
                    for fo in range(2):
                        pg_ = bank(); pu_ = bank()
                        for k in range(8):
                            K.mm(pg_[:, 0:512], wg_[:, k, fo * 128:(fo + 1) * 128], h2T[:, k, csl], k == 0, k == 7, [wg_, h2T], [pg_])
                        for k in range(8):
                            K.mm(pu_[:, 0:512], wg_[:, k, 256 + fo * 128:256 + (fo + 1) * 128], h2T[:, k, csl], k == 0, k == 7, [wg_, h2T], [pu_])
                        s_ = sg[fo]
                        K.act(s_[:, :], pg_[:, 0:512], ACT.Silu, [pg_], [s_])
                        K.tt("vector", a_[:, fo, :], pu_[:, 0:512], s_[:, :], ALU.mult, [pu_, s_], [a_])

                def moe_down(item):
                    e_, tch = item
                    wg_, wd_ = wts[e_]
                    a_ = aT[tch % 2]
                    for tj in range(4):
                        tile_ = tch * 4 + tj
                        for hf in range(2):
                            po_ = bank((4, 5, 6, 7))
                            for fo in range(2):
                                K.mm(po_[:, 0:512], a_[:, fo, tj * 128:(tj + 1) * 128], wd_[:, fo, hf * 512:(hf + 1) * 512],
                                     fo == 0, fo == 1, [a_, wd_], [po_])
                            ysl = yacc[:, tile_, hf * 512:(hf + 1) * 512]
                            if e_ == 0:
                                K.ts("vector", ysl, po_[:, 0:512], wgt[:, tile_, e_:e_ + 1], None, ALU.mult, None, [po_, wgt], [yacc])
                            else:
                                K.stt("vector", ysl, po_[:, 0:512], wgt[:, tile_, e_:e_ + 1], ysl, ALU.mult, ALU.add, [po_, wgt, yacc], [yacc])

                items = [(e_, tch) for e_ in range(moe_experts) for tch in range(HT // 4)]
                for i_, it_ in enumerate(items):
                    moe_up(it_)
                    if i_ >= 1:
                        moe_down(items[i_ - 1])
                moe_down(items[-1])
                for j in range(HT):
                    tt_ = half * HT + j
                    xt = xm[xi[0] % 2]; xi[0] += 1
                    K.dma("sync", xt[:], x1_d[tt_ * 128:(tt_ + 1) * 128, :], [x1_tok], [xt])
                    K.tt("gpsimd", xt[:], xt[:], yacc[:, j, :], ALU.add, [xt, yacc], [xt])
                    K.memset("vector", s2[:, 12:13], 0.0, [s2])
                    K.act(sq2[:], xt[:], ACT.Square, [xt, s2], [sq2, s2], accum_out=s2[:, 12:13])
                    K.act(s2[:, 13:14], s2[:, 12:13], ACT.Sqrt, [s2, cpi], [s2], scale=1.0 / D, bias=cpi[:, 1:2])
                    K.recip(s2[:, 14:15], s2[:, 13:14], [s2], [s2])
                    K.stt("vector", xn[:], xt[:], s2[:, 14:15], gfin[:], ALU.mult, ALU.mult, [xt, s2, gfin], [xn])
                    K.dma("sync", out_d[tt_ * 128:(tt_ + 1) * 128, :], xn[:], [xn], [])
            P.emit()
        P.emit(final=True)
    return nc, K


def _consts():
    p = np.arange(128)
    invf = np.stack([10000.0 ** (-(p % 32).astype(np.float32) / 32.0),
                     10000.0 ** (-(p % 16).astype(np.float32) / 16.0)], axis=1).astype(np.float32)
    selb = np.zeros((64, 64), np.float32)
    for c in range(64):
        for j in range(64):
            if j > c:
                selb[c, j] = -100.0
            elif j == 0 or j == c or j == c - 1:
                selb[c, j] = 100.0
    esel = np.zeros((64, 32, 128), np.float32)
    for i in range(32):
        for s_ in range(128):
            esel[2 * i + s_ // 64, i, s_] = 1.0
    return invf, selb, esel.reshape(64, 4096), np.eye(24, dtype=np.float32)


def make_in_map(inp, b):
    invf, selb, esel, eye = _consts()
    f = lambda a: np.ascontiguousarray(np.asarray(a))
    return {
        "x": f(inp["x"][b]), "positions": f(inp["positions"][b][None, :]),
        "norm_mix": f(np.asarray(inp["norm_mix"][0]).reshape(8, 128).T), "w_in": f(inp["w_in"][0]),
        "pe_k": f(inp["pe_k"][0]), "w1_k": f(inp["w1_k"][0]), "w2_k": f(inp["w2_k"][0]),
        "pe_v": f(inp["pe_v"][0]), "w1_v": f(inp["w1_v"][0]), "w2_v": f(inp["w2_v"][0]),
        "w_br_a": f(inp["w_br_a"][0]), "w_br_b": f(inp["w_br_b"][0]), "w_out": f(inp["w_out"][0]),
        "norm_ffn": f(np.asarray(inp["norm_ffn"][0]).reshape(8, 128).T),
        "w_group": f(inp["w_group"][0]), "b_group": f(inp["b_group"][0][None, :]),
        "w_expert": f(inp["w_expert"][0]), "b_expert": f(inp["b_expert"][0][None, :]),
        "w_gate_up": f(inp["w_gate_up"][0]), "w_down": f(inp["w_down"][0]),
        "norm_final": f(inp["norm_final"][None, :]),
        "c_invf": invf, "c_selb": selb, "c_esel": esel, "c_eye": eye,
    }


def kernel(**inputs):
    nc, K = build()
    in_maps = [make_in_map(inputs, b) for b in range(8)]
    res = run_bass_kernel_spmd(nc, in_maps, core_ids=list(range(8)))
    return np.stack([np.asarray(r["out"]) for r in res.results], axis=0).astype(np.float32)
```

```python
from contextlib import ExitStack
import numpy as np
import concourse.bass as bass
import concourse.mybir as mybir
from concourse.bass_utils import run_bass_kernel_spmd

F32 = mybir.dt.float32
BF16 = mybir.dt.bfloat16
I32 = mybir.dt.int32
ALU = mybir.AluOpType
ACT = mybir.ActivationFunctionType
AX = mybir.AxisListType

ENGS = ("tensor", "vector", "scalar", "gpsimd", "sync")

PIPE_DEPTH = 2
NEB = 6
PAIR = 1
L = 4096
D = 1024
NT = 32
NCH = 8
EPS = 1e-6
PI = float(np.pi)
TWO_PI = float(2 * np.pi)

QA, KA, VA, QI, KI, WI, QB = 0, 512, 576, 640, 896, 928, 936
KC, VC, KS, VS, KW, VW, GB, GA, GBT = 1448, 1576, 1704, 1832, 1960, 2088, 2216, 2240, 3264


class Tok:
    __slots__ = ("last_w", "readers")

    def __init__(self):
        self.last_w = None
        self.readers = []


class Op:
    __slots__ = ("eng", "fn", "deps", "is_dma", "sig", "signal", "idx", "prev")

    def __init__(self, eng, fn, is_dma):
        self.eng = eng
        self.fn = fn
        self.deps = set()
        self.is_dma = is_dma
        self.sig = None
        self.signal = False
        self.prev = None


class Prog:
    N_DMA_SEMS = 8

    def __init__(self, nc, ctx):
        self.nc = nc
        self.ops = []
        self.done = 0
        self.eng_sems = {e: ctx.enter_context(nc.semaphore(f"s_{e}")) for e in ENGS}
        self.dma_sems = {e: [ctx.enter_context(nc.semaphore(f"d_{e}_{i}")) for i in range(self.N_DMA_SEMS)]
                         for e in ENGS}
        self.eng_cnt = {e: 0 for e in ENGS}
        self.dma_rr = {e: 0 for e in ENGS}
        self.dma_cnt = {e: [0] * self.N_DMA_SEMS for e in ENGS}

    def _add(self, eng, fn, reads, writes, is_dma=False):
        op = Op(eng, fn, is_dma)
        op.idx = len(self.ops)
        for t in reads:
            if t.last_w is not None:
                op.deps.add(t.last_w)
        for t in writes:
            if t.last_w is not None:
                op.deps.add(t.last_w)
            op.deps.update(t.readers)
        op.deps.discard(op.idx)
        for t in reads:
            t.readers.append(op.idx)
        for t in writes:
            t.last_w = op.idx
            t.readers = []
        self.ops.append(op)
        return op

    def op(self, eng, fn, reads=(), writes=()):
        return self._add(eng, fn, list(reads), list(writes))

    def dma(self, eng, out, in_, reads=(), writes=(), **kw):
        return self._add(eng, lambda e: e.dma_start(out=out, in_=in_, **kw), list(reads), list(writes), True)

    def _sem(self, key):
        return self.eng_sems[key[1]] if key[0] == "e" else self.dma_sems[key[1]][key[2]]

    def emit(self, final=False):
        nc = self.nc
        ops = self.ops
        new = ops[self.done:]
        pre = []
        for e in ENGS:
            if self.eng_cnt[e] > 0:
                pre.append((("e", e), self.eng_cnt[e]))
            for k in range(self.N_DMA_SEMS):
                if self.dma_cnt[e][k] > 0:
                    pre.append((("d", e, k), self.dma_cnt[e][k]))
        for op in new:
            for d in op.deps:
                if d >= self.done:
                    ops[d].signal = True
        per_eng = {e: [] for e in ENGS}
        for op in new:
            per_eng[op.eng].append(op)
        for e in ENGS:
            for op in reversed(per_eng[e]):
                if not op.is_dma:
                    op.signal = True
                    break
        for op in new:
            if op.is_dma:
                k = self.dma_rr[op.eng]
                self.dma_rr[op.eng] = (k + 1) % self.N_DMA_SEMS
                prev = self.dma_cnt[op.eng][k]
                self.dma_cnt[op.eng][k] = prev + 16
                op.sig = (("d", op.eng, k), prev + 16)
                op.prev = (("d", op.eng, k), prev)
            elif op.signal:
                self.eng_cnt[op.eng] += 1
                op.sig = (("e", op.eng), self.eng_cnt[op.eng])
        finals = []
        if final:
            for e in ENGS:
                for k in range(self.N_DMA_SEMS):
                    if self.dma_cnt[e][k] > 0:
                        finals.append((("d", e, k), self.dma_cnt[e][k]))
        done = self.done

        def run_engine(ename, eobj):
            known = {}
            for key, val in pre:
                if key == ("e", ename):
                    continue
                eobj.wait_ge(self._sem(key), val)
                known[key] = val
            for op in per_eng[ename]:
                waits = {}
                for d in op.deps:
                    if d < done:
                        continue
                    dop = ops[d]
                    key, val = dop.sig
                    if ename == "tensor" and dop.eng == "tensor" and not dop.is_dma:
                        continue
                    if known.get(key, 0) >= val:
                        continue
                    waits[key] = max(waits.get(key, 0), val)
                if op.is_dma:
                    key, val = op.prev
                    if val > 0 and known.get(key, 0) < val:
                        waits[key] = max(waits.get(key, 0), val)
                for key, val in waits.items():
                    eobj.wait_ge(self._sem(key), val)
                    known[key] = val
                ins = op.fn(eobj)
                if op.sig is not None:
                    ins.then_inc(self._sem(op.sig[0]), 16 if op.is_dma else 1)
            if ename == "sync":
                for key, val in finals:
                    if known.get(key, 0) < val:
                        eobj.wait_ge(self._sem(key), val)

        with nc.Block() as block:
            @block.tensor
            def _(e):
                run_engine("tensor", e)

            @block.vector
            def _(e):
                run_engine("vector", e)

            @block.scalar
            def _(e):
                run_engine("scalar", e)

            @block.gpsimd
            def _(e):
                run_engine("gpsimd", e)

            @block.sync
            def _(e):
                run_engine("sync", e)
        self.done = len(ops)


class Buf:
    def __init__(self, t):
        self.t = t
        self.k = Tok()

    def __getitem__(self, key):
        return self.t[key]


class View:
    def __init__(self, ap, k):
        self.ap = ap
        self.k = k

    def __getitem__(self, key):
        return self.ap[key]


class KB:
    def __init__(self, nc, dbg=None):
        self.nc = nc
        self.n = 0
        self.dbg = dbg if dbg is not None else {}
        self.dbg_out = {}

    def sb(self, ctx, shape, dt=F32):
        self.n += 1
        return Buf(ctx.enter_context(self.nc.sbuf_tensor(f"sb{self.n}", list(shape), dt)))

    def ps(self, ctx, shape, dt=F32):
        self.n += 1
        b = Buf(ctx.enter_context(self.nc.psum_tensor(f"ps{self.n}", list(shape), dt)))
        b.is_psum = True
        return b

    def dump(self, name, ap, shape, dt, reads):
        if name not in self.dbg:
            return
        d = self.nc.dram_tensor("dbg_" + name, list(shape), dt, kind="ExternalOutput").ap()
        self.dbg_out[name] = d
        self.P.dma("sync", d, ap, reads=reads)

    @staticmethod
    def _tk(lst):
        return [b.k if hasattr(b, "k") else b for b in lst]

    @staticmethod
    def _rw(r, w):
        rr_, ww_ = [], list(w)
        for b in r:
            if getattr(b, "is_psum", False):
                if b not in ww_:
                    ww_.append(b)
            else:
                rr_.append(b)
        tk = lambda lst: [b.k if hasattr(b, "k") else b for b in lst]
        return tk(rr_), tk(ww_)

    def mm(self, out, lhsT, rhs, start, stop, r, w):
        self.P.op("tensor", lambda e: e.matmul(out, lhsT=lhsT, rhs=rhs, start=start, stop=stop), *self._rw(r, w))

    def tr(self, out, in_, ident, r, w):
        self.P.op("tensor", lambda e: e.transpose(out=out, in_=in_, identity=ident), *self._rw(r, w))

    def act(self, out, in_, func, r, w, **kw):
        self.P.op("scalar", lambda e: e.activation(out=out, in_=in_, func=func, **kw), *self._rw(r, w))

    def tt(self, eng, out, in0, in1, op, r, w):
        self.P.op(eng, lambda e: e.tensor_tensor(out=out, in0=in0, in1=in1, op=op), *self._rw(r, w))

    def ts(self, eng, out, in0, s1, s2, op0, op1, r, w, accum_out=None):
        if op1 is None:
            self.P.op(eng, lambda e: e.tensor_scalar(out=out, in0=in0, scalar1=s1, scalar2=None, op0=op0), *self._rw(r, w))
        elif accum_out is None:
            self.P.op(eng, lambda e: e.tensor_scalar(out=out, in0=in0, scalar1=s1, scalar2=s2, op0=op0, op1=op1), *self._rw(r, w))
        else:
            self.P.op(eng, lambda e: e.tensor_scalar(out=out, in0=in0, scalar1=s1, scalar2=s2, op0=op0, op1=op1, accum_out=accum_out), *self._rw(r, w))

    def stt(self, eng, out, in0, scalar, in1, op0, op1, r, w):
        self.P.op(eng, lambda e: e.scalar_tensor_tensor(out=out, in0=in0, scalar=scalar, in1=in1, op0=op0, op1=op1), *self._rw(r, w))

    def cp(self, eng, out, in_, r, w):
        if eng == "scalar":
            self.P.op(eng, lambda e: e.copy(out=out, in_=in_), *self._rw(r, w))
        else:
            self.P.op(eng, lambda e: e.tensor_copy(out=out, in_=in_), *self._rw(r, w))

    def memset(self, eng, ap, val, w):
        self.P.op(eng, lambda e: e.memset(ap, val), [], self._tk(w))

    def asel(self, out, in_, pattern, cmp, fill, base, cm, r, w):
        self.P.op("gpsimd", lambda e: e.affine_select(out=out, in_=in_, pattern=pattern, compare_op=cmp, fill=fill, base=base, channel_multiplier=cm), *self._rw(r, w))

    def recip(self, out, in_, r, w):
        self.P.op("vector", lambda e: e.reciprocal(out=out, in_=in_), *self._rw(r, w))

    def dma(self, eng, out, in_, r, w, **kw):
        r_, w_ = self._rw(r, w)
        self.P.dma(eng, out, in_, r_, w_, **kw)


def bc(ap, shape):
    return ap.to_broadcast(list(shape))


def build(dbg=None, qtiles=None, stop_after=None, moe_experts=32, lvl=99, moe_from_x=False, skip_att=False):
    nc = bass.Bass("TRN2", target_bir_lowering=False)
    K = KB(nc, dbg)

    def din(name, shape, dt=F32):
        return nc.dram_tensor(name, list(shape), dt, kind="ExternalInput").ap()

    x_d = din("x", [L, D])
    pos_d = din("positions", [1, L], I32)
    norm_mix_d = din("norm_mix", [128, 8])
    w_in_d = din("w_in", [D, 4288])
    pe_k_d = din("pe_k", [32, 64]); w1_k_d = din("w1_k", [2048, 128]); w2_k_d = din("w2_k", [128, 64])
    pe_v_d = din("pe_v", [32, 64]); w1_v_d = din("w1_v", [2048, 128]); w2_v_d = din("w2_v", [128, 64])
    w_br_a_d = din("w_br_a", [512, D]); w_br_b_d = din("w_br_b", [512, D]); w_out_d = din("w_out", [D, D])
    norm_ffn_d = din("norm_ffn", [128, 8])
    w_group_d = din("w_group", [D, 4]); b_group_d = din("b_group", [1, 4])
    w_expert_d = din("w_expert", [D, 32]); b_expert_d = din("b_expert", [1, 32])
    w_gu_d = din("w_gate_up", [32, D, 512]); w_dn_d = din("w_down", [32, 256, D])
    norm_final_d = din("norm_final", [1, D])
    c_invf_d = din("c_invf", [128, 2])
    c_selb_d = din("c_selb", [64, 64])
    c_esel_d = din("c_esel", [64, 4096])
    c_eye_d = din("c_eye", [24, 24])
    out_d = nc.dram_tensor("out", [L, D], F32, kind="ExternalOutput").ap()
    x1_d = nc.dram_tensor("x1s", [L, D], F32, kind="Internal").ap()
    NG_Q = 40
    wq_d = nc.dram_tensor("wq_bf", [NG_Q, 128, 1024], BF16, kind="Internal").ap()

    w_in_v = w_in_d.rearrange("(c p) n -> p c n", p=128)
    wq_tok = Tok()
    x1_tok = Tok()

    with ExitStack() as top:
        P = Prog(nc, top)
        K.P = P
        ident = K.sb(top, [128, 128], BF16)
        diagT = K.sb(top, [128, 128], BF16)
        antiT = K.sb(top, [128, 128], BF16)
        invf = K.sb(top, [128, 2])
        gmix = K.sb(top, [128, 8])
        cpi = K.sb(top, [128, 2])
        with ExitStack() as c0:
            tmpf = K.sb(c0, [128, 128])
            K.memset("gpsimd", tmpf[:], 1.0, [tmpf])
            K.asel(tmpf[:], tmpf[:], [[-1, 128]], ALU.is_equal, 0.0, 0, 1, [tmpf], [tmpf])
            K.cp("vector", ident[:], tmpf[:], [tmpf], [ident])
            tmp2 = K.sb(c0, [128, 128])
            K.memset("gpsimd", tmp2[:], 1.0, [tmp2])
            K.asel(tmp2[:], tmp2[:], [[1, 128]], ALU.is_ge, 0.0, 0, -1, [tmp2], [tmp2])
            K.cp("vector", diagT[:], tmp2[:], [tmp2], [diagT])
            tmp3 = K.sb(c0, [128, 128])
            K.memset("gpsimd", tmp3[:], 1.0, [tmp3])
            K.asel(tmp3[:], tmp3[:], [[-1, 128]], ALU.is_gt, 0.0, 0, 1, [tmp3], [tmp3])
            K.cp("vector", antiT[:], tmp3[:], [tmp3], [antiT])
            K.dma("sync", invf[:], c_invf_d, [], [invf])
            K.dma("sync", gmix[:], norm_mix_d, [], [gmix])
            K.memset("vector", cpi[:, 0:1], PI / 2, [cpi])
            K.memset("vector", cpi[:, 1:2], EPS, [cpi])
            P.emit()

        banks = [K.ps(top, [128, 512]) for _ in range(8)]
        rr = [0]

        def bank(pool=(0, 1, 2, 3)):
            b = banks[pool[rr[0] % len(pool)]]
            rr[0] += 1
            return b

        def bfv(b):
            return b.t[:].bitcast(BF16)

        def make_hT(c, hTc, xb, sq, hn, st, g):
            for j in range(4):
                tt_ = 4 * c + j
                xt = xb[j % len(xb)]
                K.dma("sync", xt[:], x_d[tt_ * 128:(tt_ + 1) * 128, :], [], [xt])
                K.memset("vector", st[:, 0:1], 0.0, [st])
                K.act(sq[:], xt[:], ACT.Square, [xt, st], [sq, st], accum_out=st[:, 0:1])
                K.act(st[:, 1:2], st[:, 0:1], ACT.Sqrt, [st, cpi], [st], scale=1.0 / D, bias=cpi[:, 1:2])
                K.recip(st[:, 2:3], st[:, 1:2], [st], [st])
                K.ts("vector", hn[:], xt[:], st[:, 2:3], None, ALU.mult, None, [xt, st], [hn])
                pb = bank()
                for k in range(8):
                    K.tr(bfv(pb)[:, k * 128:(k + 1) * 128], hn[:, k * 128:(k + 1) * 128], ident[:], [hn, ident], [pb])
                K.tt("vector", hTc[:, :, j * 128:(j + 1) * 128], bfv(pb).rearrange("p (k t) -> p k t", k=8),
                     bc(g[:, :].unsqueeze(2), [128, 8, 128]), ALU.mult, [pb, g], [hTc])

        def make_rope(c, posi, posf, tq, tabs):
            K.dma("sync", posi[:], pos_d[:, c * 512:(c + 1) * 512].partition_broadcast(128), [], [posi])
            K.cp("vector", posf[:], posi[:], [posi], [posf])
            for col, (cn, sn) in enumerate((("cos64", "sin64"), ("cos32", "sin32"))):
                ang = tq[0]; kf = tq[1]; ki = tq[2]
                K.ts("vector", ang[:], posf[:], invf[:, col:col + 1], None, ALU.mult, None, [posf, invf], [ang])
                K.ts("vector", ki[:], ang[:], 1.0 / TWO_PI, None, ALU.mult, None, [ang], [ki])
                K.cp("vector", kf[:], ki[:], [ki], [kf])
                K.stt("vector", ang[:], kf[:], -TWO_PI, ang[:], ALU.mult, ALU.add, [kf, ang], [ang])
                K.ts("vector", kf[:], ang[:], PI, -TWO_PI, ALU.is_gt, ALU.mult, [ang], [kf])
                K.tt("vector", ang[:], ang[:], kf[:], ALU.add, [ang, kf], [ang])
                K.ts("vector", kf[:], ang[:], -PI, TWO_PI, ALU.is_lt, ALU.mult, [ang], [kf])
                K.tt("vector", ang[:], ang[:], kf[:], ALU.add, [ang, kf], [ang])
                K.act(tabs[sn][:], ang[:], ACT.Sin, [ang], [tabs[sn]])
                K.stt("vector", kf[:], ang[:], -1.0, ang[:], ALU.mult, ALU.max, [ang], [kf])
                K.act(tabs[cn][:], kf[:], ACT.Sin, [kf, cpi], [tabs[cn]], scale=-1.0, bias=cpi[:, 0:1])

        def proj_fm(hTc, wA, M, dst, dstb, wB=None, cos=None, sin=None, tmp=None, evac="scalar", func=None, N=512):
            pa = bank()
            for k in range(8):
                K.mm(pa[0:M, 0:N], wA[0][:, k, :], hTc[:, k, 0:N], k == 0, k == 7, [wA[1], hTc], [pa])
            if wB is None:
                if func is not None:
                    K.act(dst, pa[0:M, 0:N], func, [pa], [dstb])
                else:
                    K.cp(evac, dst, pa[0:M, 0:N], [pa], [dstb])
                return
            pb = bank()
            for k in range(8):
                K.mm(pb[0:M, 0:N], wB[0][:, k, :], hTc[:, k, 0:N], k == 0, k == 7, [wB[1], hTc], [pb])
            t1, t2 = tmp
            K.tt("vector", t1[0:M, 0:N], pa[0:M, 0:N], cos[0:M, 0:N], ALU.mult, [pa, cos], [t1])
            K.tt("vector", t2[0:M, 0:N], pb[0:M, 0:N], sin[0:M, 0:N], ALU.mult, [pb, sin], [t2])
            K.tt("gpsimd", dst, t1[0:M, 0:N], t2[0:M, 0:N], ALU.add, [t1, t2], [dstb])

        att = ExitStack()
        kaT2 = K.sb(att, [128, L], BF16)
        kiT3 = K.sb(att, [96, L], BF16)
        ksT = K.sb(att, [128, L], BF16)
        kwT = K.sb(att, [128, L], BF16)
        va3 = K.sb(att, [128, NT, 192], BF16)
        vs3 = K.sb(att, [128, NT, 192], BF16)
        vw3 = K.sb(att, [128, NT, 192], BF16)
        kcT2 = K.sb(att, [128, 256], BF16)
        vctm = K.sb(att, [128, 2, 128], BF16)

        with ExitStack() as pw:
            stg = [K.sb(pw, [128, 8, 512]) for _ in range(2)]
            grp = [K.sb(pw, [128, 8, 128], BF16) for _ in range(3)]
            wBfm = K.sb(pw, [128, 8, 1248], BF16)
            wBtm = K.sb(pw, [128, 8, 320], BF16)
            gi = [0]

            def load_seg(i, c0, n):
                s = stg[i % 2]
                K.dma("sync", s[:, :, 0:n], w_in_v[:, :, c0:c0 + n], [], [s])
                return s

            def rot_into(dst_ap_fn, dstb, s, off, half):
                K.ts("vector", dst_ap_fn(0, half), s[:, :, off + half:off + 2 * half], -1.0, None, ALU.mult, None, [s], [dstb])
                K.cp("gpsimd", dst_ap_fn(half, 2 * half), s[:, :, off:off + half], [s], [dstb])

            def store_grp(gidx, g):
                K.dma("sync", wq_d[gidx].rearrange("p (c m) -> p c m", c=8), g[:], [g], [wq_tok])

            def new_grp():
                g = grp[gi[0] % 3]
                gi[0] += 1
                return g

            s = load_seg(0, QA, 512)
            for g_ in range(4):
                g = new_grp()
                for u in range(2):
                    h = 4 * u + g_
                    K.cp("scalar", g[:, :, u * 64:(u + 1) * 64], s[:, :, h * 64:(h + 1) * 64], [s], [g])
                store_grp(g_, g)
                g = new_grp()
                for u in range(2):
                    h = 4 * u + g_
                    rot_into(lambda a, b, u=u, g=g: g[:, :, u * 64 + a:u * 64 + b], g, s, h * 64, 32)
                store_grp(4 + g_, g)
            s = load_seg(1, 512, 424)
            o_ka, o_va, o_qi, o_ki, o_wi = 0, 64, 128, 384, 416
            BF_KA_A, BF_KA_B, BF_KI_A, BF_KI_B, BF_KS_A, BF_KS_B, BF_KW_A, BF_KW_B, BF_KC, BF_VC = \
                0, 128, 256, 352, 448, 576, 704, 832, 960, 1088
            for u in range(2):
                K.cp("scalar", wBfm[:, :, BF_KA_A + u * 64:BF_KA_A + (u + 1) * 64], s[:, :, o_ka:o_ka + 64], [s], [wBfm])
                rot_into(lambda a, b, u=u: wBfm[:, :, BF_KA_B + u * 64 + a:BF_KA_B + u * 64 + b], wBfm, s, o_ka, 32)
            for r_ in range(3):
                K.cp("scalar", wBfm[:, :, BF_KI_A + r_ * 32:BF_KI_A + (r_ + 1) * 32], s[:, :, o_ki:o_ki + 32], [s], [wBfm])
                rot_into(lambda a, b, r_=r_: wBfm[:, :, BF_KI_B + r_ * 32 + a:BF_KI_B + r_ * 32 + b], wBfm, s, o_ki, 16)
            K.cp("scalar", wBtm[:, :, 0:64], s[:, :, o_va:o_va + 64], [s], [wBtm])
            for q_ in range(3):
                hs = [3 * q_ + i for i in range(3) if 3 * q_ + i < 8]
                g = new_grp()
                K.memset("vector", g[:], 0.0, [g])
                for i, h in enumerate(hs):
                    K.cp("scalar", g[:, :, i * 32:(i + 1) * 32], s[:, :, o_qi + h * 32:o_qi + (h + 1) * 32], [s], [g])
                store_grp(8 + q_, g)
                g = new_grp()
                K.memset("vector", g[:], 0.0, [g])
                for i, h in enumerate(hs):
                    rot_into(lambda a, b, i=i, g=g: g[:, :, i * 32 + a:i * 32 + b], g, s, o_qi + h * 32, 16)
                store_grp(11 + q_, g)
            s = load_seg(0, QB, 512)
            for g_ in range(4):
                g = new_grp() if True else None
                for u in range(2):
                    h = 4 * u + g_
                    K.cp("scalar", g[:, :, u * 64:(u + 1) * 64], s[:, :, h * 64:(h + 1) * 64], [s], [g])
                store_grp(14 + g_, g)
                g = new_grp()
                for u in range(2):
                    h = 4 * u + g_
                    rot_into(lambda a, b, u=u, g=g: g[:, :, u * 64 + a:u * 64 + b], g, s, h * 64, 32)
                store_grp(18 + g_, g)
            s = load_seg(1, KC, 512)
            K.cp("scalar", wBfm[:, :, BF_KC:BF_KC + 128], s[:, :, 0:128], [s], [wBfm])
            K.cp("scalar", wBfm[:, :, BF_VC:BF_VC + 128], s[:, :, 128:256], [s], [wBfm])
            K.cp("scalar", wBfm[:, :, BF_KS_A:BF_KS_A + 128], s[:, :, 256:384], [s], [wBfm])
            for u in range(2):
                rot_into(lambda a, b, u=u: wBfm[:, :, BF_KS_B + u * 64 + a:BF_KS_B + u * 64 + b], wBfm, s, 256 + u * 64, 32)
            K.cp("scalar", wBtm[:, :, 64:192], s[:, :, 384:512], [s], [wBtm])
            s = load_seg(0, KW, 280)
            K.cp("scalar", wBfm[:, :, BF_KW_A:BF_KW_A + 128], s[:, :, 0:128], [s], [wBfm])
            for u in range(2):
                rot_into(lambda a, b, u=u: wBfm[:, :, BF_KW_B + u * 64 + a:BF_KW_B + u * 64 + b], wBfm, s, u * 64, 32)
            K.cp("scalar", wBtm[:, :, 192:320], s[:, :, 128:256], [s], [wBtm])
            gG = K.sb(pw, [128, 8, 128], BF16)
            K.memset("vector", gG[:], 0.0, [gG])
            K.cp("scalar", gG[:, :, 0:24], s[:, :, 256:280], [s], [gG])
            wis = K.sb(pw, [128, 8, 8])
            K.dma("sync", wis[:], w_in_v[:, :, WI:WI + 8], [], [wis])
            K.cp("scalar", gG[:, :, 24:32], wis[:], [wis], [gG])
            store_grp(38, gG)
            for half in range(4):
                s = load_seg(half + 1, GA + half * 512, 512)
                for q_ in range(4):
                    g = new_grp()
                    K.cp("scalar" if q_ % 2 == 0 else "vector", g[:], s[:, :, q_ * 128:(q_ + 1) * 128], [s], [g])
                    store_grp(22 + half * 4 + q_, g)

            xb = [K.sb(pw, [128, D]) for _ in range(2)]
            sq = K.sb(pw, [128, D], BF16)
            hn = K.sb(pw, [128, D], BF16)
            st = K.sb(pw, [128, 4])
            hTc = K.sb(pw, [128, 8, 512], BF16)
            posi = K.sb(pw, [128, 512], I32)
            posf = K.sb(pw, [128, 512])
            tq = [K.sb(pw, [128, 512]), K.sb(pw, [128, 512]), K.sb(pw, [128, 512], I32)]
            tabs = {n: K.sb(pw, [128, 512], BF16) for n in ("cos64", "sin64", "cos32", "sin32")}
            rt = (K.sb(pw, [128, 512]), K.sb(pw, [128, 512]))
            kcmpT = K.sb(pw, [128, L], BF16)
            vcmpT = K.sb(pw, [128, L], BF16)
            for v3 in (va3, vs3, vw3):
                K.memset("gpsimd", v3[:, :, 64:128], 1.0, [v3])
            for c in range(NCH):
                make_hT(c, hTc, xb, sq, hn, st, gmix)
                make_rope(c, posi, posf, tq, tabs)
                cs = slice(c * 512, (c + 1) * 512)
                W = lambda off, m: (wBfm[:, :, off:off + m], wBfm)
                proj_fm(hTc, W(BF_KA_A, 128), 128, kaT2[:, cs], kaT2, W(BF_KA_B, 128), tabs["cos64"], tabs["sin64"], rt)
                proj_fm(hTc, W(BF_KI_A, 96), 96, kiT3[:, cs], kiT3, W(BF_KI_B, 96), tabs["cos32"], tabs["sin32"], rt)
                proj_fm(hTc, W(BF_KS_A, 128), 128, ksT[:, cs], ksT, W(BF_KS_B, 128), tabs["cos64"], tabs["sin64"], rt)
                proj_fm(hTc, W(BF_KW_A, 128), 128, kwT[:, cs], kwT, W(BF_KW_B, 128), tabs["cos64"], tabs["sin64"], rt)
                proj_fm(hTc, W(BF_KC, 128), 128, kcmpT[:, cs], kcmpT)
                proj_fm(hTc, W(BF_VC, 128), 128, vcmpT[:, cs], vcmpT, evac="vector")
                for j in range(4):
                    tt_ = 4 * c + j
                    pv = bank()
                    for k in range(8):
                        K.mm(pv[:, 0:320], hTc[:, k, j * 128:(j + 1) * 128], wBtm[:, k, :], k == 0, k == 7, [hTc, wBtm], [pv])
                    K.cp("scalar", va3[:, tt_, 0:64], pv[:, 0:64], [pv], [va3])
                    K.cp("vector", va3[:, tt_, 128:192], pv[:, 0:64], [pv], [va3])
                    K.cp("scalar", vs3[:, tt_, 0:64], pv[:, 64:128], [pv], [vs3])
                    K.cp("vector", vs3[:, tt_, 128:192], pv[:, 128:192], [pv], [vs3])
                    K.cp("scalar", vw3[:, tt_, 0:64], pv[:, 192:256], [pv], [vw3])
                    K.cp("vector", vw3[:, tt_, 128:192], pv[:, 256:320], [pv], [vw3])
            K.dump("kaT2", kaT2[:], [128, L], BF16, [kaT2.k])
            K.dump("kiT3", kiT3[:], [96, L], BF16, [kiT3.k])
            K.dump("ksT", ksT[:], [128, L], BF16, [ksT.k])
            K.dump("kwT", kwT[:], [128, L], BF16, [kwT.k])
            K.dump("va3", va3[:].rearrange("p a b -> p (a b)"), [128, NT * 192], BF16, [va3.k])
            K.dump("vs3", vs3[:].rearrange("p a b -> p (a b)"), [128, NT * 192], BF16, [vs3.k])

            w1 = K.sb(pw, [128, 32, 128], BF16)
            w2d = K.sb(pw, [128, 128], BF16)
            peT = K.sb(pw, [64, 32], BF16)
            hid = K.sb(pw, [128, 256], BF16)
            cb = K.sb(pw, [128, 1])
            K.memset("vector", kcT2[:], 0.0, [kcT2])
            for kind, (pe_d, w1_d, w2_d, srcT) in enumerate(((pe_k_d, w1_k_d, w2_k_d, kcmpT), (pe_v_d, w1_v_d, w2_v_d, vcmpT))):
                w1v = w1_d.rearrange("(j d) c -> d j c", d=64)
                K.dma("gpsimd", w1[0:64, :, :], w1v, [], [w1])
                K.dma("gpsimd", w1[64:128, :, :], w1v, [], [w1])
                K.dma("gpsimd", w2d[:, 0:64], w2_d, [], [w2d])
                K.dma("gpsimd", w2d[:, 64:128], w2_d, [], [w2d])
                K.dma("gpsimd", peT[:], pe_d.rearrange("j d -> d j"), [], [peT], allow_slow_non_contiguous=True)
                pbias = bank()
                for j in range(32):
                    K.mm(pbias[:, 0:1], w1[0:64, j, :], peT[:, j:j + 1], j == 0, j == 31, [w1, peT], [pbias])
                K.cp("vector", cb[:], pbias[:, 0:1], [pbias], [cb])
                for kk in range(2):
                    ph = bank()
                    lo = 64 * kk
                    for j in range(32):
                        K.mm(ph[:, 0:255], w1[lo:lo + 64, j, :], srcT[lo:lo + 64, j:j + 16 * 254 + 1:16],
                             j == 0, j == 31, [w1, srcT], [ph])
                    K.memset("vector", hid[:, 255:256], 0.0, [hid])
                    K.act(hid[:, 0:255], ph[:, 0:255], ACT.Silu, [ph, cb], [hid], bias=cb[:, 0:1])
                    if kind == 0:
                        po = bank()
                        K.mm(po[:, 0:256], w2d[:], hid[:], True, True, [w2d, hid], [po])
                        K.cp("vector", kcT2[lo:lo + 64, :], po[lo:lo + 64, 0:256], [po], [kcT2])
                    else:
                        for ch in range(2):
                            po = bank()
                            K.mm(po[:, 0:64], hid[:, ch * 128:(ch + 1) * 128], w2d[:, 0:64], True, True, [hid, w2d], [po])
                            K.cp("vector", vctm[:, ch, lo:lo + 64], po[:, 0:64], [po], [vctm])
            K.dump("kcT2", kcT2[:], [128, 256], BF16, [kcT2.k])
            K.dump("vctm", vctm[:].rearrange("p a b -> p (a b)"), [128, 256], BF16, [vctm.k])
            P.emit()
        if stop_after == "B":
            P.emit(final=True)
            att.close()
            return nc, K
        with ExitStack() as pc:
            qt_list = list(range(NT)) if qtiles is None else list(qtiles)
            ch_list = sorted(set(q // 4 for q in qt_list))
            RA = K.sb(pc, [128, L])
            idx = RA
            wout = View(RA.t[:].bitcast(BF16).rearrange("p (c f) -> p c f", c=8), RA.k)
            RBm = K.sb(pc, [128, L], BF16)
            mask = RBm
            wbra = View(RBm.t[:].rearrange("p (g f) -> p g f", g=4), RBm.k)
            RC = K.sb(pc, [128, NT, 128], BF16)
            maskT = RC
            wbrb = View(RC.t[:].rearrange("p a b -> p (a b)").rearrange("p (g f) -> p g f", g=4), RC.k)
            RD = K.sb(pc, [128, 8, 256])
            Ecmp = RD
            mergedT = View(RD.t[:].rearrange("p a b -> p (a b)").bitcast(BF16).rearrange("p (c t) -> p c t", c=8), RD.k)
            RE = K.sb(pc, [128, 2560])
            kEa, kEb, kEc = Tok(), Tok(), Tok()
            posi = View(RE.t[:, 0:512].bitcast(I32), kEa)
            posf = View(RE.t[:, 512:1024], kEa)
            ang_ = View(RE.t[:, 1024:1536], kEb)
            kf_ = View(RE.t[:, 1536:2048], kEb)
            ki_ = View(RE.t[:, 2048:2560].bitcast(I32), kEc)
            p_bf = View(RE.t[:, 0:1024].bitcast(BF16).rearrange("p (h n) -> p h n", h=8), kEa)
            pT = View(RE.t[:, 1024:2048].bitcast(BF16).rearrange("p (c h t) -> p c h t", c=2, h=8), kEb)
            R0 = View(RE.t[:, 2048:2560], kEc)
            R1 = K.sb(pc, [128, 512])
            ob = K.sb(pc, [128, 512])
            rt2 = (R1, ob)
            xb = [K.sb(pc, [128, D])]
            hn = K.sb(pc, [128, D], BF16)
            st = K.sb(pc, [128, 4])
            hTc = K.sb(pc, [128, 8, 512], BF16)
            tabs = {n: K.sb(pc, [128, 512], BF16) for n in ("cos64", "sin64", "cos32", "sin32")}
            ws = [K.sb(pc, [128, 8, 128], BF16) for _ in range(2)]
            wG = K.sb(pc, [128, 8, 128], BF16)
            qaT = K.sb(pc, [128, 4, 512], BF16)
            qiT = K.sb(pc, [96, 3, 512], BF16)
            qnT = K.sb(pc, [128, 4, 512], BF16)
            qrT = K.sb(pc, [128, 4, 512], BF16)
            gT = K.sb(pc, [32, 512], BF16)
            oaTc = K.sb(pc, [128, 4, 512], BF16)
            obTc = K.sb(pc, [128, 4, 512], BF16)
            Eb = [K.sb(pc, [128, 512], BF16) for _ in range(NEB)]
            Pb = [K.sb(pc, [128, 512], BF16) for _ in range(NEB)]
            Esel = K.sb(pc, [64, 32, 128], BF16)
            eye24 = K.sb(pc, [24, 24], BF16)
            ones24 = K.sb(pc, [24, 128], BF16)
            Dg = K.sb(pc, [24, 8, 128], BF16)
            gBs = [K.sb(pc, [128, 512], BF16) for _ in range(2)]
            rs = K.sb(pc, [128, 512])
            sm = K.sb(pc, [128, 64])
            wi_sb2 = [K.sb(pc, [128, 8]) for _ in range(2)]
            smb2 = [K.sb(pc, [128, 32]) for _ in range(2)]
            P4 = K.sb(pc, [128, 2, 256])
            imp = K.sb(pc, [128, 2, 64])
            scs = K.sb(pc, [128, 2, 64])
            sc2 = K.sb(pc, [128, 64])
            selb = K.sb(pc, [128, 64])
            bm = K.sb(pc, [128, 2, 64], BF16)
            bmT = K.sb(pc, [64, 2, 128], BF16)
            mexp = [K.sb(pc, [128, 2, 128], BF16) for _ in range(4)]
            er = [0]

            def Enext():
                er[0] += 1
                return Eb[er[0] % NEB], Pb[er[0] % NEB]

            def pipe(units, qk_fn, pv_fn, depth=PIPE_DEPTH, hook=None):
                if PAIR and len(units) >= 2 and hasattr(qk_fn, "mm"):
                    sis = []
                    for un in units:
                        if un[0] not in sis:
                            sis.append(un[0])
                    pend = []
                    for si in sis:
                        sts = [qk_fn.mm((si, u)) for u in range(2)]
                        outs = [qk_fn.post((si, u), sts[u]) for u in range(2)]
                        pend.append((si, outs))
                        if hook is not None:
                            hook(); hook()
                        if len(pend) > 2:
                            si0, o0 = pend.pop(0)
                            for u in range(2):
                                pv_fn((si0, u), o0[u])
                    for si0, o0 in pend:
                        for u in range(2):
                            pv_fn((si0, u), o0[u])
                    return
                pend = []
                for un in units:
                    pend.append((un, qk_fn(un)))
                    if hook is not None:
                        hook()
                    if len(pend) > depth:
                        pv_fn(*pend.pop(0))
                for p_ in pend:
                    pv_fn(*p_)

            with ExitStack() as cc:
                K.dma("gpsimd", Esel[:].rearrange("p a b -> p (a b)"), c_esel_d, [], [Esel])
                K.dma("gpsimd", eye24[:], c_eye_d, [], [eye24])
                K.memset("vector", ones24[:], 1.0, [ones24])
                K.memset("vector", sm[:, 32:33], 0.5, [sm])
                P.emit()

            def load_ws(gidx, i):
                w = ws[i % 2]
                K.dma("sync", w[:], wq_d[gidx].rearrange("p (c m) -> p c m", c=8), [wq_tok], [w])
                return w

            A_banks = (banks[4], banks[5])
            B_banks = (banks[6], banks[7])
            SC = 0.125
            wsi = [0]

            for c in ch_list:
                make_hT(c, hTc, xb, hn, hn, st, gmix)
                make_rope(c, posi, posf, (ang_, kf_, ki_), tabs)
                if lvl < 0.2:
                    continue
                for g_ in range(4):
                    wA = load_ws(g_, wsi[0]); wsi[0] += 1
                    wB = load_ws(4 + g_, wsi[0]); wsi[0] += 1
                    proj_fm(hTc, (wA[:], wA), 128, qaT[:, g_, :], qaT, (wB[:], wB), tabs["cos64"], tabs["sin64"], rt2)
                for q_ in (range(3) if lvl >= 0.5 else []):
                    wA = load_ws(8 + q_, wsi[0]); wsi[0] += 1
                    wB = load_ws(11 + q_, wsi[0]); wsi[0] += 1
                    proj_fm(hTc, (wA[:, :, 0:96], wA), 96, qiT[:, q_, :], qiT, (wB[:, :, 0:96], wB), tabs["cos32"], tabs["sin32"], rt2)
                for g_ in (range(4) if lvl >= 0.75 else []):
                    wA = load_ws(14 + g_, wsi[0]); wsi[0] += 1
                    wB = load_ws(18 + g_, wsi[0]); wsi[0] += 1
                    pa = bank(); pb = bank()
                    for k in range(8):
                        K.mm(pa[:, 0:512], wA[:, k, :], hTc[:, k, :], k == 0, k == 7, [wA, hTc], [pa])
                    for k in range(8):
                        K.mm(pb[:, 0:512], wB[:, k, :], hTc[:, k, :], k == 0, k == 7, [wB, hTc], [pb])
                    K.cp("scalar", qnT[:, g_, :], pa[:, 0:512], [pa], [qnT])
                    t1, t2 = rt2
                    K.tt("vector", t1[:, :], pa[:, 0:512], tabs["cos64"][:, :], ALU.mult, [pa, tabs["cos64"]], [t1])
                    K.tt("vector", t2[:, :], pb[:, 0:512], tabs["sin64"][:, :], ALU.mult, [pb, tabs["sin64"]], [t2])
                    K.tt("gpsimd", qrT[:, g_, :], t1[:, :], t2[:, :], ALU.add, [t1, t2], [qrT])
                if lvl >= 0.9:
                    K.dma("sync", wG[:], wq_d[38].rearrange("p (c m) -> p c m", c=8), [wq_tok], [wG])
                    proj_fm(hTc, (wG[:, :, 0:32], wG), 32, gT[:, :], gT, func=ACT.Sigmoid)
                K.dump(f"qaT{c}", qaT[:].rearrange("p a b -> p (a b)"), [128, 2048], BF16, [qaT.k])
                K.dump(f"qiT{c}", qiT[:].rearrange("p a b -> p (a b)"), [96, 1536], BF16, [qiT.k])
                K.dump(f"qrT{c}", qrT[:].rearrange("p a b -> p (a b)"), [128, 2048], BF16, [qrT.k])
                K.dump(f"gT{c}", gT[:], [32, 512], BF16, [gT.k])

                NIT = 14
                tiles_c = ([q for q in qt_list if q // 4 == c] if lvl >= 2 else [])

                def pre_a(qt):
                    t0 = qt * 128
                    tl = (qt % 4) * 128
                    tsl = slice(tl, tl + 128)
                    n = t0 + 128
                    wi_ = wi_sb2[qt % 2]
                    smb = smb2[qt % 2]
                    pw_ = bank()
                    for k in range(8):
                        K.mm(pw_[:, 0:8], hTc[:, k, tsl], wG[:, k, 24:32], k == 0, k == 7, [hTc, wG], [pw_])
                    K.cp("vector", wi_[:], pw_[:, 0:8], [pw_], [wi_])
                    nsc = (n + 511) // 512
                    ri = 0
                    for sc_i in range(nsc):
                        c0 = sc_i * 512
                        ncol = min(512, n - c0)
                        for h in range(8):
                            q_, r_ = h // 3, h % 3
                            pi_ = bank()
                            K.mm(pi_[:, 0:ncol], qiT[32 * r_:32 * r_ + 32, q_, tsl], kiT3[32 * r_:32 * r_ + 32, c0:c0 + ncol],
                                 True, True, [qiT, kiT3], [pi_])
                            Rb = (R0, R1)[ri % 2]; ri += 1
                            K.act(Rb[:, 0:ncol], pi_[:, 0:ncol], ACT.Relu, [pi_], [Rb])
                            if h == 0:
                                K.ts("vector", idx[:, c0:c0 + ncol], Rb[:, 0:ncol], wi_[:, 0:1], None, ALU.mult, None, [Rb, wi_], [idx])
                            else:
                                K.stt("vector", idx[:, c0:c0 + ncol], Rb[:, 0:ncol], wi_[:, h:h + 1], idx[:, c0:c0 + ncol],
                                      ALU.mult, ALU.add, [Rb, wi_, idx], [idx])
                    P.op("vector", lambda e, n=n: e.tensor_reduce(out=smb[:, 0:1], in_=idx[:, 0:n], axis=AX.X, op=ALU.max), K._tk([idx]), K._tk([smb]))
                    P.op("vector", lambda e, n=n: e.tensor_reduce(out=smb[:, 1:2], in_=idx[:, 0:n], axis=AX.X, op=ALU.min), K._tk([idx]), K._tk([smb]))
                    K.asel(idx[:, t0:t0 + 128], idx[:, t0:t0 + 128], [[-1, 128]], ALU.is_ge, -1e30, 0, 1, [idx], [idx])
                    K.ts("vector", smb[:, 2:3], smb[:, 1:2], -1.0, None, ALU.add, None, [smb], [smb])
                    K.stt("vector", smb[:, 3:4], smb[:, 0:1], 1.0, smb[:, 2:3], ALU.add, ALU.subtract, [smb], [smb])
                    K.memset("vector", smb[:, 8:8 + NIT], 0.0, [smb])

                def bis_step(qt, it):
                    n = qt * 128 + 128
                    smb = smb2[qt % 2]
                    f = 2.0 ** -(it + 1)
                    K.stt("vector", smb[:, 4:5], smb[:, 3:4], f, smb[:, 2:3], ALU.mult, ALU.add, [smb], [smb])
                    K.ts("vector", mask[:, 0:n], idx[:, 0:n], smb[:, 4:5], 0.0, ALU.is_ge, ALU.add, [idx, smb, mask], [mask, smb],
                         accum_out=smb[:, 8 + it:9 + it])
                    K.ts("vector", smb[:, 5:6], smb[:, 8 + it:9 + it], 256.0, f, ALU.is_ge, ALU.mult, [smb], [smb])
                    K.stt("vector", smb[:, 2:3], smb[:, 3:4], smb[:, 5:6], smb[:, 2:3], ALU.mult, ALU.add, [smb], [smb])

                def pre_c(qt):
                    n = qt * 128 + 128
                    smb = smb2[qt % 2]
                    K.ts("vector", mask[:, 0:n], idx[:, 0:n], smb[:, 2:3], None, ALU.is_ge, None, [idx, smb], [mask])
                    K.dump(f"mask{qt}", mask[:, 0:n], [128, n], BF16, [mask.k])
                    if lvl < 3:
                        return
                    for b0 in range(0, qt + 1, 8):
                        nb = min(8, qt + 1 - b0)
                        pm_ = bank()
                        for i in range(nb):
                            si = b0 + i
                            K.tr(bfv(pm_)[:, i * 128:(i + 1) * 128], mask[:, si * 128:(si + 1) * 128], ident[:], [mask, ident], [pm_])
                        K.cp("scalar", maskT[:, b0:b0 + nb, :], bfv(pm_)[:, 0:nb * 128].rearrange("p (a b) -> p a b", b=128), [pm_], [maskT])

                if tiles_c:
                    pre_a(tiles_c[0])
                    for it in range(NIT):
                        bis_step(tiles_c[0], it)
                    pre_c(tiles_c[0])
                for qj, qt in enumerate(tiles_c):
                    t0 = qt * 128
                    tl = (qt % 4) * 128
                    tsl = slice(tl, tl + 128)
                    n = t0 + 128
                    nxt = tiles_c[qj + 1] if qj + 1 < len(tiles_c) else None
                    if lvl < 3:
                        if nxt is not None:
                            pre_a(nxt)
                            for it in range(NIT):
                                bis_step(nxt, it)
                            pre_c(nxt)
                        continue
                    steps_left = list(range(NIT)) if nxt is not None else []
                    if nxt is not None:
                        pre_a(nxt)

                    def bis_hook():
                        if steps_left:
                            bis_step(nxt, steps_left.pop(0))

                    def dsa_mm(un):
                        si, u = un
                        ssl = slice(si * 128, (si + 1) * 128)
                        lo = 64 * u
                        ps_ = bank()
                        K.mm(ps_[:, 0:512].rearrange("p (g t) -> p g t", g=4), kaT2[lo:lo + 64, ssl], qaT[lo:lo + 64, :, tsl],
                             True, True, [kaT2, qaT], [ps_])
                        return ps_

                    def dsa_post(un, ps_):
                        si, u = un
                        E_, Pm_ = Enext()
                        K.act(E_[:, :], ps_[:, 0:512], ACT.Exp, [ps_], [E_], scale=SC)
                        K.tt("vector", Pm_[:, :].rearrange("p (g t) -> p g t", g=4), E_[:, :].rearrange("p (g t) -> p g t", g=4),
                             bc(maskT[:, si, :].unsqueeze(1), [128, 4, 128]), ALU.mult, [E_, maskT], [Pm_])
                        return Pm_

                    def dsa_qk(un):
                        return dsa_post(un, dsa_mm(un))
                    dsa_qk.mm = dsa_mm
                    dsa_qk.post = dsa_post

                    def dsa_pv(un, Pm_):
                        si, u = un
                        lo = 64 * u
                        K.mm(A_banks[u][:, 0:512], va3[:, si, lo:lo + 128], Pm_[:, :], si == 0, si == qt, [va3, Pm_], [A_banks[u]])

                    pipe([(si, u) for si in range(qt + 1) for u in range(2)], dsa_qk, dsa_pv, hook=bis_hook)
                    for u in range(2):
                        lo = 64 * u; lr = 64 * (1 - u)
                        K.recip(rs[lo:lo + 64, :], A_banks[u][lr:lr + 64, 0:512], [A_banks[u]], [rs])
                        K.tt("vector", oaTc[lo:lo + 64, :, tsl], A_banks[u][lo:lo + 64, 0:512].rearrange("p (g t) -> p g t", g=4),
                             rs[lo:lo + 64, :].rearrange("p (g t) -> p g t", g=4), ALU.mult, [A_banks[u], rs], [oaTc])
                    while steps_left:
                        bis_step(nxt, steps_left.pop(0))
                    if nxt is not None:
                        pre_c(nxt)
                    if lvl < 4:
                        continue
                    for k in range(2):
                        lo = 64 * k
                        for gp in range(2):
                            ps_ = bank()
                            for jj in range(2):
                                g_ = 2 * gp + jj
                                K.mm(ps_[:, jj * 256:(jj + 1) * 256], qnT[lo:lo + 64, g_, tsl], kcT2[lo:lo + 64, 0:256], True, True, [qnT, kcT2], [ps_])
                            h0 = 4 * k + 2 * gp
                            K.act(Ecmp[:, h0:h0 + 2, :], ps_[:, 0:512].rearrange("p (a n) -> p a n", a=2), ACT.Exp, [ps_], [Ecmp], scale=SC)
                    K.asel(Ecmp[:], Ecmp[:], [[0, 8], [-16, 256]], ALU.is_ge, 0.0, t0 - 31, 1, [Ecmp], [Ecmp])
                    P.op("vector", lambda e: e.tensor_reduce(out=sm[:, 40:48], in_=Ecmp[:], axis=AX.X, op=ALU.add), K._tk([Ecmp]), K._tk([sm]))
                    K.ts("vector", sm[:, 40:48], sm[:, 40:48], 1e-30, None, ALU.add, None, [sm], [sm])
                    K.recip(sm[:, 48:56], sm[:, 40:48], [sm], [sm])
                    K.tt("vector", Ecmp[:], Ecmp[:], bc(sm[:, 48:56].unsqueeze(2), [128, 8, 256]), ALU.mult, [Ecmp, sm], [Ecmp])
                    K.cp("gpsimd", p_bf[:], Ecmp[:], [Ecmp], [p_bf])
                    P.op("vector", lambda e: e.tensor_reduce(out=P4[:], in_=Ecmp[:].rearrange("p (k g) n -> p k n g", k=2), axis=AX.X, op=ALU.add),
                         K._tk([Ecmp]), K._tk([P4]))
                    P.op("vector", lambda e: e.tensor_reduce(out=imp[:], in_=P4[:].rearrange("p k (j i) -> p k j i", i=4), axis=AX.X, op=ALU.add),
                         K._tk([P4]), K._tk([imp]))
                    K.tt("vector", imp[:, :, 1:64], imp[:, :, 1:64], P4[:, :, 3:252:4], ALU.add, [imp, P4], [imp])
                    K.dma("sync", selb[0:64, :], c_selb_d[2 * qt:2 * qt + 1, :].partition_broadcast(64), [], [selb])
                    K.dma("sync", selb[64:128, :], c_selb_d[2 * qt + 1:2 * qt + 2, :].partition_broadcast(64), [], [selb])
                    K.tt("vector", scs[:], imp[:], bc(selb[:, :].unsqueeze(1), [128, 2, 64]), ALU.add, [imp, selb], [scs])
                    for k in range(2):
                        P.op("vector", lambda e, k=k: e.max(out=sm[:, 16:24], in_=scs[:, k, :]), K._tk([scs]), K._tk([sm]))
                        P.op("vector", lambda e, k=k: e.match_replace(out=sc2[:], in_to_replace=sm[:, 16:24], in_values=scs[:, k, :], imm_value=-1e9),
                             K._tk([scs, sm]), K._tk([sc2]))
                        P.op("vector", lambda e: e.max(out=sm[:, 24:32], in_=sc2[:]), K._tk([sc2]), K._tk([sm]))
                        K.ts("vector", bm[:, k, :], scs[:, k, :], sm[:, 31:32], None, ALU.is_ge, None, [scs, sm], [bm])
                    K.dump(f"bm{qt}", bm[:].rearrange("p a b -> p (a b)"), [128, 128], BF16, [bm.k])
                    pb_ = bank()
                    for k in range(2):
                        K.tr(bfv(pb_)[0:64, k * 128:(k + 1) * 128], bm[:, k, :], ident[:], [bm, ident], [pb_])
                    K.cp("scalar", bmT[:], bfv(pb_)[0:64, 0:256].rearrange("p (k t) -> p k t", k=2), [pb_], [bmT])
                    for ch in range(2):
                        pp_ = bank()
                        for h in range(8):
                            K.tr(bfv(pp_)[:, h * 128:(h + 1) * 128], p_bf[:, h, ch * 128:(ch + 1) * 128], ident[:], [p_bf, ident], [pp_])
                        K.cp("scalar", pT[:, ch, :, :], bfv(pp_).rearrange("p (h t) -> p h t", h=8), [pp_], [pT])
                    for k in range(2):
                        for ch in range(2):
                            K.mm(B_banks[k][:, 0:512].rearrange("p (g t) -> p g t", g=4), vctm[:, ch, :], pT[:, ch, 4 * k:4 * k + 4, :],
                                 ch == 0, ch == 1, [vctm, pT], [B_banks[k]])
                    def gate_bcast(cidx, k):
                        pg_ = bank()
                        K.mm(pg_[:, 0:512].rearrange("p (g t) -> p g t", g=4), ones24[:, :], Dg[:, 4 * k:4 * k + 4, :], True, True, [ones24, Dg], [pg_])
                        gb_ = gBs[k]
                        K.cp("scalar", gb_[64 * k:64 * k + 64, :], pg_[64 * k:64 * k + 64, 0:512], [pg_], [gb_])
                        return gb_

                    def make_Dg(cidx):
                        K.tt("vector", Dg[:], bc(gT[0:24, tsl].unsqueeze(1), [24, 8, 128]),
                             bc(eye24[:, cidx * 8:cidx * 8 + 8].unsqueeze(2), [24, 8, 128]), ALU.mult, [gT, eye24], [Dg])

                    make_Dg(0)
                    for k in range(2):
                        lo = 64 * k
                        gb_ = gate_bcast(0, k)
                        K.tt("vector", ob[lo:lo + 64, :], B_banks[k][lo:lo + 64, 0:512], gb_[lo:lo + 64, :], ALU.mult, [B_banks[k], gb_], [ob])
                    if lvl < 5:
                        continue
                    mes = {}

                    def slc_mm(un):
                        si, k = un
                        ssl = slice(si * 128, (si + 1) * 128)
                        if k == 0:
                            pm_ = bank()
                            K.mm(pm_[:, 0:256].rearrange("p (k t) -> p k t", k=2), Esel[:, si, :], bmT[:, :, :], True, True, [Esel, bmT], [pm_])
                            me = mexp[si % 4]
                            K.cp("scalar", me[:], pm_[:, 0:256].rearrange("p (k t) -> p k t", k=2), [pm_], [me])
                            if si == qt:
                                K.tt("gpsimd", me[:], me[:], bc(diagT[:, :].unsqueeze(1), [128, 2, 128]), ALU.mult, [me, diagT], [me])
                            mes[si] = me
                        lo = 64 * k
                        ps_ = bank()
                        K.mm(ps_[:, 0:512].rearrange("p (g t) -> p g t", g=4), ksT[lo:lo + 64, ssl], qrT[lo:lo + 64, :, tsl], True, True, [ksT, qrT], [ps_])
                        return ps_

                    def slc_post(un, ps_):
                        si, k = un
                        me = mes[si]
                        E_, Pm_ = Enext()
                        K.act(E_[:, :], ps_[:, 0:512], ACT.Exp, [ps_], [E_], scale=SC)
                        K.tt("vector", Pm_[:, :].rearrange("p (g t) -> p g t", g=4), E_[:, :].rearrange("p (g t) -> p g t", g=4),
                             bc(me[:, k, :].unsqueeze(1), [128, 4, 128]), ALU.mult, [E_, me], [Pm_])
                        return Pm_

                    def slc_qk(un):
                        return slc_post(un, slc_mm(un))
                    slc_qk.mm = slc_mm
                    slc_qk.post = slc_post

                    def slc_pv(un, Pm_):
                        si, k = un
                        lo = 64 * k
                        K.mm(A_banks[k][:, 0:512], vs3[:, si, lo:lo + 128], Pm_[:, :], si == 0, si == qt, [vs3, Pm_], [A_banks[k]])

                    pipe([(si, k) for si in range(qt + 1) for k in range(2)], slc_qk, slc_pv)

                    def fin(acc, cidx, last):
                        make_Dg(cidx)
                        for k in range(2):
                            lo = 64 * k; lr = 64 * (1 - k)
                            gb_ = gate_bcast(cidx, k)
                            K.recip(rs[lo:lo + 64, :], acc[k][lr:lr + 64, 0:512], [acc[k]], [rs])
                            K.tt("gpsimd", rs[lo:lo + 64, :], rs[lo:lo + 64, :], gb_[lo:lo + 64, :], ALU.mult, [rs, gb_], [rs])
                            tmp = R1
                            K.tt("vector", tmp[lo:lo + 64, :], acc[k][lo:lo + 64, 0:512], rs[lo:lo + 64, :], ALU.mult, [acc[k], rs], [tmp])
                            if not last:
                                K.tt("gpsimd", ob[lo:lo + 64, :], ob[lo:lo + 64, :], tmp[lo:lo + 64, :], ALU.add, [ob, tmp], [ob])
                            else:
                                K.tt("gpsimd", obTc[lo:lo + 64, :, tsl], ob[lo:lo + 64, :].rearrange("p (g t) -> p g t", g=4),
                                     tmp[lo:lo + 64, :].rearrange("p (g t) -> p g t", g=4), ALU.add, [ob, tmp], [obTc])

                    fin(A_banks, 1, False)
                    if lvl < 6:
                        continue
                    s_lo = max(0, qt - 4)

                    def win_mm(un):
                        si, k = un
                        ssl = slice(si * 128, (si + 1) * 128)
                        lo = 64 * k
                        ps_ = bank()
                        K.mm(ps_[:, 0:512].rearrange("p (g t) -> p g t", g=4), kwT[lo:lo + 64, ssl], qrT[lo:lo + 64, :, tsl], True, True, [kwT, qrT], [ps_])
                        return ps_

                    def win_post(un, ps_):
                        si, k = un
                        E_, Pm_ = Enext()
                        K.act(E_[:, :], ps_[:, 0:512], ACT.Exp, [ps_], [E_], scale=SC)
                        mk = diagT if si == qt else (antiT if si == qt - 4 else None)
                        src = E_
                        if mk is not None:
                            K.tt("vector", Pm_[:, :].rearrange("p (g t) -> p g t", g=4), E_[:, :].rearrange("p (g t) -> p g t", g=4),
                                 bc(mk[:, :].unsqueeze(1), [128, 4, 128]), ALU.mult, [E_, mk], [Pm_])
                            src = Pm_
                        return src

                    def win_qk(un):
                        return win_post(un, win_mm(un))
                    win_qk.mm = win_mm
                    win_qk.post = win_post

                    def win_pv(un, src):
                        si, k = un
                        lo = 64 * k
                        K.mm(B_banks[k][:, 0:512], vw3[:, si, lo:lo + 128], src[:, :], si == s_lo, si == qt, [vw3, src], [B_banks[k]])

                    pipe([(si, k) for si in range(s_lo, qt + 1) for k in range(2)], win_qk, win_pv)
                    fin(B_banks, 2, True)

                for qt in [q for q in qt_list if q // 4 == c]:
                    tl = (qt % 4) * 128
                    for nm_, bt_ in (("oaT", oaTc), ("obT", obTc)):
                        if f"{nm_}{qt}" in K.dbg:
                            d_ = nc.dram_tensor(f"dbg_{nm_}{qt}", [128, 4, 128], BF16, kind="ExternalOutput").ap()
                            K.dma("sync", d_, bt_[:, :, tl:tl + 128], [bt_], [])
                if lvl < 7:
                    continue
                for u in range(2):
                    K.dma("gpsimd", wbra[64 * u:64 * u + 64, :, :], w_br_a_d[256 * u:256 * (u + 1), :].rearrange("(g d) f -> d g f", d=64), [], [wbra])
                    K.dma("gpsimd", wbrb[64 * u:64 * u + 64, :, :], w_br_b_d[256 * u:256 * (u + 1), :].rearrange("(g d) f -> d g f", d=64), [], [wbrb])
                K.dma("gpsimd", wout[:], w_out_d.rearrange("(c p) f -> p c f", p=128), [], [wout])
                for fc in range(8):
                    fsl = slice(fc * 128, (fc + 1) * 128)
                    outs_ = []
                    for br, (gbase, wbr, oT) in enumerate(((22, wbra, oaTc), (30, wbrb, obTc))):
                        wg_ = load_ws(gbase + fc, wsi[0]); wsi[0] += 1
                        pg_ = bank()
                        for k in range(8):
                            K.mm(pg_[:, 0:512], wg_[:, k, :], hTc[:, k, :], k == 0, k == 7, [wg_, hTc], [pg_])
                        E_, Pm_ = Enext()
                        K.act(E_[:, :], pg_[:, 0:512], ACT.Sigmoid, [pg_], [E_])
                        pbr = bank()
                        for g_ in range(4):
                            K.mm(pbr[:, 0:512], wbr[:, g_, fsl], oT[:, g_, :], g_ == 0, g_ == 3, [wbr, oT], [pbr])
                        K.tt("vector", Pm_[:, :], pbr[:, 0:512], E_[:, :], ALU.mult, [pbr, E_], [Pm_])
                        outs_.append(Pm_)
                    K.tt("gpsimd", mergedT[:, fc, :], outs_[0][:, :], outs_[1][:, :], ALU.add, [outs_[0], outs_[1]], [mergedT])
                K.dump(f"mergedT{c}", mergedT[:].rearrange("p a b -> p (a b)"), [128, 4096], BF16, [mergedT.k])
                for j in range(4):
                    tt_ = 4 * c + j
                    xt = xb[0]
                    K.dma("sync", xt[:], x_d[tt_ * 128:(tt_ + 1) * 128, :], [], [xt])
                    for half in range(2):
                        po_ = bank()
                        for fc in range(8):
                            K.mm(po_[:, 0:512], mergedT[:, fc, j * 128:(j + 1) * 128], wout[:, fc, half * 512:(half + 1) * 512],
                                 fc == 0, fc == 7, [mergedT, wout], [po_])
                        K.tt("vector", xt[:, half * 512:(half + 1) * 512], po_[:, 0:512], xt[:, half * 512:(half + 1) * 512], ALU.add, [po_, xt], [xt])
                    K.dma("sync", x1_d[tt_ * 128:(tt_ + 1) * 128, :], xt[:], [xt], [x1_tok])
                    K.dump(f"x1_{tt_}", xt[:], [128, D], F32, [xt.k])
            P.emit()
        if stop_after == "C":
            P.emit(final=True)
            att.close()
            return nc, K
        att.close()
        if moe_from_x:
            x1_d = x_d
        with ExitStack() as pm:
            HT = 16
            identf = K.sb(pm, [128, 128])
            gffn = K.sb(pm, [128, 8])
            gfin = K.sb(pm, [128, D])
            wr = K.sb(pm, [128, 8, 36])
            rb = K.sb(pm, [128, 36])
            h2T = K.sb(pm, [128, 8, HT * 128], BF16)
            yacc = K.sb(pm, [128, HT, D])
            wgt = K.sb(pm, [128, HT, 32])
            wgu = [K.sb(pm, [128, 8, 512], BF16) for _ in range(2)]
            wdn = [K.sb(pm, [128, 2, D], BF16) for _ in range(2)]
            aT = [K.sb(pm, [128, 2, 512], BF16) for _ in range(2)]
            sg = [K.sb(pm, [128, 512], BF16) for _ in range(2)]
            xm = [K.sb(pm, [128, D]) for _ in range(2)]
            xn = K.sb(pm, [128, D])
            h2f = K.sb(pm, [128, 8, 128])
            sq2 = K.sb(pm, [128, D], BF16)
            s2 = K.sb(pm, [128, 16])
            lg = K.sb(pm, [128, 36])
            me = K.sb(pm, [128, 32])
            ex = K.sb(pm, [128, 32])
            m8 = K.sb(pm, [128, 8])
            K.memset("gpsimd", identf[:], 1.0, [identf])
            K.asel(identf[:], identf[:], [[-1, 128]], ALU.is_equal, 0.0, 0, 1, [identf], [identf])
            K.dma("sync", gffn[:], norm_ffn_d, [], [gffn])
            K.dma("sync", gfin[:], norm_final_d.partition_broadcast(128), [], [gfin])
            K.dma("sync", wr[:, :, 0:4], w_group_d.rearrange("(c p) n -> p c n", p=128), [], [wr])
            K.dma("sync", wr[:, :, 4:36], w_expert_d.rearrange("(c p) n -> p c n", p=128), [], [wr])
            K.dma("sync", rb[:, 0:4], b_group_d.partition_broadcast(128), [], [rb])
            K.dma("sync", rb[:, 4:36], b_expert_d.partition_broadcast(128), [], [rb])
            BIG = 30000.0
            xi = [0]
            wl = [0]
            for half in range(2):
                for j in range(HT):
                    tt_ = half * HT + j
                    xt = xm[xi[0] % 2]; xi[0] += 1
                    K.dma("sync", xt[:], x1_d[tt_ * 128:(tt_ + 1) * 128, :], [x1_tok], [xt])
                    K.memset("vector", s2[:, 0:1], 0.0, [s2])
                    K.act(sq2[:], xt[:], ACT.Square, [xt, s2], [sq2, s2], accum_out=s2[:, 0:1])
                    K.act(s2[:, 1:2], s2[:, 0:1], ACT.Sqrt, [s2, cpi], [s2], scale=1.0 / D, bias=cpi[:, 1:2])
                    K.recip(s2[:, 2:3], s2[:, 1:2], [s2], [s2])
                    K.ts("vector", xn[:], xt[:], s2[:, 2:3], None, ALU.mult, None, [xt, s2], [xn])
                    for hb in range(2):
                        pb = bank()
                        for k in range(4):
                            kk = hb * 4 + k
                            K.tr(pb[:, k * 128:(k + 1) * 128], xn[:, kk * 128:(kk + 1) * 128], identf[:], [xn, identf], [pb])
                        K.tt("vector", h2f[:, hb * 4:hb * 4 + 4, :], pb[:, 0:512].rearrange("p (k t) -> p k t", k=4),
                             bc(gffn[:, hb * 4:hb * 4 + 4].unsqueeze(2), [128, 4, 128]), ALU.mult, [pb, gffn], [h2f])
                    K.cp("scalar", h2T[:, :, j * 128:(j + 1) * 128], h2f[:], [h2f], [h2T])
                    pr = bank()
                    for k in range(8):
                        K.mm(pr[:, 0:36], h2f[:, k, :], wr[:, k, :], k == 0, k == 7, [h2f, wr], [pr])
                    K.tt("vector", lg[:], pr[:, 0:36], rb[:], ALU.add, [pr, rb], [lg])
                    P.op("vector", lambda e: e.tensor_reduce(out=s2[:, 4:5], in_=lg[:, 0:4], axis=AX.X, op=ALU.max), K._tk([lg]), K._tk([s2]))
                    K.ts("vector", s2[:, 5:6], s2[:, 4:5], -1.0, None, ALU.mult, None, [s2], [s2])
                    K.memset("vector", s2[:, 6:7], 0.0, [s2])
                    K.act(ex[:, 0:4], lg[:, 0:4], ACT.Exp, [lg, s2], [ex, s2], bias=s2[:, 5:6], accum_out=s2[:, 6:7])
                    K.recip(s2[:, 7:8], s2[:, 6:7], [s2], [s2])
                    K.ts("vector", ex[:, 4:8], lg[:, 0:4], s2[:, 4:5], BIG, ALU.is_ge, ALU.mult, [lg, s2], [ex])
                    K.ts("vector", ex[:, 4:8], ex[:, 4:8], -BIG, None, ALU.add, None, [ex], [ex])
                    K.tt("vector", me[:].rearrange("p (g i) -> p g i", g=4), lg[:, 4:36].rearrange("p (g i) -> p g i", g=4),
                         bc(ex[:, 4:8].unsqueeze(2), [128, 4, 8]), ALU.add, [lg, ex], [me])
                    P.op("vector", lambda e: e.max(out=m8[:], in_=me[:]), K._tk([me]), K._tk([m8]))
                    K.ts("vector", s2[:, 8:9], m8[:, 0:1], -1.0, None, ALU.mult, None, [m8], [s2])
                    K.act(ex[:], me[:], ACT.Exp, [me, s2], [ex], bias=s2[:, 8:9])
                    K.act(s2[:, 9:10], m8[:, 1:2], ACT.Exp, [m8, s2], [s2], bias=s2[:, 8:9])
                    K.ts("vector", s2[:, 9:10], s2[:, 9:10], 1.0, None, ALU.add, None, [s2], [s2])
                    K.recip(s2[:, 10:11], s2[:, 9:10], [s2], [s2])
                    K.tt("vector", s2[:, 11:12], s2[:, 10:11], s2[:, 7:8], ALU.mult, [s2], [s2])
                    K.ts("vector", me[:], me[:], m8[:, 1:2], None, ALU.is_ge, None, [me, m8], [me])
                    K.tt("vector", ex[:], ex[:], me[:], ALU.mult, [ex, me], [ex])
                    K.ts("vector", wgt[:, j, :], ex[:], s2[:, 11:12], None, ALU.mult, None, [ex, s2], [wgt])
                K.dump(f"wgt{half}", wgt[:].rearrange("p a b -> p (a b)"), [128, HT * 32], F32, [wgt.k])
                wts = {}

                def moe_up(item):
                    e_, tch = item
                    if tch == 0:
                        wg_ = wgu[wl[0] % 2]; wd_ = wdn[wl[0] % 2]; wl[0] += 1
                        K.dma("gpsimd", wg_[:], w_gu_d[e_].rearrange("(c p) f -> p c f", p=128), [], [wg_])
                        K.dma("gpsimd", wd_[:], w_dn_d[e_].rearrange("(c p) f -> p c f", p=128), [], [wd_])
                        wts[e_] = (wg_, wd_)
                    wg_, wd_ = wts[e_]
                    a_ = aT[tch % 2]
                    csl = slice(tch * 512, (tch + 1) * 512)
                    for fo in range(2):
                        pg_ = bank(); pu_ = bank()
                        for k in range(8):
                            K.mm(pg_[:, 0:512], wg_[:, k, fo * 128:(fo + 1) * 128], h2T[:, k, csl], k == 0, k == 7, [wg_, h2T], [pg_])
                        for k in range(8):
                            K.mm(pu_[:, 0:512], wg_[:, k, 256 + fo * 128:256 + (fo + 1) * 128], h2T[:, k, csl], k == 0, k == 7, [wg_, h2T], [pu_])
                        s_ = sg[fo]
                        K.act(s_[:, :], pg_[:, 0:512], ACT.Silu, [pg_], [s_])
                        K.tt("vector", a_[:, fo, :], pu_[:, 0:512], s_[:, :], ALU.mult, [pu_, s_], [a_])

                def moe_down(item):
                    e_, tch = item
                    wg_, wd_ = wts[e_]
                    a_ = aT[tch % 2]
                    for tj in range(4):
                        tile_ = tch * 4 + tj
                        for hf in range(2):
                            po_ = bank((4, 5, 6, 7))
                            for fo in range(2):
                                K.mm(po_[:, 0:512], a_[:, fo, tj * 128:(tj + 1) * 128], wd_[:, fo, hf * 512:(hf + 1) * 512],
                                     fo == 0, fo == 1, [a_, wd_], [po_])
                            ysl = yacc[:, tile_, hf * 512:(hf + 1) * 512]
                            if e_ == 0:
                                K.ts("vector", ysl, po_[:, 0:512], wgt[:, tile_, e_:e_ + 1], None, ALU.mult, None, [po_, wgt], [yacc])
                            else:
                                K.stt("vector", ysl, po_[:, 0:512], wgt[:, tile_, e_:e_ + 1], ysl, ALU.mult, ALU.add, [po_, wgt, yacc], [yacc])

                items = [(e_, tch) for e_ in range(moe_experts) for tch in range(HT // 4)]
                for i_, it_ in enumerate(items):
                    moe_up(it_)
                    if i_ >= 1:
                        moe_down(items[i_ - 1])
                moe_down(items[-1])
                for j in range(HT):
                    tt_ = half * HT + j
                    xt = xm[xi[0] % 2]; xi[0] += 1
                    K.dma("sync", xt[:], x1_d[tt_ * 128:(tt_ + 1) * 128, :], [x1_tok], [xt])
                    K.tt("gpsimd", xt[:], xt[:], yacc[:, j, :], ALU.add, [xt, yacc], [xt])
                    K.memset("vector", s2[:, 12:13], 0.0, [s2])
                    K.act(sq2[:], xt[:], ACT.Square, [xt, s2], [sq2, s2], accum_out=s2[:, 12:13])
                    K.act(s2[:, 13:14], s2[:, 12:13], ACT.Sqrt, [s2, cpi], [s2], scale=1.0 / D, bias=cpi[:, 1:2])
                    K.recip(s2[:, 14:15], s2[:, 13:14], [s2], [s2])
                    K.stt("vector", xn[:], xt[:], s2[:, 14:15], gfin[:], ALU.mult, ALU.mult, [xt, s2, gfin], [xn])
                    K.dma("sync", out_d[tt_ * 128:(tt_ + 1) * 128, :], xn[:], [xn], [])
            P.emit()
        P.emit(final=True)
    return nc, K


def _consts():
    p = np.arange(128)
    invf = np.stack([10000.0 ** (-(p % 32).astype(np.float32) / 32.0),
                     10000.0 ** (-(p % 16).astype(np.float32) / 16.0)], axis=1).astype(np.float32)
    selb = np.zeros((64, 64), np.float32)
    for c in range(64):
        for j in range(64):
            if j > c:
                selb[c, j] = -100.0
            elif j == 0 or j == c or j == c - 1:
                selb[c, j] = 100.0
    esel = np.zeros((64, 32, 128), np.float32)
    for i in range(32):
        for s_ in range(128):
            esel[2 * i + s_ // 64, i, s_] = 1.0
    return invf, selb, esel.reshape(64, 4096), np.eye(24, dtype=np.float32)


def make_in_map(inp, b):
    invf, selb, esel, eye = _consts()
    f = lambda a: np.ascontiguousarray(np.asarray(a))
    return {
        "x": f(inp["x"][b]), "positions": f(inp["positions"][b][None, :]),
        "norm_mix": f(np.asarray(inp["norm_mix"][0]).reshape(8, 128).T), "w_in": f(inp["w_in"][0]),
        "pe_k": f(inp["pe_k"][0]), "w1_k": f(inp["w1_k"][0]), "w2_k": f(inp["w2_k"][0]),
        "pe_v": f(inp["pe_v"][0]), "w1_v": f(inp["w1_v"][0]), "w2_v": f(inp["w2_v"][0]),
        "w_br_a": f(inp["w_br_a"][0]), "w_br_b": f(inp["w_br_b"][0]), "w_out": f(inp["w_out"][0]),
        "norm_ffn": f(np.asarray(inp["norm_ffn"][0]).reshape(8, 128).T),
        "w_group": f(inp["w_group"][0]), "b_group": f(inp["b_group"][0][None, :]),
        "w_expert": f(inp["w_expert"][0]), "b_expert": f(inp["b_expert"][0][None, :]),
        "w_gate_up": f(inp["w_gate_up"][0]), "w_down": f(inp["w_down"][0]),
        "norm_final": f(inp["norm_final"][None, :]),
        "c_invf": invf, "c_selb": selb, "c_esel": esel, "c_eye": eye,
    }


def kernel(**inputs):
    nc, K = build()
    in_maps = [make_in_map(inputs, b) for b in range(8)]
    res = run_bass_kernel_spmd(nc, in_maps, core_ids=list(range(8)))
    return np.stack([np.asarray(r["out"]) for r in res.results], axis=0).astype(np.float32)
```
